# Optimizing a Trainium2 kernel written in Bass

```python
import math
import jax, jax.numpy as jnp
from jax import lax
import numpy as np


D_MODEL = 2048
BATCH = 1
SEQ = 8192
DEPTH = 4

D_MIX = D_MODEL
HEAD_DIM = 128
A_HEADS = 6
A_WIDTH = A_HEADS * HEAD_DIM
IDX_HEADS = 8
IDX_DIM = 64
TOPK_MAX = 256
M_HEADS = 6
M_DH = HEAD_DIM
M_WIDTH = M_HEADS * M_DH
M_CHUNK = 64
CONV_W = 4
C_HEADS = 4
C_QK = 64
C_DV = 2 * C_QK
C_WIDTH = C_HEADS * C_DV
D_FF = 4 * D_MODEL
Q_BLOCK = 128
ALPHA = (2.0 * DEPTH) ** 0.25
BETA = (8.0 * DEPTH) ** -0.25
EPS = 1e-5

SIZES = (A_WIDTH, A_WIDTH, A_WIDTH, IDX_HEADS * IDX_DIM, IDX_DIM, IDX_HEADS,
         M_WIDTH, M_WIDTH, M_WIDTH, M_WIDTH, M_HEADS, M_HEADS,
         C_HEADS * 2 * C_QK, C_HEADS * 2 * C_QK, C_WIDTH)
VALUE_SEGMENTS = (2, 8, 14)
D_IN = sum(SIZES)
SPLIT_POINTS = tuple(int(p) for p in np.cumsum(SIZES)[:-1])

kernel_name = "hybrid_dsa_mlstm_diffattn_deepnorm"


def alibi_slopes(n):
    return 2.0 ** (-8.0 * jnp.arange(1, n + 1, dtype=jnp.float32) / n)


def layer_norm(x, g, b):
    xf = x.astype(jnp.float32)
    mu = jnp.mean(xf, axis=-1, keepdims=True)
    var = jnp.mean(jnp.square(xf - mu), axis=-1, keepdims=True)
    return ((xf - mu) * lax.rsqrt(var + EPS) * g + b).astype(x.dtype)


def head_rmsnorm(h, g):
    hf = h.astype(jnp.float32)
    return (hf * lax.rsqrt(jnp.mean(hf * hf, axis=-1, keepdims=True) + EPS) * g).astype(h.dtype)


def causal_conv(x, w):
    return lax.conv_general_dilated(x, w.astype(x.dtype), window_strides=(1,),
                                    padding=[(CONV_W - 1, 0)],
                                    dimension_numbers=('NWC', 'WIO', 'NWC'),
                                    feature_group_count=x.shape[-1])


def dsa_attention(q, k, v, q_idx, k_idx, w_idx):
    B, S, H, D = q.shape
    nb = S // Q_BLOCK
    k_sel = min(TOPK_MAX, S // 4)
    slopes = alibi_slopes(H)
    scale = D ** -0.5
    pos_k = jnp.arange(S)

    def block(args):
        qb, qib, wb, t0 = args
        pos_q = t0 + jnp.arange(Q_BLOCK)
        sc = jax.nn.relu(jnp.einsum('bqhd,bsd->bqhs', qib, k_idx).astype(jnp.float32))
        idx_score = jnp.einsum('bqhs,bqh->bqs', sc, wb.astype(jnp.float32))
        causal = pos_k[None, :] <= pos_q[:, None]
        idx_score = jnp.where(causal[None], idx_score, -jnp.inf)
        _, sel = lax.top_k(idx_score, k_sel)
        valid = sel <= pos_q[None, :, None]
        k_g = jax.vmap(lambda kk, ii: kk[ii])(k, sel)
        v_g = jax.vmap(lambda vv, ii: vv[ii])(v, sel)
        logits = jnp.einsum('bqhd,bqkhd->bhqk', qb, k_g).astype(jnp.float32) * scale
        dist = (pos_q[None, :, None] - sel).astype(jnp.float32)
        logits = logits - slopes[None, :, None, None] * dist[:, None]
        logits = jnp.where(valid[:, None], logits, -jnp.inf)
        p = jax.nn.softmax(logits, axis=-1)
        return jnp.einsum('bhqk,bqkhd->bqhd', p.astype(v.dtype), v_g)

    qs = q.reshape(B, nb, Q_BLOCK, H, D).transpose(1, 0, 2, 3, 4)
    qis = q_idx.reshape(B, nb, Q_BLOCK, IDX_HEADS, IDX_DIM).transpose(1, 0, 2, 3, 4)
    ws = w_idx.reshape(B, nb, Q_BLOCK, IDX_HEADS).transpose(1, 0, 2, 3)
    t0s = jnp.arange(nb) * Q_BLOCK
    out = lax.map(block, (qs, qis, ws, t0s))
    return out.transpose(1, 0, 2, 3, 4).reshape(B, S, H, D)


def mlstm_chunkwise(q, k, v, i_pre, f_pre):
    B, S, H, D = q.shape
    L = M_CHUNK
    nc = S // L
    f32 = jnp.float32

    def chunks(t):
        return t.astype(f32).reshape(B, nc, L, H, D).transpose(1, 0, 3, 2, 4)

    def gchunks(t):
        return t.reshape(B, nc, L, H).transpose(1, 0, 3, 2)

    log_f = gchunks(jax.nn.log_sigmoid(f_pre.astype(f32)))
    log_i = gchunks(i_pre.astype(f32))
    tril = jnp.tril(jnp.ones((L, L), dtype=bool))

    def step(carry, inp):
        C, n, m = carry
        qj, kj, vj, lfj, lij = inp
        a = jnp.cumsum(lfj, axis=-1)
        g = a[..., -1]
        d_intra = a[..., :, None] - a[..., None, :] + lij[..., None, :]
        d_intra = jnp.where(tril, d_intra, -jnp.inf)
        inter = a + m[..., None]
        m_row = jnp.maximum(inter, jnp.max(d_intra, axis=-1))
        w_intra = jnp.exp(d_intra - m_row[..., None])
        w_inter = jnp.exp(inter - m_row)
        qk = jnp.einsum('bhld,bhsd->bhls', qj, kj) * w_intra
        num = jnp.einsum('bhls,bhse->bhle', qk, vj) + w_inter[..., None] * jnp.einsum('bhld,bhde->bhle', qj, C)
        den = jnp.sum(qk, axis=-1) + w_inter * jnp.einsum('bhld,bhd->bhl', qj, n)
        h = num / jnp.maximum(jnp.abs(den), jnp.exp(-m_row))[..., None]
        decay = g[..., None] - a + lij
        m_new = jnp.maximum(g + m, jnp.max(decay, axis=-1))
        ws = jnp.exp(decay - m_new[..., None])
        wc = jnp.exp(g + m - m_new)
        C_new = wc[..., None, None] * C + jnp.einsum('bhs,bhsd,bhse->bhde', ws, kj, vj)
        n_new = wc[..., None] * n + jnp.einsum('bhs,bhsd->bhd', ws, kj)
        return (C_new, n_new, m_new), h

    init = (jnp.zeros((B, H, D, D), f32), jnp.zeros((B, H, D), f32), jnp.zeros((B, H), f32))
    _, hs = lax.scan(step, init, (chunks(q), chunks(k), chunks(v), log_f, log_i))
    return hs.transpose(1, 0, 3, 2, 4).reshape(B, S, H, D).astype(v.dtype)


def diff_attention(q, k, v, lam):
    B, S, H, _, d = q.shape
    nb = S // Q_BLOCK
    slopes = alibi_slopes(H)
    scale = d ** -0.5
    pos_k = jnp.arange(S)

    def block(args):
        qb, t0 = args
        pos_q = t0 + jnp.arange(Q_BLOCK)
        s = jnp.einsum('bqhmd,bshmd->bmhqs', qb, k).astype(jnp.float32) * scale
        dist = (pos_q[:, None] - pos_k[None, :]).astype(jnp.float32)
        s = jnp.where(dist >= 0, s - slopes[:, None, None] * dist, -jnp.inf)
        p = jax.nn.softmax(s, axis=-1)
        attn = p[:, 0] - lam * p[:, 1]
        return jnp.einsum('bhqs,bshe->bqhe', attn.astype(v.dtype), v)

    qs = q.reshape(B, nb, Q_BLOCK, H, 2, d).transpose(1, 0, 2, 3, 4, 5)
    t0s = jnp.arange(nb) * Q_BLOCK
    out = lax.map(block, (qs, t0s))
    return out.transpose(1, 0, 2, 3, 4).reshape(B, S, H, v.shape[-1])


def token_mixers(xn, layer_idx, w_in, conv_w, b_i, b_f, m_norm_g, lq1, lk1, lq2, lk2, c_norm_g):
    B, S, _ = xn.shape
    proj = jnp.einsum('bsd,de->bse', xn, w_in)
    (q_a, k_a, v_a, q_i, k_i, w_i, q_m, k_m, v_m, o_m, i_m, f_m,
     q_c, k_c, v_c) = jnp.split(proj, SPLIT_POINTS, axis=-1)

    y_a = dsa_attention(q_a.reshape(B, S, A_HEADS, HEAD_DIM), k_a.reshape(B, S, A_HEADS, HEAD_DIM),
                        v_a.reshape(B, S, A_HEADS, HEAD_DIM), q_i.reshape(B, S, IDX_HEADS, IDX_DIM),
                        k_i, w_i).reshape(B, S, A_WIDTH)

    qk_m = jax.nn.silu(causal_conv(jnp.concatenate([q_m, k_m], axis=-1), conv_w))
    q_m, k_m = jnp.split(qk_m, 2, axis=-1)
    h_m = mlstm_chunkwise(q_m.reshape(B, S, M_HEADS, M_DH),
                          k_m.reshape(B, S, M_HEADS, M_DH) * (M_DH ** -0.5),
                          v_m.reshape(B, S, M_HEADS, M_DH), i_m + b_i, f_m + b_f)
    y_m = jax.nn.sigmoid(o_m) * head_rmsnorm(h_m, m_norm_g.reshape(M_HEADS, M_DH)).reshape(B, S, M_WIDTH)

    lam_init = 0.8 - 0.6 * math.exp(-0.3 * layer_idx)
    lam = (jnp.exp(jnp.sum(lq1.astype(jnp.float32) * lk1)) - jnp.exp(jnp.sum(lq2.astype(jnp.float32) * lk2))
           + lam_init)
    h_c = diff_attention(q_c.reshape(B, S, C_HEADS, 2, C_QK), k_c.reshape(B, S, C_HEADS, 2, C_QK),
                         v_c.reshape(B, S, C_HEADS, C_DV), lam)
    y_c = (head_rmsnorm(h_c, c_norm_g.reshape(C_HEADS, C_DV)) * (1.0 - lam_init)).reshape(B, S, C_WIDTH)

    return jnp.concatenate([y_a, y_m, y_c], axis=-1)


def setup_inputs(seed: int = 0) -> dict:
    key = jax.random.key(seed)
    ks = jax.random.split(key, 20)
    f32 = jnp.float32
    col_scale = jnp.concatenate([jnp.full((s,), BETA if i in VALUE_SEGMENTS else 1.0, f32)
                                 for i, s in enumerate(SIZES)])
    x = jax.random.normal(ks[0], (BATCH, SEQ, D_MODEL), f32)
    w_in = jax.random.normal(ks[1], (DEPTH, D_MODEL, D_IN), f32) * (D_MODEL ** -0.5) * col_scale
    conv_m = jax.random.normal(ks[2], (DEPTH, CONV_W, 1, 2 * M_WIDTH), f32) * (CONV_W ** -0.5)
    b_i = 0.01 * jax.random.normal(ks[3], (DEPTH, M_HEADS), f32)
    b_f = jnp.linspace(3.0, 6.0, M_HEADS, dtype=f32)[None] + 0.01 * jax.random.normal(ks[4], (DEPTH, M_HEADS), f32)
    m_norm_g = 1.0 + 0.01 * jax.random.normal(ks[5], (DEPTH, M_WIDTH), f32)
    lam_q1 = 0.1 * jax.random.normal(ks[6], (DEPTH, C_QK), f32)
    lam_k1 = 0.1 * jax.random.normal(ks[7], (DEPTH, C_QK), f32)
    lam_q2 = 0.1 * jax.random.normal(ks[8], (DEPTH, C_QK), f32)
    lam_k2 = 0.1 * jax.random.normal(ks[9], (DEPTH, C_QK), f32)
    c_norm_g = 1.0 + 0.01 * jax.random.normal(ks[10], (DEPTH, C_WIDTH), f32)
    w_out = jax.random.normal(ks[11], (DEPTH, D_MIX, D_MODEL), f32) * (D_MIX ** -0.5) * BETA
    ln1_g = 1.0 + 0.01 * jax.random.normal(ks[12], (DEPTH, D_MODEL), f32)
    ln1_b = 0.01 * jax.random.normal(ks[13], (DEPTH, D_MODEL), f32)
    w_up = jax.random.normal(ks[14], (DEPTH, D_MODEL, D_FF), f32) * (D_MODEL ** -0.5) * BETA
    w_down = jax.random.normal(ks[15], (DEPTH, D_FF, D_MODEL), f32) * (D_FF ** -0.5) * BETA
    ln2_g = 1.0 + 0.01 * jax.random.normal(ks[16], (DEPTH, D_MODEL), f32)
    ln2_b = 0.01 * jax.random.normal(ks[17], (DEPTH, D_MODEL), f32)
    return {"x": x, "w_in": w_in, "conv_m": conv_m, "b_i": b_i, "b_f": b_f, "m_norm_g": m_norm_g,
            "lam_q1": lam_q1, "lam_k1": lam_k1, "lam_q2": lam_q2, "lam_k2": lam_k2,
            "c_norm_g": c_norm_g, "w_out": w_out, "ln1_g": ln1_g, "ln1_b": ln1_b,
            "w_up": w_up, "w_down": w_down, "ln2_g": ln2_g, "ln2_b": ln2_b}


def reference(x, w_in, conv_m, b_i, b_f, m_norm_g, lam_q1, lam_k1, lam_q2, lam_k2,
              c_norm_g, w_out, ln1_g, ln1_b, w_up, w_down, ln2_g, ln2_b):
    for l in range(DEPTH):
        mixed = token_mixers(x, l, w_in[l], conv_m[l], b_i[l], b_f[l], m_norm_g[l],
                             lam_q1[l], lam_k1[l], lam_q2[l], lam_k2[l], c_norm_g[l])
        x = layer_norm(ALPHA * x + jnp.einsum('bse,ed->bsd', mixed, w_out[l]), ln1_g[l], ln1_b[l])
        h = jnp.square(jax.nn.relu(jnp.einsum('bsd,df->bsf', x, w_up[l])))
        x = layer_norm(ALPHA * x + jnp.einsum('bsf,fd->bsd', h, w_down[l]), ln2_g[l], ln2_b[l])
    return x
```

```python
import math
import numpy as np
import ml_dtypes
import concourse.bass as bass
import concourse.mybir as mybir
from concourse.bass_utils import run_bass_kernel_spmd


ENGS = ("pe", "act", "dve", "pool", "sp")


class Sched:
    def __init__(self, nc, n_dma_sems=12):
        self.nc = nc
        self.eng = {"pe": nc.tensor, "act": nc.scalar, "dve": nc.vector,
                    "pool": nc.gpsimd, "sp": nc.sync}
        self.sem = {}
        self.cnt = {e: 0 for e in ENGS}
        self.stream = {e: [] for e in ENGS}
        self.waited = {e: {} for e in ENGS}
        self.last_w = {}
        self.readers = {}
        self.n_dma_sems = n_dma_sems
        self.dma_sems = {}
        self.dma_rr = {e: 0 for e in ENGS}
        self._ctx = []

    def open(self):
        nc = self.nc
        for e in ENGS:
            c = nc.semaphore("s_" + e)
            self.sem[e] = c.__enter__()
            self._ctx.append(c)
        for q in ("sp", "pool", "act"):
            for i in range(self.n_dma_sems):
                c = nc.semaphore("d_%s%d" % (q, i))
                self.dma_sems[(q, i)] = [c.__enter__(), 0]
                self._ctx.append(c)

    def _sem_of(self, key):
        return self.sem[key] if isinstance(key, str) else self.dma_sems[key][0]

    def _deps(self, eng, reads, writes):
        toks = []
        for b in reads:
            t = self.last_w.get(b)
            if t is not None:
                toks.append(t)
        for b in writes:
            t = self.last_w.get(b)
            if t is not None:
                toks.append(t)
            toks.extend(self.readers.get(b, ()))
        need = {}
        for key, val in toks:
            if key == eng and eng == "pe":
                continue
            if val > need.get(key, 0):
                need[key] = val
        out = []
        w = self.waited[eng]
        for key, val in need.items():
            if w.get(key, 0) >= val:
                continue
            w[key] = val
            out.append((key, val))
        return out

    def _commit(self, tok, reads, writes):
        for b in reads:
            self.readers.setdefault(b, []).append(tok)
        for b in writes:
            self.last_w[b] = tok
            self.readers[b] = []

    def op(self, eng, fname, *args, reads=(), writes=(), **kw):
        fn = (fname, args, kw)
        waits = self._deps(eng, reads, writes)
        self.cnt[eng] += 1
        tok = (eng, self.cnt[eng])
        self.stream[eng].append((waits, fn, (eng, 1)))
        self._commit(tok, reads, writes)
        return tok

    def dma(self, queue, *args, reads=(), writes=(), fname="dma_start", **kw):
        if fname != "dma_start":
            args, kw = kw["args"], kw["kw"]
        fn = (fname, args, kw)
        idx = self.dma_rr[queue]
        self.dma_rr[queue] = (idx + 1) % self.n_dma_sems
        key = (queue, idx)
        ent = self.dma_sems[key]
        waits = self._deps(queue, reads, writes)
        w = self.waited[queue]
        if ent[1] > 0 and w.get(key, 0) < ent[1]:
            waits.append((key, ent[1]))
            w[key] = ent[1]
        ent[1] += 16
        tok = (key, ent[1])
        self.stream[queue].append((waits, fn, (key, 16)))
        self._commit(tok, reads, writes)
        return tok

    def wait_all(self, eng, toks):
        waits = []
        w = self.waited[eng]
        for key, val in toks:
            if w.get(key, 0) < val:
                w[key] = val
                waits.append((key, val))
        self.stream[eng].append((waits, None, None))

    def emit(self):
        nc = self.nc
        with nc.Block() as block:
            def mk(e):
                def body(engine):
                    for waits, fn, inc in self.stream[e]:
                        for key, val in waits:
                            engine.wait_ge(self._sem_of(key), val)
                        if fn is not None:
                            ins = getattr(engine, fn[0])(*fn[1], **fn[2])
                            ins.then_inc(self._sem_of(inc[0]), inc[1])
                return body
            block.tensor(mk("pe"))
            block.scalar(mk("act"))
            block.vector(mk("dve"))
            block.gpsimd(mk("pool"))
            block.sync(mk("sp"))

    def close(self):
        for c in reversed(self._ctx):
            c.__exit__(None, None, None)
        self._ctx = []


F32 = mybir.dt.float32
BF16 = mybir.dt.bfloat16
AF = mybir.ActivationFunctionType
ALU = mybir.AluOpType
NT = 1024
D = 2048
DIN = 7508

GROUPS = [
    ("qaT", 0, 768, "F", BF16), ("kaT", 768, 768, "F", BF16), ("va", 1536, 768, "T", BF16),
    ("qiT", 2304, 512, "F", BF16), ("kiT", 2816, 64, "F", BF16), ("wi", 2880, 8, "T", F32),
    ("qmT", 2888, 768, "F", F32), ("kmT", 3656, 768, "F", F32), ("vm", 4424, 768, "T", BF16),
    ("om", 5192, 768, "T", F32), ("ifm", 5960, 12, "T", F32),
    ("qcT", 5972, 512, "F", BF16), ("kcT", 6484, 512, "F", BF16), ("vc", 6996, 512, "T", BF16),
]


def build_p1():
    nc = bass.Bass("TRN2", target_bir_lowering=False)
    x = nc.dram_tensor("x", [NT, D], F32, kind="ExternalInput").ap()
    w_in = nc.dram_tensor("w_in", [D, DIN], F32, kind="ExternalInput").ap()
    ident_d = nc.dram_tensor("ident", [128, 128], BF16, kind="ExternalInput").ap()
    outs = {}
    for (name, off, n, lay, dt) in GROUPS:
        shape = [n, NT] if lay == "F" else [NT, n]
        outs[name] = nc.dram_tensor(name, shape, dt, kind="ExternalOutput").ap()

    S = Sched(nc)
    S.open()
    ctxs = []

    def sb(name, shape, dt):
        c = nc.sbuf_tensor(name, shape, dt)
        t = c.__enter__()
        ctxs.append(c)
        return t

    def ps(name, shape, dt):
        c = nc.psum_tensor(name, shape, dt)
        t = c.__enter__()
        ctxs.append(c)
        return t

    ident = sb("ident_s", [128, 128], BF16)
    xT = sb("xT", [128, 16, NT], BF16)
    xf = [sb("xf%d" % i, [128, D], F32) for i in range(2)]
    xb = [sb("xb%d" % i, [128, D], BF16) for i in range(2)]
    wb = [sb("wb%d" % i, [128, 16, 512], BF16) for i in range(3)]
    stg = [sb("stg%d" % i, [128, 512], F32) for i in range(4)]
    pacc = [ps("pacc%d" % i, [128, 512], F32) for i in range(4)]
    ptr = [ps("ptr%d" % i, [128, 512], BF16) for i in range(2)]

    S.dma("sp", out=ident[:], in_=ident_d[:, :], writes=["ident"])
    for t in range(8):
        i = t % 2
        S.dma("sp", out=xf[i][:], in_=x[128 * t:128 * t + 128, :], writes=["xf%d" % i])
        S.op("dve" if t % 2 == 0 else "pool", "tensor_copy", out=xb[i][:], in_=xf[i][:],
             reads=["xf%d" % i], writes=["xb%d" % i])
        for c4 in range(4):
            pt = ptr[c4 % 2]
            for k in range(4):
                c = 4 * c4 + k
                S.op("pe", "transpose", out=pt[:, 128 * k:128 * k + 128], in_=xb[i][:, 128 * c:128 * c + 128],
                     identity=ident[:], reads=["xb%d" % i, "ident"], writes=["ptr%d" % (c4 % 2)])
            S.op("act", "copy", out=xT[:, 4 * c4:4 * c4 + 4, 128 * t:128 * t + 128],
                 in_=pt[:, 0:512].rearrange("p (k t) -> p k t", k=4),
                 reads=["ptr%d" % (c4 % 2)], writes=["xT"])

    wcnt = [0]
    ecnt = [0]

    def evac(pa, pname, rows, cols, dt):
        k = ecnt[0] % 4
        ecnt[0] += 1
        sv = stg[k][:] if dt == F32 else stg[k][:].bitcast(BF16)
        dst = sv[0:rows, 0:cols]
        if k % 2 == 0:
            S.op("act", "copy", out=dst, in_=pa[0:rows, 0:cols], reads=[pname], writes=["stg%d" % k])
        else:
            S.op("dve", "tensor_copy", out=dst, in_=pa[0:rows, 0:cols], reads=[pname], writes=["stg%d" % k])
        return dst, "stg%d" % k

    for (name, off, n, lay, dt) in GROUPS:
        for c0 in range(0, n, 512):
            ncol = min(512, n - c0)
            wi = wcnt[0] % 3
            wcnt[0] += 1
            S.dma("pool", out=wb[wi][:, :, 0:ncol],
                  in_=w_in[:, off + c0:off + c0 + ncol].rearrange("(c p) e -> p c e", p=128),
                  writes=["wb%d" % wi])
            if lay == "F":
                for e0 in range(0, ncol, 128):
                    ne = min(128, ncol - e0)
                    for tg in range(2):
                        pi = ecnt[0] % 4
                        pa = pacc[pi]
                        for c in range(16):
                            S.op("pe", "matmul", pa[0:ne, :], lhsT=wb[wi][:, c, e0:e0 + ne],
                                 rhs=xT[:, c, 512 * tg:512 * tg + 512], start=(c == 0), stop=(c == 15),
                                 reads=["wb%d" % wi, "xT"], writes=["pacc%d" % pi])
                        dst, sname = evac(pa, "pacc%d" % pi, ne, 512, dt)
                        S.dma("sp", out=outs[name][c0 + e0:c0 + e0 + ne, 512 * tg:512 * tg + 512], in_=dst,
                              reads=[sname], writes=["out_" + name])
            else:
                for t in range(8):
                    pi = ecnt[0] % 4
                    pa = pacc[pi]
                    for c in range(16):
                        S.op("pe", "matmul", pa[:, 0:ncol], lhsT=xT[:, c, 128 * t:128 * t + 128],
                             rhs=wb[wi][:, c, 0:ncol], start=(c == 0), stop=(c == 15),
                             reads=["wb%d" % wi, "xT"], writes=["pacc%d" % pi])
                    dst, sname = evac(pa, "pacc%d" % pi, 128, ncol, dt)
                    S.dma("sp", out=outs[name][128 * t:128 * t + 128, c0:c0 + ncol], in_=dst,
                          reads=[sname], writes=["out_" + name])

    alltoks = [(k, v[1]) for k, v in S.dma_sems.items() if v[1] > 0]
    S.wait_all("sp", alltoks)
    S.emit()
    S.close()
    for c in reversed(ctxs):
        c.__exit__(None, None, None)
    return nc


FLAGS = ''

F32 = mybir.dt.float32
BF16 = mybir.dt.bfloat16
AF = mybir.ActivationFunctionType
ALU = mybir.AluOpType
NT = 1024
SEQ = 8192
H = 4
EPS = 1e-5


def core_blocks(c):
    out = []
    for g in range(4):
        out += [16 * g + c, 16 * g + 15 - c]
    return out


def c_tables(c, slopes, nheads):
    blocks = core_blocks(c)
    bias = np.full((128, nheads, 4, 64, 2), -30000.0, np.float32)
    ar = np.arange(128, dtype=np.float32)
    for g in range(4):
        for tile in range(2):
            qb = blocks[2 * g + tile]
            qref = 128 * qb + 127
            for kt in range(qb + 1):
                for h in range(nheads):
                    bias[:, h, g, kt, tile] = slopes[h] * (128 * kt + ar - qref)
    mask = np.zeros((128, 16, 2, 128), np.float32)
    tri = (ar[:, None] <= ar[None, :]).astype(np.float32)
    for tile in range(2):
        qslot = c if tile == 0 else 15 - c
        for i in range(16):
            if i < qslot:
                mask[:, i, tile, :] = 1.0
            elif i == qslot:
                mask[:, i, tile, :] = tri
    return bias.reshape(128, -1), mask.reshape(128, -1)


def build_pc(HR=4, GR=4):
    nc = bass.Bass("TRN2", target_bir_lowering=False)
    qT = nc.dram_tensor("qT", [512, NT], BF16, kind="ExternalInput").ap()
    kT = nc.dram_tensor("kT", [512, SEQ], BF16, kind="ExternalInput").ap()
    v = nc.dram_tensor("v", [SEQ, 512], BF16, kind="ExternalInput").ap()
    lam_d = nc.dram_tensor("lam", [4, 64], F32, kind="ExternalInput").ap()
    gc_d = nc.dram_tensor("gc", [512], F32, kind="ExternalInput").ap()
    cst_d = nc.dram_tensor("cst", [2], F32, kind="ExternalInput").ap()
    bias_d = nc.dram_tensor("bias", [128, H * 4 * 64 * 2], F32, kind="ExternalInput").ap()
    mask_d = nc.dram_tensor("mask", [128, 16 * 2 * 128], BF16, kind="ExternalInput").ap()
    y = nc.dram_tensor("y", [NT, 512], BF16, kind="ExternalOutput").ap()

    S = Sched(nc)
    S.open()
    ctxs = []

    def sb(name, shape, dt):
        c = nc.sbuf_tensor(name, shape, dt)
        t = c.__enter__()
        ctxs.append(c)
        return t

    def ps(name, shape, dt):
        c = nc.psum_tensor(name, shape, dt)
        t = c.__enter__()
        ctxs.append(c)
        return t

    qs = sb("qs", [128, H, 2, NT], BF16)
    ks = [sb("ks%d" % i, [128, SEQ], BF16) for i in range(2)]
    vs = [sb("vs%d" % i, [128, 64, 132], BF16) for i in range(2)]
    bias = sb("bias_s", [128, H * 4 * 64 * 2], F32)
    mask = sb("mask_s", [128, 16, 256], BF16)
    lamb = sb("lamb", [128, 4, 64], F32)
    gcb = sb("gcb", [128, 512], F32)
    cst = sb("cst_s", [128, 2], F32)
    sm = sb("sm", [128, 16], F32)
    epsb = sb("epsb", [128, 1], F32)
    pT = [sb("pT%d" % i, [128, 256], BF16) for i in range(4)]
    ys = sb("ys", [128, 8, 512], BF16)
    t1 = [sb("t1_%d" % i, [128, 128], F32) for i in range(2)]
    t2 = [sb("t2_%d" % i, [128, 128], F32) for i in range(2)]
    junk = sb("junk", [128, 128], F32)
    fs = [sb("fs%d" % i, [128, 8], F32) for i in range(2)]
    pS = [ps("pS%d" % i, [128, 512], F32) for i in range(2)]
    pO = [ps("pO%d" % i, [128, 2, 132], F32) for i in range(4)]

    S.op("pool", "memset", qs[:], 0.0, writes=["qs"])
    for m in range(2):
        S.dma("sp", out=qs[64 * m:64 * m + 64, :, m, :], in_=qT.rearrange("(h p) t -> p h t", p=128)[64 * m:64 * m + 64],
              writes=["qs"])
    S.dma("sp", out=bias[:], in_=bias_d[:, :], writes=["bias"])
    S.dma("sp", out=mask[:].rearrange("p a b -> p (a b)"), in_=mask_d[:, :], writes=["mask"])
    S.dma("sp", out=lamb[:].rearrange("p a b -> p (a b)"), in_=lam_d.rearrange("a b -> (a b)").partition_broadcast(128),
          writes=["lamb"])
    S.dma("sp", out=gcb[:], in_=gc_d.partition_broadcast(128), writes=["gcb"])
    S.dma("sp", out=cst[:], in_=cst_d.partition_broadcast(128), writes=["cst"])
    S.op("pool", "memset", epsb[:], EPS, writes=["epsb"])
    S.op("pool", "memset", ys[:], 0.0, writes=["ys"])
    for i in range(2):
        S.op("pool", "memset", vs[i][:, :, 128:129], 1.0, writes=["vone%d" % i])
    S.op("dve", "tensor_tensor", out=lamb[:, 0, :], in0=lamb[:, 0, :], in1=lamb[:, 1, :], op=ALU.mult,
         reads=["lamb"], writes=["lamb"])
    S.op("dve", "tensor_tensor", out=lamb[:, 2, :], in0=lamb[:, 2, :], in1=lamb[:, 3, :], op=ALU.mult,
         reads=["lamb"], writes=["lamb"])
    S.op("dve", "reduce_sum", out=sm[:, 1:2], in_=lamb[:, 0, :], axis=mybir.AxisListType.X, reads=["lamb"], writes=["sm1"])
    S.op("dve", "reduce_sum", out=sm[:, 2:3], in_=lamb[:, 2, :], axis=mybir.AxisListType.X, reads=["lamb"], writes=["sm2"])
    S.op("act", "activation", out=sm[:, 1:3], in_=sm[:, 1:3], func=AF.Exp, reads=["sm1", "sm2"], writes=["sm12"])
    S.op("dve", "tensor_tensor", out=sm[:, 0:1], in0=sm[:, 2:3], in1=sm[:, 1:2], op=ALU.subtract,
         reads=["sm12"], writes=["sm0"])
    S.op("dve", "tensor_tensor", out=sm[:, 0:1], in0=sm[:, 0:1], in1=cst[:, 0:1], op=ALU.subtract,
         reads=["sm0", "cst"], writes=["sm0"])
    S.op("dve", "tensor_scalar", out=gcb[:], in0=gcb[:], scalar1=cst[:, 1:2], scalar2=None, op0=ALU.mult,
         reads=["gcb", "cst"], writes=["gcb"])

    ucnt = 0
    fcnt = 0
    for h in range(HR):
        hi = h % 2
        S.dma("sp", out=ks[hi][:], in_=kT[128 * h:128 * h + 128, :], writes=["ks%d" % hi])
        for q4 in range(4):
            S.dma("pool", out=vs[hi][:, 16 * q4:16 * q4 + 16, 0:128],
                  in_=v[2048 * q4:2048 * q4 + 2048, 128 * h:128 * h + 128].rearrange("(kt p) e -> p kt e", p=128),
                  writes=["vs%d_%d" % (hi, q4)])
        units = [(g, kt) for g in range(GR) for kt in range(16 * g + 16)]

        def emit_qk(u, sbuf_i):
            g, kt = u
            for m in range(2):
                S.op("pe", "matmul", pS[sbuf_i][:, 256 * m:256 * m + 256],
                     lhsT=ks[hi][:, 128 * kt:128 * kt + 128],
                     rhs=qs[:, h, m, 256 * g:256 * g + 256], start=True, stop=True,
                     reads=["ks%d" % hi, "qs"], writes=["pS%d" % sbuf_i])

        def emit_rest(u, sbuf_i):
            nonlocal fcnt
            g, kt = u
            nkt = 16 * g + 16
            ob = (h * 4 + g) % 2
            pSb = pS[sbuf_i]
            for m in range(2):
                pt = pT[2 * sbuf_i + m]
                ptn = "pT%d" % (2 * sbuf_i + m)
                for tile in range(2):
                    bcol = ((h * 4 + g) * 64 + kt) * 2 + tile
                    S.op("act", "activation", out=pt[:, 128 * tile:128 * tile + 128],
                         in_=pSb[:, 256 * m + 128 * tile:256 * m + 128 * tile + 128], func=AF.Exp,
                         bias=bias[:, bcol:bcol + 1], scale=0.125,
                         reads=["pS%d" % sbuf_i, "bias"], writes=[ptn])
                if kt >= 16 * g:
                    S.op("dve" if m == 0 else "pool", "tensor_tensor", out=pt[:], in0=pt[:],
                         in1=mask[:, kt - 16 * g, :], op=ALU.mult, reads=[ptn, "mask"], writes=[ptn])
                for tile in range(2):
                    S.op("pe", "matmul", pO[2 * ob + m][:, tile, 0:129], lhsT=pt[:, 128 * tile:128 * tile + 128],
                         rhs=vs[hi][:, kt, 0:129], start=(kt == 0 and tile == 0), stop=(kt == nkt - 1 and tile == 1),
                         reads=[ptn, "vs%d_%d" % (hi, kt // 16), "vone%d" % hi], writes=["pO%d" % (2 * ob + m)])
            if kt != nkt - 1:
                return
            for tile in range(2):
                fi = fcnt % 2
                fcnt += 1
                f = fs[fi]
                fn = "fs%d" % fi
                a1 = pO[2 * ob + 0]
                a2 = pO[2 * ob + 1]
                S.op("dve", "reciprocal", out=f[:, 0:1], in_=a1[:, tile, 128:129], reads=["pO%d" % (2 * ob)], writes=[fn])
                S.op("dve", "reciprocal", out=f[:, 1:2], in_=a2[:, tile, 128:129], reads=["pO%d" % (2 * ob + 1)], writes=[fn])
                S.op("dve", "tensor_tensor", out=f[:, 1:2], in0=f[:, 1:2], in1=sm[:, 0:1], op=ALU.mult,
                     reads=[fn, "sm0"], writes=[fn])
                S.op("dve", "tensor_scalar", out=t1[fi][:], in0=a1[:, tile, 0:128], scalar1=f[:, 0:1], scalar2=None,
                     op0=ALU.mult, reads=["pO%d" % (2 * ob), fn], writes=["t1_%d" % fi])
                S.op("dve", "scalar_tensor_tensor", out=t2[fi][:], in0=a2[:, tile, 0:128], scalar=f[:, 1:2],
                     in1=t1[fi][:], op0=ALU.mult, op1=ALU.add,
                     reads=["pO%d" % (2 * ob + 1), fn, "t1_%d" % fi], writes=["t2_%d" % fi])
                S.op("act", "activation", out=junk[:], in_=t2[fi][:], func=AF.Square, accum_out=f[:, 2:3],
                     reads=["t2_%d" % fi], writes=["junk", fn])
                S.op("act", "activation", out=f[:, 3:4], in_=f[:, 2:3], func=AF.Sqrt, bias=epsb[:], scale=1.0 / 128,
                     reads=[fn, "epsb"], writes=[fn])
                S.op("dve", "reciprocal", out=f[:, 3:4], in_=f[:, 3:4], reads=[fn], writes=[fn])
                S.op("dve", "scalar_tensor_tensor", out=ys[:, 2 * g + tile, 128 * h:128 * h + 128], in0=t2[fi][:],
                     scalar=f[:, 3:4], in1=gcb[:, 128 * h:128 * h + 128], op0=ALU.mult, op1=ALU.mult,
                     reads=["t2_%d" % fi, fn, "gcb"], writes=["ys"])

        for idx in range(len(units) + 1):
            if idx < len(units):
                emit_qk(units[idx], (ucnt + idx) % 2)
            if idx >= 1:
                emit_rest(units[idx - 1], (ucnt + idx - 1) % 2)
        ucnt += len(units)
    S.dma("sp", out=y.rearrange("(j p) e -> p j e", p=128), in_=ys[:], reads=["ys"], writes=["yout"])
    alltoks = [(k, v_[1]) for k, v_ in S.dma_sems.items() if v_[1] > 0]
    S.wait_all("sp", alltoks)
    S.emit()
    S.close()
    for c in reversed(ctxs):
        c.__exit__(None, None, None)
    return nc


F32 = mybir.dt.float32
BF16 = mybir.dt.bfloat16
AF = mybir.ActivationFunctionType
ALU = mybir.AluOpType
AX = mybir.AxisListType
SEQ = 8192
NCH = 64
EPS = 1e-5
LNS = math.log(128 ** -0.5)


def build_pb(NJ=NCH):
    nc = bass.Bass("TRN2", target_bir_lowering=False)
    qT_d = nc.dram_tensor("qT", [128, SEQ], F32, kind="ExternalInput").ap()
    kT_d = nc.dram_tensor("kT", [128, SEQ], F32, kind="ExternalInput").ap()
    v_d = nc.dram_tensor("v", [SEQ, 128], BF16, kind="ExternalInput").ap()
    o_d = nc.dram_tensor("o", [SEQ, 128], F32, kind="ExternalInput").ap()
    if_d = nc.dram_tensor("ifg", [SEQ, 2], F32, kind="ExternalInput").ap()
    cw_d = nc.dram_tensor("cw", [128, 8], F32, kind="ExternalInput").ap()
    bif_d = nc.dram_tensor("bif", [2], F32, kind="ExternalInput").ap()
    gn_d = nc.dram_tensor("gn", [128], F32, kind="ExternalInput").ap()
    tri_d = nc.dram_tensor("tri", [128, 128], F32, kind="ExternalInput").ap()
    ident_d = nc.dram_tensor("ident", [128, 128], BF16, kind="ExternalInput").ap()
    y_d = nc.dram_tensor("y", [SEQ, 128], BF16, kind="ExternalOutput").ap()

    S = Sched(nc)
    S.open()
    ctxs = []

    def sb(name, shape, dt):
        c = nc.sbuf_tensor(name, shape, dt)
        t = c.__enter__()
        ctxs.append(c)
        return t

    def ps(name, shape, dt):
        c = nc.psum_tensor(name, shape, dt)
        t = c.__enter__()
        ctxs.append(c)
        return t

    ident = sb("ident_s", [128, 128], BF16)
    tri = sb("tri_s", [128, 128], F32)
    trib = sb("trib", [128, 128], BF16)
    ones = sb("ones", [128, 128], F32)
    cw = sb("cw_s", [128, 8], F32)
    bif = sb("bif_s", [128, 2], F32)
    gnb = sb("gnb", [128, 128], F32)
    epsb = sb("epsb", [128, 1], F32)
    gi = sb("gi", [128, NCH, 2], F32)
    lf = sb("lf", [128, NCH], F32)
    li = sb("li", [128, NCH], F32)
    a_s = sb("a_s", [128, NCH], F32)
    g_s = sb("g_s", [128, NCH], F32)
    u_s = sb("u_s", [128, NCH], F32)
    wi_s = sb("wi_s", [128, NCH], F32)
    ws_s = sb("ws_s", [128, NCH], F32)
    eg = sb("eg", [128, NCH], F32)
    ea = sb("ea", [128, NCH], F32)
    xr = [sb("xr%d" % i, [128, 2048 + 4], F32) for i in range(2)]
    ac = [sb("ac%d" % i, [128, 2048], F32) for i in range(2)]
    QT = sb("QT", [128, SEQ], BF16)
    KT = sb("KT", [128, SEQ], BF16)
    Kt = sb("Kt", [128, NCH, 128], BF16)
    vs = sb("vs", [128, NCH, 132], BF16)
    vi = sb("vi", [128, NCH, 132], BF16)
    vst = sb("vst", [128, NCH, 132], BF16)
    og = sb("og", [128, NCH, 128], BF16)
    of = [sb("of%d" % i, [128, 128], F32) for i in range(2)]
    ys = sb("ys", [128, NCH, 128], BF16)
    C = sb("C", [128, 132], F32)
    Cb = [sb("Cb%d" % i, [128, 132], BF16) for i in range(2)]
    qk = [sb("qk%d" % i, [128, 128], BF16) for i in range(2)]
    hh = [sb("hh%d" % i, [128, 128], F32) for i in range(2)]
    junk = sb("junk", [128, 128], F32)
    fs = [sb("fs%d" % i, [128, 4], F32) for i in range(2)]
    pS = [ps("pS%d" % i, [128, 128], F32) for i in range(2)]
    pX = [ps("pX%d" % i, [128, 132], F32) for i in range(2)]
    pC = [ps("pC%d" % i, [128, 132], F32) for i in range(2)]
    pT = ps("pT", [128, 512], BF16)
    pG = ps("pG", [128, 2, NCH], F32)

    S.dma("sp", out=ident[:], in_=ident_d[:, :], writes=["ident"])
    S.dma("sp", out=tri[:], in_=tri_d[:, :], writes=["tri"])
    S.dma("sp", out=cw[:], in_=cw_d[:, :], writes=["cw"])
    S.dma("sp", out=bif[:], in_=bif_d.partition_broadcast(128), writes=["bif"])
    S.dma("sp", out=gnb[:], in_=gn_d.partition_broadcast(128), writes=["gnb"])
    S.dma("sp", out=gi[:], in_=if_d.rearrange("(j p) c -> p j c", p=128), writes=["gi"])
    S.dma("sp", out=vs[:, :, 0:128], in_=v_d.rearrange("(j p) e -> p j e", p=128), writes=["vs"])
    S.op("pool", "memset", epsb[:], EPS, writes=["epsb"])
    S.op("pool", "memset", ones[:], 1.0, writes=["ones"])
    S.op("pool", "memset", vs[:, :, 128:129], 1.0, reads=[], writes=["vs1"])
    S.op("pool", "memset", C[:], 0.0, writes=["C"])
    S.op("pool", "tensor_copy", out=trib[:], in_=tri[:], reads=["tri"], writes=["trib"])
    S.op("dve", "tensor_scalar", out=li[:], in0=gi[:, :, 0], scalar1=bif[:, 0:1], scalar2=None, op0=ALU.add,
         reads=["gi", "bif"], writes=["li"])
    S.op("dve", "tensor_scalar", out=lf[:], in0=gi[:, :, 1], scalar1=bif[:, 1:2], scalar2=None, op0=ALU.add,
         reads=["gi", "bif"], writes=["lf"])
    S.op("act", "activation", out=lf[:], in_=lf[:], func=AF.Exp, scale=-1.0, reads=["lf"], writes=["lf"])
    S.op("act", "activation", out=lf[:], in_=lf[:], func=AF.Ln, bias=1.0, scale=1.0, reads=["lf"], writes=["lf"])
    S.op("dve", "tensor_scalar", out=lf[:], in0=lf[:], scalar1=-1.0, scalar2=None, op0=ALU.mult,
         reads=["lf"], writes=["lf"])
    S.op("pe", "matmul", pG[:, 0, :], lhsT=tri[:], rhs=lf[:], start=True, stop=False, reads=["tri", "lf"], writes=["pG"])
    S.op("pe", "matmul", pG[:, 1, :], lhsT=ones[:], rhs=lf[:], start=False, stop=True, reads=["ones", "lf"], writes=["pG"])
    S.op("dve", "tensor_copy", out=a_s[:], in_=pG[:, 0, :], reads=["pG"], writes=["a_s"])
    S.op("dve", "tensor_copy", out=g_s[:], in_=pG[:, 1, :], reads=["pG"], writes=["g_s"])
    S.op("dve", "tensor_tensor", out=u_s[:], in0=li[:], in1=a_s[:], op=ALU.subtract, reads=["li", "a_s"], writes=["u_s"])
    S.op("act", "activation", out=wi_s[:], in_=u_s[:], func=AF.Exp, bias=LNS, scale=1.0, reads=["u_s"], writes=["wi_s"])
    S.op("dve", "tensor_tensor", out=u_s[:], in0=u_s[:], in1=g_s[:], op=ALU.add, reads=["u_s", "g_s", "wi_s"], writes=["u_s"])
    S.op("act", "activation", out=ws_s[:], in_=u_s[:], func=AF.Exp, bias=LNS, scale=1.0, reads=["u_s"], writes=["ws_s"])
    S.op("act", "activation", out=eg[:], in_=g_s[:], func=AF.Exp, reads=["g_s"], writes=["eg"])
    S.op("act", "activation", out=ea[:], in_=a_s[:], func=AF.Exp, scale=-1.0, reads=["a_s"], writes=["ea"])
    S.op("dve", "tensor_tensor", out=vi[:, :, 0:129], in0=vs[:, :, 0:129],
         in1=wi_s[:].unsqueeze(2).to_broadcast([128, NCH, 129]), op=ALU.mult,
         reads=["vs", "vs1", "wi_s"], writes=["vi"])
    S.op("pool", "tensor_tensor", out=vst[:, :, 0:129], in0=vs[:, :, 0:129],
         in1=ws_s[:].unsqueeze(2).to_broadcast([128, NCH, 129]), op=ALU.mult,
         reads=["vs", "vs1", "ws_s"], writes=["vst"])
    for which, (src, dst, dname) in enumerate(((qT_d, QT, "QT"), (kT_d, KT, "KT"))):
        for p in range(4):
            i = (which * 4 + p) % 2
            if p == 0:
                S.op("pool", "memset", xr[i][:, 0:4], 0.0, writes=["xr%d" % i])
                S.dma("sp", out=xr[i][:, 4:2052], in_=src[:, 0:2048], writes=["xr%d" % i])
            else:
                S.dma("sp", out=xr[i][:, 1:2052], in_=src[:, 2048 * p - 3:2048 * p + 2048], writes=["xr%d" % i])
            S.op("dve", "tensor_scalar", out=ac[i][:], in0=xr[i][:, 1:2049], scalar1=cw[:, 4 * which:4 * which + 1],
                 scalar2=None, op0=ALU.mult, reads=["xr%d" % i, "cw"], writes=["ac%d" % i])
            for w in range(1, 4):
                S.op("dve", "scalar_tensor_tensor", out=ac[i][:], in0=xr[i][:, 1 + w:2049 + w],
                     scalar=cw[:, 4 * which + w:4 * which + w + 1], in1=ac[i][:], op0=ALU.mult, op1=ALU.add,
                     reads=["xr%d" % i, "cw", "ac%d" % i], writes=["ac%d" % i])
            S.op("act", "activation", out=dst[:, 2048 * p:2048 * p + 2048], in_=ac[i][:], func=AF.Silu,
                 reads=["ac%d" % i], writes=[dname + "%d" % p])
    for j in range(NJ):
        i = j % 2
        S.dma("sp", out=of[i][:], in_=o_d[128 * j:128 * j + 128, :], writes=["of%d" % i])
        S.op("act", "activation", out=of[i][:], in_=of[i][:], func=AF.Sigmoid, reads=["of%d" % i], writes=["of%d" % i])
        S.op("pool", "tensor_tensor", out=og[:, j, :], in0=of[i][:], in1=gnb[:], op=ALU.mult,
             reads=["of%d" % i, "gnb"], writes=["og%d" % j])
    for j4 in range(NJ // 4):
        for k in range(4):
            j = 4 * j4 + k
            S.op("pe", "transpose", out=pT[:, 128 * k:128 * k + 128], in_=KT[:, 128 * j:128 * j + 128], identity=ident[:],
                 reads=["KT%d" % (j // 16), "ident"], writes=["pT"])
        S.op("dve", "tensor_copy", out=Kt[:, 4 * j4:4 * j4 + 4, :], in_=pT[:, 0:512].rearrange("p (k t) -> p k t", k=4),
             reads=["pT"], writes=["Kt%d" % j4])

    for j in range(NJ):
        i = j % 2
        qn = "QT%d" % (j // 16)
        kn = "KT%d" % (j // 16)
        S.op("pe", "matmul", pS[i][:], lhsT=KT[:, 128 * j:128 * j + 128], rhs=QT[:, 128 * j:128 * j + 128],
             start=True, stop=True, reads=[qn, kn], writes=["pS%d" % i])
        S.op("dve", "tensor_tensor", out=qk[i][:], in0=pS[i][:], in1=trib[:], op=ALU.mult,
             reads=["pS%d" % i, "trib"], writes=["qk%d" % i])
        S.op("pe", "matmul", pX[i][:, 0:129], lhsT=qk[i][:], rhs=vi[:, j, 0:129], start=True, stop=(j == 0),
             reads=["qk%d" % i, "vi"], writes=["pX%d" % i])
        if j > 0:
            S.op("pe", "matmul", pX[i][:, 0:129], lhsT=QT[:, 128 * j:128 * j + 128], rhs=Cb[i][:, 0:129],
                 start=False, stop=True, reads=[qn, "Cb%d" % i], writes=["pX%d" % i])
        if j < NJ - 1:
            S.op("pe", "matmul", pC[i][:, 0:129], lhsT=Kt[:, j, :], rhs=vst[:, j, 0:129], start=True, stop=True,
                 reads=["Kt%d" % (j // 4), "vst"], writes=["pC%d" % i])
            S.op("dve", "scalar_tensor_tensor", out=C[:, 0:129], in0=C[:, 0:129], scalar=eg[:, j:j + 1],
                 in1=pC[i][:, 0:129], op0=ALU.mult, op1=ALU.add, reads=["C", "eg", "pC%d" % i], writes=["C"])
            S.op("pool", "tensor_copy", out=Cb[1 - i][:, 0:129], in_=C[:, 0:129], reads=["C"], writes=["Cb%d" % (1 - i)])
        f = fs[i]
        fn = "fs%d" % i
        S.op("dve", "tensor_scalar", out=f[:, 3:4], in0=pX[i][:, 128:129], scalar1=-1.0, scalar2=ea[:, j:j + 1],
             op0=ALU.mult, op1=ALU.max, reads=["pX%d" % i, "ea"], writes=[fn])
        S.op("dve", "tensor_tensor", out=f[:, 0:1], in0=pX[i][:, 128:129], in1=f[:, 3:4], op=ALU.max,
             reads=["pX%d" % i, fn], writes=[fn])
        S.op("dve", "reciprocal", out=f[:, 0:1], in_=f[:, 0:1], reads=[fn], writes=[fn])
        S.op("dve", "tensor_scalar", out=hh[i][:], in0=pX[i][:, 0:128], scalar1=f[:, 0:1], scalar2=None, op0=ALU.mult,
             reads=["pX%d" % i, fn], writes=["hh%d" % i])
        S.op("dve", "scalar_tensor_tensor", out=junk[:], in0=hh[i][:], scalar=1.0, in1=hh[i][:], op0=ALU.mult,
             op1=ALU.mult, accum_out=f[:, 1:2], reads=["hh%d" % i], writes=["junk", fn])
        S.op("act", "activation", out=f[:, 2:3], in_=f[:, 1:2], func=AF.Sqrt, bias=epsb[:], scale=1.0 / 128,
             reads=[fn, "epsb"], writes=[fn])
        S.op("dve", "reciprocal", out=f[:, 2:3], in_=f[:, 2:3], reads=[fn], writes=[fn])
        S.op("dve", "scalar_tensor_tensor", out=ys[:, j, :], in0=hh[i][:], scalar=f[:, 2:3], in1=og[:, j, :],
             op0=ALU.mult, op1=ALU.mult, reads=["hh%d" % i, fn, "og%d" % j], writes=["ys"])
    if NJ < NCH:
        S.op("pool", "memset", ys[:, NJ:NCH, :], 0.0, reads=[], writes=["ys"])
    S.dma("sp", out=y_d.rearrange("(j p) e -> p j e", p=128), in_=ys[:], reads=["ys"], writes=["yout"])
    alltoks = [(k, v_[1]) for k, v_ in S.dma_sems.items() if v_[1] > 0]
    S.wait_all("sp", alltoks)
    S.emit()
    S.close()
    for c in reversed(ctxs):
        c.__exit__(None, None, None)
    return nc


F32 = mybir.dt.float32
BF16 = mybir.dt.bfloat16
AF = mybir.ActivationFunctionType
ALU = mybir.AluOpType
AX = mybir.AxisListType
NT = 1024
SEQ = 8192
HA = 6
HI = 8
KSEL = 256
NIT = 18
SCALE = 128 ** -0.5
NEG = -1.0e30


def core_blocks(c):
    out = []
    for g in range(4):
        out += [16 * g + c, 16 * g + 15 - c]
    return out


def a_tables(c):
    slopes = 2.0 ** (-8.0 * np.arange(1, HA + 1) / HA)
    ar = np.arange(128, dtype=np.float32)
    bias = np.zeros((128, HA, 4, 64), np.float32)
    rbc = np.zeros((128, 4), np.float32)
    for g in range(4):
        rb = 16 * g + 15 - c
        R = 128 * rb + 127
        rbc[:, g] = 128.0 * (rb + 1)
        for h in range(HA):
            for kt in range(64):
                bias[:, h, g, kt] = slopes[h] * (128 * kt + ar - R)
    negb = np.full((128, 16, 2, 128), NEG, np.float32)
    tri = np.where(ar[None, :] <= ar[:, None], 0.0, NEG)
    for tile in range(2):
        qslot = c if tile == 0 else 15 - c
        for i in range(16):
            if i < qslot:
                negb[:, i, tile, :] = 0.0
            elif i == qslot:
                negb[:, i, tile, :] = tri
    sl = np.zeros((1, HA, 128), np.float32)
    for h in range(HA):
        sl[0, h, :] = slopes[h] / SCALE
    ktp1 = np.tile(np.arange(1, 65, dtype=np.float32)[None, :], (128, 1))
    return dict(bias=bias.reshape(128, -1), rbc=rbc, negb=negb.reshape(128, -1), sl=sl.reshape(1, -1), ktp1=ktp1)


def build_pa(GR=4, HR=HA):
    nc = bass.Bass("TRN2", target_bir_lowering=False)
    qT_d = nc.dram_tensor("qT", [768, NT], BF16, kind="ExternalInput").ap()
    kT_d = nc.dram_tensor("kT", [768, SEQ], BF16, kind="ExternalInput").ap()
    v_d = nc.dram_tensor("v", [SEQ, 768], BF16, kind="ExternalInput").ap()
    qiT_d = nc.dram_tensor("qiT", [512, NT], BF16, kind="ExternalInput").ap()
    kiT_d = nc.dram_tensor("kiT", [64, SEQ], BF16, kind="ExternalInput").ap()
    wi_d = nc.dram_tensor("wi", [NT, 8], F32, kind="ExternalInput").ap()
    bias_d = nc.dram_tensor("bias", [128, HA * 4 * 64], F32, kind="ExternalInput").ap()
    rbc_d = nc.dram_tensor("rbc", [128, 4], F32, kind="ExternalInput").ap()
    negb_d = nc.dram_tensor("negb", [128, 16 * 2 * 128], BF16, kind="ExternalInput").ap()
    sl_d = nc.dram_tensor("sl", [1, HA * 128], BF16, kind="ExternalInput").ap()
    ktp1_d = nc.dram_tensor("ktp1", [128, 64], F32, kind="ExternalInput").ap()
    ident_d = nc.dram_tensor("ident", [128, 128], BF16, kind="ExternalInput").ap()
    identf_d = nc.dram_tensor("identf", [128, 128], F32, kind="ExternalInput").ap()
    y_d = nc.dram_tensor("y", [NT, 768], BF16, kind="ExternalOutput").ap()

    S = Sched(nc)
    S.open()
    ctxs = []

    def sb(name, shape, dt):
        c = nc.sbuf_tensor(name, shape, dt)
        t = c.__enter__()
        ctxs.append(c)
        return t

    def ps(name, shape, dt):
        c = nc.psum_tensor(name, shape, dt)
        t = c.__enter__()
        ctxs.append(c)
        return t

    ident = sb("ident_s", [128, 128], BF16)
    identf = sb("identf_s", [128, 128], F32)
    qs = sb("qs", [128, HA, NT], BF16)
    ks = sb("ks", [128, SEQ], BF16)
    vs = sb("vs", [128, 64, 132], BF16)
    kis = sb("kis", [64, SEQ], BF16)
    qis = sb("qis", [64, HI, NT], BF16)
    wi = sb("wi_s", [128, 8, 8], F32)
    wa = sb("wa", [128, 8, 8], F32)
    wsg = sb("wsg", [128, 8, 8], F32)
    bias = sb("bias_s", [128, HA * 4 * 64], F32)
    rbc = sb("rbc_s", [128, 4], F32)
    negb = sb("negb_s", [128, 16, 2, 128], BF16)
    sl = sb("sl_s", [1, HA, 128], BF16)
    ktp1 = sb("ktp1_s", [128, 64], F32)
    I = sb("I", [128, SEQ], F32)
    nsq = sb("nsq", [128, SEQ], BF16)
    nsT = sb("nsT", [128, 64, 256], BF16)
    rl = [sb("rl%d" % i, [128, 512], F32) for i in range(2)]
    bs = sb("bs", [128, 8 + NIT + 4], F32)
    an = sb("an", [128, 64], F32)
    dcol = sb("dcol", [128, 2], F32)
    drow = sb("drow", [1, 256], BF16)
    pT = [sb("pT%d" % i, [128, 256], BF16) for i in range(3)]
    ys = sb("ys", [128, 8, 768], BF16)
    fs = [sb("fs%d" % i, [128, 2], F32) for i in range(2)]
    pI = [ps("pI%d" % i, [128, 512], F32) for i in range(2)]
    pTr = ps("pTr", [128, 512], BF16)
    pS = [ps("pS%d" % i, [128, 256], F32) for i in range(2)]
    pO = [ps("pO%d" % i, [128, 2, 132], F32) for i in range(2)]
    pD = ps("pD", [128, 128], F32)

    S.dma("sp", out=ident[:], in_=ident_d[:, :], writes=["ident"])
    S.dma("sp", out=identf[:], in_=identf_d[:, :], writes=["identf"])
    S.dma("sp", out=qs[:], in_=qT_d.rearrange("(h p) t -> p h t", p=128), writes=["qs"])
    S.dma("sp", out=kis[:], in_=kiT_d[:, :], writes=["kis"])
    S.dma("sp", out=qis[:], in_=qiT_d.rearrange("(h p) t -> p h t", p=64), writes=["qis"])
    S.dma("sp", out=wi[:], in_=wi_d.rearrange("(j p) h -> p j h", p=128), writes=["wi"])
    S.dma("sp", out=bias[:], in_=bias_d[:, :], writes=["bias"])
    S.dma("sp", out=rbc[:], in_=rbc_d[:, :], writes=["rbc"])
    S.dma("sp", out=negb[:].rearrange("p a b c -> p (a b c)"), in_=negb_d[:, :], writes=["negb"])
    S.dma("sp", out=sl[:].rearrange("p a b -> p (a b)"), in_=sl_d[:, :], writes=["sl"])
    S.dma("sp", out=ktp1[:], in_=ktp1_d[:, :], writes=["ktp1"])
    S.op("pool", "memset", vs[:, :, 128:129], 1.0, writes=["vs1"])
    S.op("pool", "memset", ys[:], 0.0, writes=["ys"])
    S.op("dve", "tensor_scalar", out=wa[:], in0=wi[:], scalar1=-1.0, scalar2=None, op0=ALU.mult, reads=["wi"], writes=["wa"])
    S.op("dve", "tensor_tensor", out=wa[:], in0=wa[:], in1=wi[:], op=ALU.max, reads=["wa", "wi"], writes=["wa"])
    S.op("act", "activation", out=wsg[:], in_=wi[:], func=AF.Sign, reads=["wi"], writes=["wsg"])

    ucnt = 0
    icnt = 0
    ocnt = 0
    for g in range(GR):
        nkt = 16 * g + 16
        NK = 128 * nkt
        for tile in range(2):
            j = 2 * g + tile
            for kc in range(NK // 512):
                for h in range(HI):
                    pi = icnt % 2
                    icnt += 1
                    S.op("pe", "matmul", pI[pi][:], lhsT=qis[:, h, 128 * j:128 * j + 128], rhs=kis[:, 512 * kc:512 * kc + 512],
                         start=True, stop=True, reads=["qis", "kis"], writes=["pI%d" % pi])
                    S.op("act", "activation", out=rl[pi][:], in_=pI[pi][:], func=AF.Relu, scale=wa[:, j, h:h + 1],
                         reads=["pI%d" % pi, "wa"], writes=["rl%d" % pi])
                    dst = I[:, 512 * kc:512 * kc + 512]
                    if h == 0:
                        S.op("dve", "tensor_scalar", out=dst, in0=rl[pi][:], scalar1=wsg[:, j, 0:1], scalar2=None,
                             op0=ALU.mult, reads=["rl%d" % pi, "wsg"], writes=["I%d" % kc])
                    else:
                        S.op("dve", "scalar_tensor_tensor", out=dst, in0=rl[pi][:], scalar=wsg[:, j, h:h + 1], in1=dst,
                             op0=ALU.mult, op1=ALU.add, reads=["rl%d" % pi, "wsg", "I%d" % kc], writes=["I%d" % kc])
            Iall = ["I%d" % kc for kc in range(NK // 512)]
            S.op("dve", "tensor_reduce", out=bs[:, 0:1], in_=I[:, 0:NK], axis=AX.X, op=ALU.min, reads=Iall, writes=["bs"])
            S.op("dve", "tensor_tensor", out=I[:, 2048 * g:NK].rearrange("p (a b) -> p a b", a=16),
                 in0=I[:, 2048 * g:NK].rearrange("p (a b) -> p a b", a=16), in1=negb[:, :, tile, :], op=ALU.add,
                 reads=Iall + ["negb"], writes=Iall)
            S.op("dve", "tensor_reduce", out=bs[:, 1:2], in_=I[:, 0:NK], axis=AX.X, op=ALU.max, reads=Iall, writes=["bs"])
            S.op("dve", "tensor_scalar", out=bs[:, 0:1], in0=bs[:, 0:1], scalar1=-1.0, scalar2=None, op0=ALU.add,
                 reads=["bs"], writes=["bs"])
            S.op("dve", "scalar_tensor_tensor", out=bs[:, 8:9], in0=bs[:, 1:2], scalar=1.0, in1=bs[:, 0:1],
                 op0=ALU.add, op1=ALU.subtract, reads=["bs"], writes=["bs"])
            for t in range(1, NIT + 1):
                S.op("dve", "tensor_scalar", out=bs[:, 8 + t:9 + t], in0=bs[:, 7 + t:8 + t], scalar1=0.5, scalar2=None,
                     op0=ALU.mult, reads=["bs"], writes=["bs"])
            for t in range(1, NIT + 1):
                S.op("dve", "tensor_tensor", out=bs[:, 2:3], in0=bs[:, 0:1], in1=bs[:, 8 + t:9 + t], op=ALU.add,
                     reads=["bs"], writes=["bs"])
                S.op("dve", "tensor_scalar", out=nsq[:, 0:NK], in0=I[:, 0:NK], scalar1=bs[:, 2:3], scalar2=0.0,
                     op0=ALU.is_ge, op1=ALU.add, accum_out=bs[:, 3:4], reads=Iall + ["bs"], writes=["nsq", "bs"])
                S.op("dve", "tensor_scalar", out=bs[:, 4:5], in0=bs[:, 3:4], scalar1=KSEL - 0.5, scalar2=bs[:, 8 + t:9 + t],
                     op0=ALU.is_ge, op1=ALU.mult, reads=["bs"], writes=["bs"])
                S.op("dve", "tensor_tensor", out=bs[:, 0:1], in0=bs[:, 0:1], in1=bs[:, 4:5], op=ALU.add,
                     reads=["bs"], writes=["bs"])
            S.op("dve", "tensor_scalar", out=nsq[:, 0:NK], in0=I[:, 0:NK], scalar1=bs[:, 0:1], scalar2=NEG,
                 op0=ALU.is_lt, op1=ALU.mult, reads=Iall + ["bs"], writes=["nsq"])
            S.op("dve", "tensor_reduce", out=an[:, 0:nkt], in_=nsq[:, 0:NK].rearrange("p (a b) -> p a b", a=nkt),
                 axis=AX.X, op=ALU.max, reads=["nsq"], writes=["an"])
            S.op("dve", "scalar_tensor_tensor", out=an[:, 0:nkt], in0=an[:, 0:nkt], scalar=-1.0, in1=ktp1[:, 0:nkt],
                 op0=ALU.is_ge, op1=ALU.mult, reads=["an", "ktp1"], writes=["an"])
            S.op("dve", "tensor_reduce", out=dcol[:, 0:1], in_=an[:, 0:nkt], axis=AX.X, op=ALU.max, reads=["an"], writes=["dcol"])
            S.op("dve", "tensor_scalar", out=dcol[:, 1:2], in0=dcol[:, 0:1], scalar1=-128.0, scalar2=rbc[:, g:g + 1],
                 op0=ALU.mult, op1=ALU.add, reads=["dcol", "rbc"], writes=["dcol"])
            S.op("pe", "transpose", out=pD[0:1, 0:128], in_=dcol[:, 1:2], identity=identf[:],
                 reads=["dcol", "identf"], writes=["pD"])
            S.op("act", "copy", out=drow[0:1, 128 * tile:128 * tile + 128], in_=pD[0:1, 0:128], reads=["pD"], writes=["drow"])
            for k4 in range(nkt // 4):
                for k in range(4):
                    kt = 4 * k4 + k
                    S.op("pe", "transpose", out=pTr[:, 128 * k:128 * k + 128], in_=nsq[:, 128 * kt:128 * kt + 128],
                         identity=ident[:], reads=["nsq", "ident"], writes=["pTr"])
                S.op("act", "copy", out=nsT[:, 4 * k4:4 * k4 + 4, 128 * tile:128 * tile + 128],
                     in_=pTr[:, 0:512].rearrange("p (k t) -> p k t", k=4), reads=["pTr"], writes=["nsT"])
        for h in range(HR):
            S.dma("sp", out=ks[:, 0:NK], in_=kT_d[128 * h:128 * h + 128, 0:NK], writes=["ks"])
            for q4 in range(g + 1):
                S.dma("pool", out=vs[:, 16 * q4:16 * q4 + 16, 0:128],
                      in_=v_d[2048 * q4:2048 * q4 + 2048, 128 * h:128 * h + 128].rearrange("(kt p) e -> p kt e", p=128),
                      writes=["vs_%d" % q4])
            ob = ocnt % 2
            ocnt += 1
            def emit_qk(kt, si):
                S.op("pe", "matmul", pS[si][:], lhsT=ks[:, 128 * kt:128 * kt + 128], rhs=qs[:, h, 256 * g:256 * g + 256],
                     start=True, stop=False, reads=["ks", "qs"], writes=["pS%d" % si])
                S.op("pe", "matmul", pS[si][:], lhsT=sl[0:1, h, :], rhs=drow[0:1, :], start=False, stop=False,
                     reads=["sl", "drow"], writes=["pS%d" % si])
                S.op("pe", "matmul", pS[si][:], lhsT=ident[:], rhs=nsT[:, kt, :], start=False, stop=True,
                     reads=["ident", "nsT"], writes=["pS%d" % si])

            def emit_rest(kt, si, pti):
                bcol = (h * 4 + g) * 64 + kt
                S.op("act", "activation", out=pT[pti][:], in_=pS[si][:], func=AF.Exp, bias=bias[:, bcol:bcol + 1],
                     scale=SCALE, reads=["pS%d" % si, "bias"], writes=["pT%d" % pti])
                for tile in range(2):
                    S.op("pe", "matmul", pO[ob][:, tile, 0:129], lhsT=pT[pti][:, 128 * tile:128 * tile + 128],
                         rhs=vs[:, kt, 0:129], start=(kt == 0 and tile == 0), stop=(kt == nkt - 1 and tile == 1),
                         reads=["pT%d" % pti, "vs_%d" % (kt // 16), "vs1"], writes=["pO%d" % ob])

            for idx in range(nkt + 1):
                if idx < nkt:
                    emit_qk(idx, (ucnt + idx) % 2)
                if idx >= 1:
                    emit_rest(idx - 1, (ucnt + idx - 1) % 2, (ucnt + idx - 1) % 3)
            ucnt += nkt
            for tile in range(2):
                f = fs[tile]
                S.op("dve", "reciprocal", out=f[:, 0:1], in_=pO[ob][:, tile, 128:129], reads=["pO%d" % ob], writes=["fs%d" % tile])
                S.op("dve", "tensor_scalar", out=ys[:, 2 * g + tile, 128 * h:128 * h + 128], in0=pO[ob][:, tile, 0:128],
                     scalar1=f[:, 0:1], scalar2=None, op0=ALU.mult, reads=["pO%d" % ob, "fs%d" % tile], writes=["ys"])
    S.dma("sp", out=y_d.rearrange("(j p) e -> p j e", p=128), in_=ys[:], reads=["ys"], writes=["yout"])
    alltoks = [(k, v_[1]) for k, v_ in S.dma_sems.items() if v_[1] > 0]
    S.wait_all("sp", alltoks)
    S.emit()
    S.close()
    for c in reversed(ctxs):
        c.__exit__(None, None, None)
    return nc


F32 = mybir.dt.float32
BF16 = mybir.dt.bfloat16
AF = mybir.ActivationFunctionType
ALU = mybir.AluOpType
ALPHA = 8.0 ** 0.25
EPS = 1e-5

NT = 1024
D = 2048
FF = 8192
G = 512


def build_p3():
    nc = bass.Bass("TRN2", target_bir_lowering=False)
    xres = nc.dram_tensor("xres", [NT, D], F32, kind="ExternalInput").ap()
    mixT = nc.dram_tensor("mixT", [D, NT], BF16, kind="ExternalInput").ap()
    w_out = nc.dram_tensor("w_out", [D, D], F32, kind="ExternalInput").ap()
    w_up = nc.dram_tensor("w_up", [D, FF], F32, kind="ExternalInput").ap()
    w_down = nc.dram_tensor("w_down", [FF, D], F32, kind="ExternalInput").ap()
    lnp = nc.dram_tensor("lnp", [4, D], F32, kind="ExternalInput").ap()
    ident_d = nc.dram_tensor("identf", [128, 128], F32, kind="ExternalInput").ap()
    x2 = nc.dram_tensor("x2", [NT, D], F32, kind="ExternalOutput").ap()

    S = Sched(nc)
    S.open()
    ctxs = []

    def sb(name, shape, dt):
        c = nc.sbuf_tensor(name, shape, dt)
        t = c.__enter__()
        ctxs.append(c)
        return t

    def ps(name, shape, dt):
        c = nc.psum_tensor(name, shape, dt)
        t = c.__enter__()
        ctxs.append(c)
        return t

    ident = sb("ident_s", [128, 128], F32)
    lnb = sb("lnb", [128, 4, D], F32)
    big = sb("big", [128, 64 * G], BF16)
    wo = big[:].rearrange("p (c d) -> p c d", c=16)
    hT = big[:].rearrange("p (f t) -> p f t", f=64)
    mx = [sb("mx%d" % i, [128, 16, 128], BF16) for i in range(2)]
    xr = [sb("xr%d" % i, [128, D], F32) for i in range(1)]
    r = [sb("r%d" % i, [128, D], F32) for i in range(2)]
    x1 = sb("x1", [128, 4, D], F32)
    x1T = sb("x1T", [128, 16, G], BF16)
    wu = [sb("wu%d" % i, [128, 16, 256], BF16) for i in range(2)]
    wd = [sb("wd%d" % i, [128, 1024], BF16) for i in range(4)]
    sq = [sb("sq%d" % i, [128, G], F32) for i in range(2)]
    st = sb("st", [128, 4, 6], F32)
    mv = sb("mv", [128, 2], F32)
    rstd = sb("rstd", [128, 1], F32)
    nmr = sb("nmr", [128, 1], F32)
    epsb = sb("epsb", [128, 1], F32)
    pacc = [ps("pacc%d" % i, [128, 512], F32) for i in range(8)]

    E = S.eng
    S.dma("sp", out=ident[:], in_=ident_d[:, :], writes=["ident"])
    S.dma("sp", out=lnb[:].rearrange("p a d -> p (a d)"),
                                          in_=lnp.rearrange("a d -> (a d)").partition_broadcast(128),
          writes=["lnb"])
    S.op("pool", "memset", epsb[:], EPS, writes=["epsb"])

    pb = [0]

    def layer_norm(src, srcname, gi, outs):
        for c in range(4):
            S.op("dve", "bn_stats", out=st[:, c, :], in_=src[:, 512 * c:512 * c + 512],
                 reads=[srcname], writes=["st%d" % c])
        S.op("dve", "bn_aggr", out=mv[:], in_=st[:].rearrange("p a b -> p (a b)"),
             reads=["st%d" % c for c in range(4)], writes=["mv"])
        S.op("act", "activation", out=rstd[:], in_=mv[:, 1:2], func=AF.Sqrt, bias=epsb[:], scale=1.0,
             reads=["mv", "epsb"], writes=["rstd"])
        S.op("dve", "reciprocal", out=rstd[:], in_=rstd[:], reads=["rstd"], writes=["rstd"])
        S.op("dve", "tensor_scalar", out=src, in0=src, scalar1=mv[:, 0:1], scalar2=rstd[:],
                                                   op0=ALU.subtract, op1=ALU.mult,
             reads=[srcname, "mv", "rstd"], writes=[srcname])
        S.op("pool", "tensor_tensor", out=src, in0=src, in1=lnb[:, gi, :], op=ALU.mult,
             reads=[srcname, "lnb"], writes=[srcname])
        for (ap, name, eng) in outs:
            S.op(eng, "tensor_tensor", out=ap, in0=src, in1=lnb[:, gi + 1, :], op=ALU.add,
                 reads=[srcname, "lnb"], writes=[name])

    for g in range(NT // G):
        t0 = g * G
        for q in range(4):
            S.dma("pool",
                out=wo[:, 4 * q:4 * q + 4, :],
                in_=w_out[512 * q:512 * q + 512, :].rearrange("(c p) d -> p c d", p=128),
                writes=["wo%d" % q] + ["hT%d" % f for f in range(16 * q, 16 * q + 16)])
        for t in range(4):
            i = t % 2
            S.dma("sp",
                out=mx[i][:], in_=mixT[:, t0 + 128 * t:t0 + 128 * t + 128].rearrange("(c p) t -> p c t", p=128),
                writes=["mx%d" % i])
            S.dma("sp", out=xr[0][:], in_=xres[t0 + 128 * t:t0 + 128 * t + 128, :],
                  writes=["xr0"])
            for dg in range(4):
                for c in range(16):
                    S.op("pe", "matmul",
                        pacc[dg][:], lhsT=mx[i][:, c, :], rhs=wo[:, c, 512 * dg:512 * dg + 512],
                        start=(c == 0), stop=(c == 15),
                        reads=["mx%d" % i, "wo%d" % (c // 4)], writes=["pacc%d" % dg])
                S.op("dve", "scalar_tensor_tensor",
                    out=r[i][:, 512 * dg:512 * dg + 512], in0=xr[0][:, 512 * dg:512 * dg + 512], scalar=ALPHA,
                    in1=pacc[dg][:], op0=ALU.mult, op1=ALU.add,
                    reads=["xr0", "pacc%d" % dg], writes=["r%d" % i])
            layer_norm(r[i][:], "r%d" % i, 0, [(x1[:, t, :], "x1_%d" % t, "pool")])
            for c4 in range(4):
                pt = pacc[4 + (c4 % 2)]
                for k in range(4):
                    c = 4 * c4 + k
                    S.op("pe", "transpose", out=pt[:, 128 * k:128 * k + 128], in_=x1[:, t, 128 * c:128 * c + 128],
                         identity=ident[:], reads=["x1_%d" % t, "ident"], writes=["pacc%d" % (4 + c4 % 2)])
                S.op("act", "copy", out=x1T[:, 4 * c4:4 * c4 + 4, 128 * t:128 * t + 128],
                     in_=pt[:, 0:512].rearrange("p (k t) -> p k t", k=4),
                     reads=["pacc%d" % (4 + c4 % 2)], writes=["x1T_%d" % t])
        for f2 in range(32):
            wi = f2 % 2
            S.dma("pool", out=wu[wi][:], in_=w_up[:, 256 * f2:256 * f2 + 256].rearrange("(c p) f -> p c f", p=128),
                  writes=["wu%d" % wi])
            for fh in range(2):
                f = 2 * f2 + fh
                pa = pacc[f % 4]
                for c in range(16):
                    S.op("pe", "matmul", pa[:], lhsT=wu[wi][:, c, 128 * fh:128 * fh + 128], rhs=x1T[:, c, :],
                         start=(c == 0), stop=(c == 15),
                         reads=["wu%d" % wi] + ["x1T_%d" % t for t in range(4)], writes=["pacc%d" % (f % 4)])
                si = f % 2
                S.op("act", "activation", out=sq[si][:], in_=pa[:], func=AF.Square,
                     reads=["pacc%d" % (f % 4)], writes=["sq%d" % si])
                S.op("dve", "scalar_tensor_tensor", out=hT[:, f, :], in0=pa[:], scalar=0.0, in1=sq[si][:],
                     op0=ALU.is_gt, op1=ALU.mult,
                     reads=["pacc%d" % (f % 4), "sq%d" % si], writes=["hT%d" % f, "wo%d" % (f // 16)])
        for dh in range(2):
            for f in range(64):
                wi = f % 4
                S.dma("pool", out=wd[wi][:], in_=w_down[128 * f:128 * f + 128, 1024 * dh:1024 * dh + 1024],
                      writes=["wd%d" % wi])
                for t in range(4):
                    for d2 in range(2):
                        S.op("pe", "matmul", pacc[2 * t + d2][:], lhsT=hT[:, f, 128 * t:128 * t + 128],
                             rhs=wd[wi][:, 512 * d2:512 * d2 + 512], start=(f == 0), stop=(f == 63),
                             reads=["hT%d" % f, "wd%d" % wi], writes=["pacc%d" % (2 * t + d2)])
            for t in range(4):
                for d2 in range(2):
                    dg = 2 * dh + d2
                    S.op("dve", "scalar_tensor_tensor", out=x1[:, t, 512 * dg:512 * dg + 512],
                         in0=x1[:, t, 512 * dg:512 * dg + 512], scalar=ALPHA, in1=pacc[2 * t + d2][:],
                         op0=ALU.mult, op1=ALU.add, reads=["x1_%d" % t, "pacc%d" % (2 * t + d2)], writes=["x1_%d" % t])
        for t in range(4):
            layer_norm(x1[:, t, :], "x1_%d" % t, 2, [(x1[:, t, :], "x1_%d" % t, "pool")])
            S.dma("sp", out=x2[t0 + 128 * t:t0 + 128 * t + 128, :], in_=x1[:, t, :],
                  reads=["x1_%d" % t], writes=["x2out"])
    alltoks = [(k, v[1]) for k, v in S.dma_sems.items() if v[1] > 0]
    S.wait_all("sp", alltoks)
    S.emit()
    S.close()
    for c in reversed(ctxs):
        c.__exit__(None, None, None)
    return nc


N_CORES = 8
DEPTH = 4
_BF = ml_dtypes.bfloat16
_PROGS = {}


def _prog(name, builder):
    if name not in _PROGS:
        _PROGS[name] = builder()
    return _PROGS[name]


def _run(nc, in_maps):
    res = run_bass_kernel_spmd(nc, in_maps, core_ids=list(range(N_CORES)))
    return res.results


def kernel(x, w_in, conv_m, b_i, b_f, m_norm_g, lam_q1, lam_k1, lam_q2, lam_k2,
           c_norm_g, w_out, ln1_g, ln1_b, w_up, w_down, ln2_g, ln2_b):
    f32 = np.float32
    toks = [np.concatenate([np.arange(128 * b, 128 * b + 128) for b in core_blocks(c)]) for c in range(N_CORES)]
    ident_b = np.eye(128).astype(_BF)
    ident_f = np.eye(128, dtype=f32)
    ar = np.arange(128)
    tri = (ar[:, None] <= ar[None, :]).astype(f32)
    slopes_c = 2.0 ** (-8.0 * np.arange(1, 5) / 4)
    ctab = [c_tables(c, slopes_c, 4) for c in range(N_CORES)]
    atab = [a_tables(c) for c in range(N_CORES)]
    p1 = _prog("p1", build_p1)
    pa = _prog("pa", build_pa)
    pb = _prog("pb", build_pb)
    pc = _prog("pc", build_pc)
    p3 = _prog("p3", build_p3)
    xs = [np.ascontiguousarray(x[0][toks[c]]) for c in range(N_CORES)]
    for l in range(DEPTH):
        w_in_l = np.ascontiguousarray(w_in[l])
        r1 = _run(p1, [dict(x=xs[c], w_in=w_in_l, ident=ident_b) for c in range(N_CORES)])

        def gather_T(name, rows, dt):
            out = np.empty((rows, SEQ), dt)
            for c in range(N_CORES):
                out[:, toks[c]] = r1[c][name]
            return out

        def gather_tok(name, cols, dt):
            out = np.empty((SEQ, cols), dt)
            for c in range(N_CORES):
                out[toks[c]] = r1[c][name]
            return out

        kaT = gather_T("kaT", 768, _BF)
        va = gather_tok("va", 768, _BF)
        kiT = gather_T("kiT", 64, _BF)
        kcT = gather_T("kcT", 512, _BF)
        vc = gather_tok("vc", 512, _BF)
        qmT = gather_T("qmT", 768, f32)
        kmT = gather_T("kmT", 768, f32)
        vm = gather_tok("vm", 768, _BF)
        om = gather_tok("om", 768, f32)
        ifm = gather_tok("ifm", 12, f32)
        ra = _run(pa, [dict(qT=r1[c]["qaT"], kT=kaT, v=va, qiT=r1[c]["qiT"], kiT=kiT, wi=r1[c]["wi"],
                            bias=atab[c]["bias"], rbc=atab[c]["rbc"], negb=atab[c]["negb"].astype(_BF),
                            sl=atab[c]["sl"].astype(_BF), ktp1=atab[c]["ktp1"], ident=ident_b, identf=ident_f)
                       for c in range(N_CORES)])
        lam_init = 0.8 - 0.6 * math.exp(-0.3 * l)
        lamp = np.stack([lam_q1[l], lam_k1[l], lam_q2[l], lam_k2[l]]).astype(f32)
        cst = np.array([lam_init, 1.0 - lam_init], f32)
        rc = _run(pc, [dict(qT=r1[c]["qcT"], kT=kcT, v=vc, lam=lamp, gc=np.ascontiguousarray(c_norm_g[l]), cst=cst,
                            bias=ctab[c][0], mask=ctab[c][1].astype(_BF)) for c in range(N_CORES)])
        conv = conv_m[l][:, 0, :]
        inb = []
        for c in range(N_CORES):
            h = c % 6
            cw = np.concatenate([conv[:, 128 * h:128 * h + 128].T, conv[:, 768 + 128 * h:768 + 128 * h + 128].T], axis=1)
            inb.append(dict(qT=np.ascontiguousarray(qmT[128 * h:128 * h + 128]), kT=np.ascontiguousarray(kmT[128 * h:128 * h + 128]),
                            v=np.ascontiguousarray(vm[:, 128 * h:128 * h + 128]), o=np.ascontiguousarray(om[:, 128 * h:128 * h + 128]),
                            ifg=np.ascontiguousarray(np.stack([ifm[:, h], ifm[:, 6 + h]], 1)), cw=np.ascontiguousarray(cw.astype(f32)),
                            bif=np.array([b_i[l][h], b_f[l][h]], f32), gn=np.ascontiguousarray(m_norm_g[l][128 * h:128 * h + 128]),
                            tri=tri, ident=ident_b))
        rb = _run(pb, inb)
        yb = np.concatenate([rb[h]["y"] for h in range(6)], axis=1)
        lnp = np.stack([ln1_g[l], ln1_b[l], ln2_g[l], ln2_b[l]]).astype(f32)
        in3 = []
        for c in range(N_CORES):
            mixed = np.concatenate([ra[c]["y"], yb[toks[c]], rc[c]["y"]], axis=1)
            in3.append(dict(xres=xs[c], mixT=np.ascontiguousarray(mixed.T), w_out=np.ascontiguousarray(w_out[l]),
                            w_up=np.ascontiguousarray(w_up[l]), w_down=np.ascontiguousarray(w_down[l]), lnp=lnp, identf=ident_f))
        r3 = _run(p3, in3)
        xs = [r3[c]["x2"] for c in range(N_CORES)]
    out = np.empty((1, SEQ, D), f32)
    for c in range(N_CORES):
        out[0, toks[c]] = xs[c]
    return out
```

```python
import math
import numpy as np
import ml_dtypes
import concourse.bass as bass
import concourse.mybir as mybir
from concourse.bass_utils import run_bass_kernel_spmd


ENGS = ("pe", "act", "dve", "pool", "sp")


class Sched:
    def __init__(self, nc, n_dma_sems=12):
        self.nc = nc
        self.eng = {"pe": nc.tensor, "act": nc.scalar, "dve": nc.vector,
                    "pool": nc.gpsimd, "sp": nc.sync}
        self.sem = {}
        self.cnt = {e: 0 for e in ENGS}
        self.stream = {e: [] for e in ENGS}
        self.waited = {e: {} for e in ENGS}
        self.last_w = {}
        self.readers = {}
        self.n_dma_sems = n_dma_sems
        self.dma_sems = {}
        self.dma_rr = {e: 0 for e in ENGS}
        self._ctx = []

    def open(self):
        nc = self.nc
        for e in ENGS:
            c = nc.semaphore("s_" + e)
            self.sem[e] = c.__enter__()
            self._ctx.append(c)
        for q in ("sp", "pool", "act"):
            for i in range(self.n_dma_sems):
                c = nc.semaphore("d_%s%d" % (q, i))
                self.dma_sems[(q, i)] = [c.__enter__(), 0]
                self._ctx.append(c)

    def _sem_of(self, key):
        return self.sem[key] if isinstance(key, str) else self.dma_sems[key][0]

    def _deps(self, eng, reads, writes):
        toks = []
        for b in reads:
            t = self.last_w.get(b)
            if t is not None:
                toks.append(t)
        for b in writes:
            t = self.last_w.get(b)
            if t is not None:
                toks.append(t)
            toks.extend(self.readers.get(b, ()))
        need = {}
        for key, val in toks:
            if key == eng and eng == "pe":
                continue
            if val > need.get(key, 0):
                need[key] = val
        out = []
        w = self.waited[eng]
        for key, val in need.items():
            if w.get(key, 0) >= val:
                continue
            w[key] = val
            out.append((key, val))
        return out

    def _commit(self, tok, reads, writes):
        for b in reads:
            self.readers.setdefault(b, []).append(tok)
        for b in writes:
            self.last_w[b] = tok
            self.readers[b] = []

    def op(self, eng, fname, *args, reads=(), writes=(), **kw):
        fn = (fname, args, kw)
        waits = self._deps(eng, reads, writes)
        self.cnt[eng] += 1
        tok = (eng, self.cnt[eng])
        self.stream[eng].append((waits, fn, (eng, 1)))
        self._commit(tok, reads, writes)
        return tok

    def dma(self, queue, *args, reads=(), writes=(), fname="dma_start", **kw):
        if fname != "dma_start":
            args, kw = kw["args"], kw["kw"]
        fn = (fname, args, kw)
        idx = self.dma_rr[queue]
        self.dma_rr[queue] = (idx + 1) % self.n_dma_sems
        key = (queue, idx)
        ent = self.dma_sems[key]
        waits = self._deps(queue, reads, writes)
        w = self.waited[queue]
        if ent[1] > 0 and w.get(key, 0) < ent[1]:
            waits.append((key, ent[1]))
            w[key] = ent[1]
        ent[1] += 16
        tok = (key, ent[1])
        self.stream[queue].append((waits, fn, (key, 16)))
        self._commit(tok, reads, writes)
        return tok

    def wait_all(self, eng, toks):
        waits = []
        w = self.waited[eng]
        for key, val in toks:
            if w.get(key, 0) < val:
                w[key] = val
                waits.append((key, val))
        self.stream[eng].append((waits, None, None))

    def emit(self):
        nc = self.nc
        with nc.Block() as block:
            def mk(e):
                def body(engine):
                    for waits, fn, inc in self.stream[e]:
                        for key, val in waits:
                            engine.wait_ge(self._sem_of(key), val)
                        if fn is not None:
                            ins = getattr(engine, fn[0])(*fn[1], **fn[2])
                            ins.then_inc(self._sem_of(inc[0]), inc[1])
                return body
            block.tensor(mk("pe"))
            block.scalar(mk("act"))
            block.vector(mk("dve"))
            block.gpsimd(mk("pool"))
            block.sync(mk("sp"))

    def close(self):
        for c in reversed(self._ctx):
            c.__exit__(None, None, None)
        self._ctx = []


F32 = mybir.dt.float32
BF16 = mybir.dt.bfloat16
AF = mybir.ActivationFunctionType
ALU = mybir.AluOpType
NT = 1024
D = 2048
DIN = 7508

GROUPS = [
    ("qaT", 0, 768, "F", BF16), ("kaT", 768, 768, "F", BF16), ("va", 1536, 768, "T", BF16),
    ("qiT", 2304, 512, "F", BF16), ("kiT", 2816, 64, "F", BF16), ("wi", 2880, 8, "T", F32),
    ("qmT", 2888, 768, "F", F32), ("kmT", 3656, 768, "F", F32), ("vm", 4424, 768, "T", BF16),
    ("om", 5192, 768, "T", F32), ("ifm", 5960, 12, "T", F32),
    ("qcT", 5972, 512, "F", BF16), ("kcT", 6484, 512, "F", BF16), ("vc", 6996, 512, "T", BF16),
]


def build_p1():
    nc = bass.Bass("TRN2", target_bir_lowering=False)
    x = nc.dram_tensor("x", [NT, D], F32, kind="ExternalInput").ap()
    w_in = nc.dram_tensor("w_in", [D, DIN], F32, kind="ExternalInput").ap()
    ident_d = nc.dram_tensor("ident", [128, 128], BF16, kind="ExternalInput").ap()
    outs = {}
    for (name, off, n, lay, dt) in GROUPS:
        shape = [n, NT] if lay == "F" else [NT, n]
        outs[name] = nc.dram_tensor(name, shape, dt, kind="ExternalOutput").ap()

    S = Sched(nc)
    S.open()
    ctxs = []

    def sb(name, shape, dt):
        c = nc.sbuf_tensor(name, shape, dt)
        t = c.__enter__()
        ctxs.append(c)
        return t

    def ps(name, shape, dt):
        c = nc.psum_tensor(name, shape, dt)
        t = c.__enter__()
        ctxs.append(c)
        return t

    ident = sb("ident_s", [128, 128], BF16)
    xT = sb("xT", [128, 16, NT], BF16)
    xf = [sb("xf%d" % i, [128, D], F32) for i in range(2)]
    xb = [sb("xb%d" % i, [128, D], BF16) for i in range(2)]
    wb = [sb("wb%d" % i, [128, 16, 512], BF16) for i in range(3)]
    stg = [sb("stg%d" % i, [128, 512], F32) for i in range(4)]
    pacc = [ps("pacc%d" % i, [128, 512], F32) for i in range(4)]
    ptr = [ps("ptr%d" % i, [128, 512], BF16) for i in range(2)]

    S.dma("sp", out=ident[:], in_=ident_d[:, :], writes=["ident"])
    for t in range(8):
        i = t % 2
        S.dma("sp", out=xf[i][:], in_=x[128 * t:128 * t + 128, :], writes=["xf%d" % i])
        S.op("dve" if t % 2 == 0 else "pool", "tensor_copy", out=xb[i][:], in_=xf[i][:],
             reads=["xf%d" % i], writes=["xb%d" % i])
        for c4 in range(4):
            pt = ptr[c4 % 2]
            for k in range(4):
                c = 4 * c4 + k
                S.op("pe", "transpose", out=pt[:, 128 * k:128 * k + 128], in_=xb[i][:, 128 * c:128 * c + 128],
                     identity=ident[:], reads=["xb%d" % i, "ident"], writes=["ptr%d" % (c4 % 2)])
            S.op("act", "copy", out=xT[:, 4 * c4:4 * c4 + 4, 128 * t:128 * t + 128],
                 in_=pt[:, 0:512].rearrange("p (k t) -> p k t", k=4),
                 reads=["ptr%d" % (c4 % 2)], writes=["xT"])

    wcnt = [0]
    ecnt = [0]

    def evac(pa, pname, rows, cols, dt):
        k = ecnt[0] % 4
        ecnt[0] += 1
        sv = stg[k][:] if dt == F32 else stg[k][:].bitcast(BF16)
        dst = sv[0:rows, 0:cols]
        if k % 2 == 0:
            S.op("act", "copy", out=dst, in_=pa[0:rows, 0:cols], reads=[pname], writes=["stg%d" % k])
        else:
            S.op("dve", "tensor_copy", out=dst, in_=pa[0:rows, 0:cols], reads=[pname], writes=["stg%d" % k])
        return dst, "stg%d" % k

    for (name, off, n, lay, dt) in GROUPS:
        for c0 in range(0, n, 512):
            ncol = min(512, n - c0)
            wi = wcnt[0] % 3
            wcnt[0] += 1
            S.dma("pool", out=wb[wi][:, :, 0:ncol],
                  in_=w_in[:, off + c0:off + c0 + ncol].rearrange("(c p) e -> p c e", p=128),
                  writes=["wb%d" % wi])
            if lay == "F":
                for e0 in range(0, ncol, 128):
                    ne = min(128, ncol - e0)
                    for tg in range(2):
                        pi = ecnt[0] % 4
                        pa = pacc[pi]
                        for c in range(16):
                            S.op("pe", "matmul", pa[0:ne, :], lhsT=wb[wi][:, c, e0:e0 + ne],
                                 rhs=xT[:, c, 512 * tg:512 * tg + 512], start=(c == 0), stop=(c == 15),
                                 reads=["wb%d" % wi, "xT"], writes=["pacc%d" % pi])
                        dst, sname = evac(pa, "pacc%d" % pi, ne, 512, dt)
                        S.dma("sp", out=outs[name][c0 + e0:c0 + e0 + ne, 512 * tg:512 * tg + 512], in_=dst,
                              reads=[sname], writes=["out_" + name])
            else:
                for t in range(8):
                    pi = ecnt[0] % 4
                    pa = pacc[pi]
                    for c in range(16):
                        S.op("pe", "matmul", pa[:, 0:ncol], lhsT=xT[:, c, 128 * t:128 * t + 128],
                             rhs=wb[wi][:, c, 0:ncol], start=(c == 0), stop=(c == 15),
                             reads=["wb%d" % wi, "xT"], writes=["pacc%d" % pi])
                    dst, sname = evac(pa, "pacc%d" % pi, 128, ncol, dt)
                    S.dma("sp", out=outs[name][128 * t:128 * t + 128, c0:c0 + ncol], in_=dst,
                          reads=[sname], writes=["out_" + name])

    alltoks = [(k, v[1]) for k, v in S.dma_sems.items() if v[1] > 0]
    S.wait_all("sp", alltoks)
    S.emit()
    S.close()
    for c in reversed(ctxs):
        c.__exit__(None, None, None)
    return nc


FLAGS = ''

F32 = mybir.dt.float32
BF16 = mybir.dt.bfloat16
AF = mybir.ActivationFunctionType
ALU = mybir.AluOpType
NT = 1024
SEQ = 8192
H = 4
EPS = 1e-5


def core_blocks(c):
    out = []
    for g in range(4):
        out += [16 * g + c, 16 * g + 15 - c]
    return out


def c_tables(c, slopes, nheads):
    blocks = core_blocks(c)
    bias = np.full((128, nheads, 4, 64, 2), -30000.0, np.float32)
    ar = np.arange(128, dtype=np.float32)
    for g in range(4):
        for tile in range(2):
            qb = blocks[2 * g + tile]
            qref = 128 * qb + 127
            for kt in range(qb + 1):
                for h in range(nheads):
                    bias[:, h, g, kt, tile] = slopes[h] * (128 * kt + ar - qref)
    mask = np.zeros((128, 16, 2, 128), np.float32)
    tri = (ar[:, None] <= ar[None, :]).astype(np.float32)
    for tile in range(2):
        qslot = c if tile == 0 else 15 - c
        for i in range(16):
            if i < qslot:
                mask[:, i, tile, :] = 1.0
            elif i == qslot:
                mask[:, i, tile, :] = tri
    return bias.reshape(128, -1), mask.reshape(128, -1)


def build_pc(HR=4, GR=4):
    nc = bass.Bass("TRN2", target_bir_lowering=False)
    qT = nc.dram_tensor("qT", [512, NT], BF16, kind="ExternalInput").ap()
    kT = nc.dram_tensor("kT", [512, SEQ], BF16, kind="ExternalInput").ap()
    v = nc.dram_tensor("v", [SEQ, 512], BF16, kind="ExternalInput").ap()
    lam_d = nc.dram_tensor("lam", [4, 64], F32, kind="ExternalInput").ap()
    gc_d = nc.dram_tensor("gc", [512], F32, kind="ExternalInput").ap()
    cst_d = nc.dram_tensor("cst", [2], F32, kind="ExternalInput").ap()
    bias_d = nc.dram_tensor("bias", [128, H * 4 * 64 * 2], F32, kind="ExternalInput").ap()
    mask_d = nc.dram_tensor("mask", [128, 16 * 2 * 128], BF16, kind="ExternalInput").ap()
    y = nc.dram_tensor("y", [NT, 512], BF16, kind="ExternalOutput").ap()

    S = Sched(nc)
    S.open()
    ctxs = []

    def sb(name, shape, dt):
        c = nc.sbuf_tensor(name, shape, dt)
        t = c.__enter__()
        ctxs.append(c)
        return t

    def ps(name, shape, dt):
        c = nc.psum_tensor(name, shape, dt)
        t = c.__enter__()
        ctxs.append(c)
        return t

    qs = sb("qs", [128, H, 2, NT], BF16)
    ks = [sb("ks%d" % i, [128, SEQ], BF16) for i in range(2)]
    vs = [sb("vs%d" % i, [128, 64, 132], BF16) for i in range(2)]
    bias = sb("bias_s", [128, H * 4 * 64 * 2], F32)
    mask = sb("mask_s", [128, 16, 256], BF16)
    lamb = sb("lamb", [128, 4, 64], F32)
    gcb = sb("gcb", [128, 512], F32)
    cst = sb("cst_s", [128, 2], F32)
    sm = sb("sm", [128, 16], F32)
    epsb = sb("epsb", [128, 1], F32)
    pT = [sb("pT%d" % i, [128, 256], BF16) for i in range(4)]
    ys = sb("ys", [128, 8, 512], BF16)
    t1 = [sb("t1_%d" % i, [128, 128], F32) for i in range(2)]
    t2 = [sb("t2_%d" % i, [128, 128], F32) for i in range(2)]
    junk = sb("junk", [128, 128], F32)
    fs = [sb("fs%d" % i, [128, 8], F32) for i in range(2)]
    pS = [ps("pS%d" % i, [128, 512], F32) for i in range(2)]
    pO = [ps("pO%d" % i, [128, 2, 132], F32) for i in range(4)]

    S.op("pool", "memset", qs[:], 0.0, writes=["qs"])
    for m in range(2):
        S.dma("sp", out=qs[64 * m:64 * m + 64, :, m, :], in_=qT.rearrange("(h p) t -> p h t", p=128)[64 * m:64 * m + 64],
              writes=["qs"])
    S.dma("sp", out=bias[:], in_=bias_d[:, :], writes=["bias"])
    S.dma("sp", out=mask[:].rearrange("p a b -> p (a b)"), in_=mask_d[:, :], writes=["mask"])
    S.dma("sp", out=lamb[:].rearrange("p a b -> p (a b)"), in_=lam_d.rearrange("a b -> (a b)").partition_broadcast(128),
          writes=["lamb"])
    S.dma("sp", out=gcb[:], in_=gc_d.partition_broadcast(128), writes=["gcb"])
    S.dma("sp", out=cst[:], in_=cst_d.partition_broadcast(128), writes=["cst"])
    S.op("pool", "memset", epsb[:], EPS, writes=["epsb"])
    S.op("pool", "memset", ys[:], 0.0, writes=["ys"])
    for i in range(2):
        S.op("pool", "memset", vs[i][:, :, 128:129], 1.0, writes=["vone%d" % i])
    S.op("dve", "tensor_tensor", out=lamb[:, 0, :], in0=lamb[:, 0, :], in1=lamb[:, 1, :], op=ALU.mult,
         reads=["lamb"], writes=["lamb"])
    S.op("dve", "tensor_tensor", out=lamb[:, 2, :], in0=lamb[:, 2, :], in1=lamb[:, 3, :], op=ALU.mult,
         reads=["lamb"], writes=["lamb"])
    S.op("dve", "reduce_sum", out=sm[:, 1:2], in_=lamb[:, 0, :], axis=mybir.AxisListType.X, reads=["lamb"], writes=["sm1"])
    S.op("dve", "reduce_sum", out=sm[:, 2:3], in_=lamb[:, 2, :], axis=mybir.AxisListType.X, reads=["lamb"], writes=["sm2"])
    S.op("act", "activation", out=sm[:, 1:3], in_=sm[:, 1:3], func=AF.Exp, reads=["sm1", "sm2"], writes=["sm12"])
    S.op("dve", "tensor_tensor", out=sm[:, 0:1], in0=sm[:, 2:3], in1=sm[:, 1:2], op=ALU.subtract,
         reads=["sm12"], writes=["sm0"])
    S.op("dve", "tensor_tensor", out=sm[:, 0:1], in0=sm[:, 0:1], in1=cst[:, 0:1], op=ALU.subtract,
         reads=["sm0", "cst"], writes=["sm0"])
    S.op("dve", "tensor_scalar", out=gcb[:], in0=gcb[:], scalar1=cst[:, 1:2], scalar2=None, op0=ALU.mult,
         reads=["gcb", "cst"], writes=["gcb"])

    ucnt = 0
    fcnt = 0
    for h in range(HR):
        hi = h % 2
        S.dma("sp", out=ks[hi][:], in_=kT[128 * h:128 * h + 128, :], writes=["ks%d" % hi])
        for q4 in range(4):
            S.dma("pool", out=vs[hi][:, 16 * q4:16 * q4 + 16, 0:128],
                  in_=v[2048 * q4:2048 * q4 + 2048, 128 * h:128 * h + 128].rearrange("(kt p) e -> p kt e", p=128),
                  writes=["vs%d_%d" % (hi, q4)])
        units = [(g, kt) for g in range(GR) for kt in range(16 * g + 16)]

        def emit_qk(u, sbuf_i):
            g, kt = u
            for m in range(2):
                S.op("pe", "matmul", pS[sbuf_i][:, 256 * m:256 * m + 256],
                     lhsT=ks[hi][:, 128 * kt:128 * kt + 128],
                     rhs=qs[:, h, m, 256 * g:256 * g + 256], start=True, stop=True,
                     reads=["ks%d" % hi, "qs"], writes=["pS%d" % sbuf_i])

        def emit_rest(u, sbuf_i):
            nonlocal fcnt
            g, kt = u
            nkt = 16 * g + 16
            ob = (h * 4 + g) % 2
            pSb = pS[sbuf_i]
            for m in range(2):
                pt = pT[2 * sbuf_i + m]
                ptn = "pT%d" % (2 * sbuf_i + m)
                for tile in range(2):
                    bcol = ((h * 4 + g) * 64 + kt) * 2 + tile
                    S.op("act", "activation", out=pt[:, 128 * tile:128 * tile + 128],
                         in_=pSb[:, 256 * m + 128 * tile:256 * m + 128 * tile + 128], func=AF.Exp,
                         bias=bias[:, bcol:bcol + 1], scale=0.125,
                         reads=["pS%d" % sbuf_i, "bias"], writes=[ptn])
                if kt >= 16 * g:
                    S.op("dve" if m == 0 else "pool", "tensor_tensor", out=pt[:], in0=pt[:],
                         in1=mask[:, kt - 16 * g, :], op=ALU.mult, reads=[ptn, "mask"], writes=[ptn])
                for tile in range(2):
                    S.op("pe", "matmul", pO[2 * ob + m][:, tile, 0:129], lhsT=pt[:, 128 * tile:128 * tile + 128],
                         rhs=vs[hi][:, kt, 0:129], start=(kt == 0 and tile == 0), stop=(kt == nkt - 1 and tile == 1),
                         reads=[ptn, "vs%d_%d" % (hi, kt // 16), "vone%d" % hi], writes=["pO%d" % (2 * ob + m)])
            if kt != nkt - 1:
                return
            for tile in range(2):
                fi = fcnt % 2
                fcnt += 1
                f = fs[fi]
                fn = "fs%d" % fi
                a1 = pO[2 * ob + 0]
                a2 = pO[2 * ob + 1]
                S.op("dve", "reciprocal", out=f[:, 0:1], in_=a1[:, tile, 128:129], reads=["pO%d" % (2 * ob)], writes=[fn])
                S.op("dve", "reciprocal", out=f[:, 1:2], in_=a2[:, tile, 128:129], reads=["pO%d" % (2 * ob + 1)], writes=[fn])
                S.op("dve", "tensor_tensor", out=f[:, 1:2], in0=f[:, 1:2], in1=sm[:, 0:1], op=ALU.mult,
                     reads=[fn, "sm0"], writes=[fn])
                S.op("dve", "tensor_scalar", out=t1[fi][:], in0=a1[:, tile, 0:128], scalar1=f[:, 0:1], scalar2=None,
                     op0=ALU.mult, reads=["pO%d" % (2 * ob), fn], writes=["t1_%d" % fi])
                S.op("dve", "scalar_tensor_tensor", out=t2[fi][:], in0=a2[:, tile, 0:128], scalar=f[:, 1:2],
                     in1=t1[fi][:], op0=ALU.mult, op1=ALU.add,
                     reads=["pO%d" % (2 * ob + 1), fn, "t1_%d" % fi], writes=["t2_%d" % fi])
                S.op("act", "activation", out=junk[:], in_=t2[fi][:], func=AF.Square, accum_out=f[:, 2:3],
                     reads=["t2_%d" % fi], writes=["junk", fn])
                S.op("act", "activation", out=f[:, 3:4], in_=f[:, 2:3], func=AF.Sqrt, bias=epsb[:], scale=1.0 / 128,
                     reads=[fn, "epsb"], writes=[fn])
                S.op("dve", "reciprocal", out=f[:, 3:4], in_=f[:, 3:4], reads=[fn], writes=[fn])
                S.op("dve", "scalar_tensor_tensor", out=ys[:, 2 * g + tile, 128 * h:128 * h + 128], in0=t2[fi][:],
                     scalar=f[:, 3:4], in1=gcb[:, 128 * h:128 * h + 128], op0=ALU.mult, op1=ALU.mult,
                     reads=["t2_%d" % fi, fn, "gcb"], writes=["ys"])

        for idx in range(len(units) + 1):
            if idx < len(units):
                emit_qk(units[idx], (ucnt + idx) % 2)
            if idx >= 1:
                emit_rest(units[idx - 1], (ucnt + idx - 1) % 2)
        ucnt += len(units)
    S.dma("sp", out=y.rearrange("(j p) e -> p j e", p=128), in_=ys[:], reads=["ys"], writes=["yout"])
    alltoks = [(k, v_[1]) for k, v_ in S.dma_sems.items() if v_[1] > 0]
    S.wait_all("sp", alltoks)
    S.emit()
    S.close()
    for c in reversed(ctxs):
        c.__exit__(None, None, None)
    return nc


F32 = mybir.dt.float32
BF16 = mybir.dt.bfloat16
AF = mybir.ActivationFunctionType
ALU = mybir.AluOpType
AX = mybir.AxisListType
SEQ = 8192
NCH = 64
EPS = 1e-5
LNS = math.log(128 ** -0.5)


def build_pb(NJ=NCH):
    nc = bass.Bass("TRN2", target_bir_lowering=False)
    qT_d = nc.dram_tensor("qT", [128, SEQ], F32, kind="ExternalInput").ap()
    kT_d = nc.dram_tensor("kT", [128, SEQ], F32, kind="ExternalInput").ap()
    v_d = nc.dram_tensor("v", [SEQ, 128], BF16, kind="ExternalInput").ap()
    o_d = nc.dram_tensor("o", [SEQ, 128], F32, kind="ExternalInput").ap()
    if_d = nc.dram_tensor("ifg", [SEQ, 2], F32, kind="ExternalInput").ap()
    cw_d = nc.dram_tensor("cw", [128, 8], F32, kind="ExternalInput").ap()
    bif_d = nc.dram_tensor("bif", [2], F32, kind="ExternalInput").ap()
    gn_d = nc.dram_tensor("gn", [128], F32, kind="ExternalInput").ap()
    tri_d = nc.dram_tensor("tri", [128, 128], F32, kind="ExternalInput").ap()
    ident_d = nc.dram_tensor("ident", [128, 128], BF16, kind="ExternalInput").ap()
    y_d = nc.dram_tensor("y", [SEQ, 128], BF16, kind="ExternalOutput").ap()

    S = Sched(nc)
    S.open()
    ctxs = []

    def sb(name, shape, dt):
        c = nc.sbuf_tensor(name, shape, dt)
        t = c.__enter__()
        ctxs.append(c)
        return t

    def ps(name, shape, dt):
        c = nc.psum_tensor(name, shape, dt)
        t = c.__enter__()
        ctxs.append(c)
        return t

    ident = sb("ident_s", [128, 128], BF16)
    tri = sb("tri_s", [128, 128], F32)
    trib = sb("trib", [128, 128], BF16)
    ones = sb("ones", [128, 128], F32)
    cw = sb("cw_s", [128, 8], F32)
    bif = sb("bif_s", [128, 2], F32)
    gnb = sb("gnb", [128, 128], F32)
    epsb = sb("epsb", [128, 1], F32)
    gi = sb("gi", [128, NCH, 2], F32)
    lf = sb("lf", [128, NCH], F32)
    li = sb("li", [128, NCH], F32)
    a_s = sb("a_s", [128, NCH], F32)
    g_s = sb("g_s", [128, NCH], F32)
    u_s = sb("u_s", [128, NCH], F32)
    wi_s = sb("wi_s", [128, NCH], F32)
    ws_s = sb("ws_s", [128, NCH], F32)
    eg = sb("eg", [128, NCH], F32)
    ea = sb("ea", [128, NCH], F32)
    xr = [sb("xr%d" % i, [128, 2048 + 4], F32) for i in range(2)]
    ac = [sb("ac%d" % i, [128, 2048], F32) for i in range(2)]
    QT = sb("QT", [128, SEQ], BF16)
    KT = sb("KT", [128, SEQ], BF16)
    Kt = sb("Kt", [128, NCH, 128], BF16)
    vs = sb("vs", [128, NCH, 132], BF16)
    vi = sb("vi", [128, NCH, 132], BF16)
    vst = sb("vst", [128, NCH, 132], BF16)
    og = sb("og", [128, NCH, 128], BF16)
    of = [sb("of%d" % i, [128, 128], F32) for i in range(2)]
    ys = sb("ys", [128, NCH, 128], BF16)
    C = sb("C", [128, 132], F32)
    Cb = [sb("Cb%d" % i, [128, 132], BF16) for i in range(2)]
    qk = [sb("qk%d" % i, [128, 128], BF16) for i in range(2)]
    hh = [sb("hh%d" % i, [128, 128], F32) for i in range(2)]
    junk = sb("junk", [128, 128], F32)
    fs = [sb("fs%d" % i, [128, 4], F32) for i in range(2)]
    pS = [ps("pS%d" % i, [128, 128], F32) for i in range(2)]
    pX = [ps("pX%d" % i, [128, 132], F32) for i in range(2)]
    pC = [ps("pC%d" % i, [128, 132], F32) for i in range(2)]
    pT = ps("pT", [128, 512], BF16)
    pG = ps("pG", [128, 2, NCH], F32)

    S.dma("sp", out=ident[:], in_=ident_d[:, :], writes=["ident"])
    S.dma("sp", out=tri[:], in_=tri_d[:, :], writes=["tri"])
    S.dma("sp", out=cw[:], in_=cw_d[:, :], writes=["cw"])
    S.dma("sp", out=bif[:], in_=bif_d.partition_broadcast(128), writes=["bif"])
    S.dma("sp", out=gnb[:], in_=gn_d.partition_broadcast(128), writes=["gnb"])
    S.dma("sp", out=gi[:], in_=if_d.rearrange("(j p) c -> p j c", p=128), writes=["gi"])
    S.dma("sp", out=vs[:, :, 0:128], in_=v_d.rearrange("(j p) e -> p j e", p=128), writes=["vs"])
    S.op("pool", "memset", epsb[:], EPS, writes=["epsb"])
    S.op("pool", "memset", ones[:], 1.0, writes=["ones"])
    S.op("pool", "memset", vs[:, :, 128:129], 1.0, reads=[], writes=["vs1"])
    S.op("pool", "memset", C[:], 0.0, writes=["C"])
    S.op("pool", "tensor_copy", out=trib[:], in_=tri[:], reads=["tri"], writes=["trib"])
    S.op("dve", "tensor_scalar", out=li[:], in0=gi[:, :, 0], scalar1=bif[:, 0:1], scalar2=None, op0=ALU.add,
         reads=["gi", "bif"], writes=["li"])
    S.op("dve", "tensor_scalar", out=lf[:], in0=gi[:, :, 1], scalar1=bif[:, 1:2], scalar2=None, op0=ALU.add,
         reads=["gi", "bif"], writes=["lf"])
    S.op("act", "activation", out=lf[:], in_=lf[:], func=AF.Exp, scale=-1.0, reads=["lf"], writes=["lf"])
    S.op("act", "activation", out=lf[:], in_=lf[:], func=AF.Ln, bias=1.0, scale=1.0, reads=["lf"], writes=["lf"])
    S.op("dve", "tensor_scalar", out=lf[:], in0=lf[:], scalar1=-1.0, scalar2=None, op0=ALU.mult,
         reads=["lf"], writes=["lf"])
    S.op("pe", "matmul", pG[:, 0, :], lhsT=tri[:], rhs=lf[:], start=True, stop=False, reads=["tri", "lf"], writes=["pG"])
    S.op("pe", "matmul", pG[:, 1, :], lhsT=ones[:], rhs=lf[:], start=False, stop=True, reads=["ones", "lf"], writes=["pG"])
    S.op("dve", "tensor_copy", out=a_s[:], in_=pG[:, 0, :], reads=["pG"], writes=["a_s"])
    S.op("dve", "tensor_copy", out=g_s[:], in_=pG[:, 1, :], reads=["pG"], writes=["g_s"])
    S.op("dve", "tensor_tensor", out=u_s[:], in0=li[:], in1=a_s[:], op=ALU.subtract, reads=["li", "a_s"], writes=["u_s"])
    S.op("act", "activation", out=wi_s[:], in_=u_s[:], func=AF.Exp, bias=LNS, scale=1.0, reads=["u_s"], writes=["wi_s"])
    S.op("dve", "tensor_tensor", out=u_s[:], in0=u_s[:], in1=g_s[:], op=ALU.add, reads=["u_s", "g_s", "wi_s"], writes=["u_s"])
    S.op("act", "activation", out=ws_s[:], in_=u_s[:], func=AF.Exp, bias=LNS, scale=1.0, reads=["u_s"], writes=["ws_s"])
    S.op("act", "activation", out=eg[:], in_=g_s[:], func=AF.Exp, reads=["g_s"], writes=["eg"])
    S.op("act", "activation", out=ea[:], in_=a_s[:], func=AF.Exp, scale=-1.0, reads=["a_s"], writes=["ea"])
    S.op("dve", "tensor_tensor", out=vi[:, :, 0:129], in0=vs[:, :, 0:129],
         in1=wi_s[:].unsqueeze(2).to_broadcast([128, NCH, 129]), op=ALU.mult,
         reads=["vs", "vs1", "wi_s"], writes=["vi"])
    S.op("pool", "tensor_tensor", out=vst[:, :, 0:129], in0=vs[:, :, 0:129],
         in1=ws_s[:].unsqueeze(2).to_broadcast([128, NCH, 129]), op=ALU.mult,
         reads=["vs", "vs1", "ws_s"], writes=["vst"])
    for which, (src, dst, dname) in enumerate(((qT_d, QT, "QT"), (kT_d, KT, "KT"))):
        for p in range(4):
            i = (which * 4 + p) % 2
            if p == 0:
                S.op("pool", "memset", xr[i][:, 0:4], 0.0, writes=["xr%d" % i])
                S.dma("sp", out=xr[i][:, 4:2052], in_=src[:, 0:2048], writes=["xr%d" % i])
            else:
                S.dma("sp", out=xr[i][:, 1:2052], in_=src[:, 2048 * p - 3:2048 * p + 2048], writes=["xr%d" % i])
            S.op("dve", "tensor_scalar", out=ac[i][:], in0=xr[i][:, 1:2049], scalar1=cw[:, 4 * which:4 * which + 1],
                 scalar2=None, op0=ALU.mult, reads=["xr%d" % i, "cw"], writes=["ac%d" % i])
            for w in range(1, 4):
                S.op("dve", "scalar_tensor_tensor", out=ac[i][:], in0=xr[i][:, 1 + w:2049 + w],
                     scalar=cw[:, 4 * which + w:4 * which + w + 1], in1=ac[i][:], op0=ALU.mult, op1=ALU.add,
                     reads=["xr%d" % i, "cw", "ac%d" % i], writes=["ac%d" % i])
            S.op("act", "activation", out=dst[:, 2048 * p:2048 * p + 2048], in_=ac[i][:], func=AF.Silu,
                 reads=["ac%d" % i], writes=[dname + "%d" % p])
    for j in range(NJ):
        i = j % 2
        S.dma("sp", out=of[i][:], in_=o_d[128 * j:128 * j + 128, :], writes=["of%d" % i])
        S.op("act", "activation", out=of[i][:], in_=of[i][:], func=AF.Sigmoid, reads=["of%d" % i], writes=["of%d" % i])
        S.op("pool", "tensor_tensor", out=og[:, j, :], in0=of[i][:], in1=gnb[:], op=ALU.mult,
             reads=["of%d" % i, "gnb"], writes=["og%d" % j])
    for j4 in range(NJ // 4):
        for k in range(4):
            j = 4 * j4 + k
            S.op("pe", "transpose", out=pT[:, 128 * k:128 * k + 128], in_=KT[:, 128 * j:128 * j + 128], identity=ident[:],
                 reads=["KT%d" % (j // 16), "ident"], writes=["pT"])
        S.op("dve", "tensor_copy", out=Kt[:, 4 * j4:4 * j4 + 4, :], in_=pT[:, 0:512].rearrange("p (k t) -> p k t", k=4),
             reads=["pT"], writes=["Kt%d" % j4])

    for j in range(NJ):
        i = j % 2
        qn = "QT%d" % (j // 16)
        kn = "KT%d" % (j // 16)
        S.op("pe", "matmul", pS[i][:], lhsT=KT[:, 128 * j:128 * j + 128], rhs=QT[:, 128 * j:128 * j + 128],
             start=True, stop=True, reads=[qn, kn], writes=["pS%d" % i])
        S.op("dve", "tensor_tensor", out=qk[i][:], in0=pS[i][:], in1=trib[:], op=ALU.mult,
             reads=["pS%d" % i, "trib"], writes=["qk%d" % i])
        S.op("pe", "matmul", pX[i][:, 0:129], lhsT=qk[i][:], rhs=vi[:, j, 0:129], start=True, stop=(j == 0),
             reads=["qk%d" % i, "vi"], writes=["pX%d" % i])
        if j > 0:
            S.op("pe", "matmul", pX[i][:, 0:129], lhsT=QT[:, 128 * j:128 * j + 128], rhs=Cb[i][:, 0:129],
                 start=False, stop=True, reads=[qn, "Cb%d" % i], writes=["pX%d" % i])
        if j < NJ - 1:
            S.op("pe", "matmul", pC[i][:, 0:129], lhsT=Kt[:, j, :], rhs=vst[:, j, 0:129], start=True, stop=True,
                 reads=["Kt%d" % (j // 4), "vst"], writes=["pC%d" % i])
            S.op("dve", "scalar_tensor_tensor", out=C[:, 0:129], in0=C[:, 0:129], scalar=eg[:, j:j + 1],
                 in1=pC[i][:, 0:129], op0=ALU.mult, op1=ALU.add, reads=["C", "eg", "pC%d" % i], writes=["C"])
            S.op("pool", "tensor_copy", out=Cb[1 - i][:, 0:129], in_=C[:, 0:129], reads=["C"], writes=["Cb%d" % (1 - i)])
        f = fs[i]
        fn = "fs%d" % i
        S.op("dve", "tensor_scalar", out=f[:, 3:4], in0=pX[i][:, 128:129], scalar1=-1.0, scalar2=ea[:, j:j + 1],
             op0=ALU.mult, op1=ALU.max, reads=["pX%d" % i, "ea"], writes=[fn])
        S.op("dve", "tensor_tensor", out=f[:, 0:1], in0=pX[i][:, 128:129], in1=f[:, 3:4], op=ALU.max,
             reads=["pX%d" % i, fn], writes=[fn])
        S.op("dve", "reciprocal", out=f[:, 0:1], in_=f[:, 0:1], reads=[fn], writes=[fn])
        S.op("dve", "tensor_scalar", out=hh[i][:], in0=pX[i][:, 0:128], scalar1=f[:, 0:1], scalar2=None, op0=ALU.mult,
             reads=["pX%d" % i, fn], writes=["hh%d" % i])
        S.op("dve", "scalar_tensor_tensor", out=junk[:], in0=hh[i][:], scalar=1.0, in1=hh[i][:], op0=ALU.mult,
             op1=ALU.mult, accum_out=f[:, 1:2], reads=["hh%d" % i], writes=["junk", fn])
        S.op("act", "activation", out=f[:, 2:3], in_=f[:, 1:2], func=AF.Sqrt, bias=epsb[:], scale=1.0 / 128,
             reads=[fn, "epsb"], writes=[fn])
        S.op("dve", "reciprocal", out=f[:, 2:3], in_=f[:, 2:3], reads=[fn], writes=[fn])
        S.op("dve", "scalar_tensor_tensor", out=ys[:, j, :], in0=hh[i][:], scalar=f[:, 2:3], in1=og[:, j, :],
             op0=ALU.mult, op1=ALU.mult, reads=["hh%d" % i, fn, "og%d" % j], writes=["ys"])
    if NJ < NCH:
        S.op("pool", "memset", ys[:, NJ:NCH, :], 0.0, reads=[], writes=["ys"])
    S.dma("sp", out=y_d.rearrange("(j p) e -> p j e", p=128), in_=ys[:], reads=["ys"], writes=["yout"])
    alltoks = [(k, v_[1]) for k, v_ in S.dma_sems.items() if v_[1] > 0]
    S.wait_all("sp", alltoks)
    S.emit()
    S.close()
    for c in reversed(ctxs):
        c.__exit__(None, None, None)
    return nc


F32 = mybir.dt.float32
BF16 = mybir.dt.bfloat16
AF = mybir.ActivationFunctionType
ALU = mybir.AluOpType
AX = mybir.AxisListType
NT = 1024
SEQ = 8192
HA = 6
HI = 8
KSEL = 256
NIT = 18
SCALE = 128 ** -0.5
NEG = -1.0e30


def core_blocks(c):
    out = []
    for g in range(4):
        out += [16 * g + c, 16 * g + 15 - c]
    return out


def a_tables(c):
    slopes = 2.0 ** (-8.0 * np.arange(1, HA + 1) / HA)
    ar = np.arange(128, dtype=np.float32)
    bias = np.zeros((128, HA, 4, 64), np.float32)
    rbc = np.zeros((128, 4), np.float32)
    for g in range(4):
        rb = 16 * g + 15 - c
        R = 128 * rb + 127
        rbc[:, g] = 128.0 * (rb + 1)
        for h in range(HA):
            for kt in range(64):
                bias[:, h, g, kt] = slopes[h] * (128 * kt + ar - R)
    negb = np.full((128, 16, 2, 128), NEG, np.float32)
    tri = np.where(ar[None, :] <= ar[:, None], 0.0, NEG)
    for tile in range(2):
        qslot = c if tile == 0 else 15 - c
        for i in range(16):
            if i < qslot:
                negb[:, i, tile, :] = 0.0
            elif i == qslot:
                negb[:, i, tile, :] = tri
    sl = np.zeros((1, HA, 128), np.float32)
    for h in range(HA):
        sl[0, h, :] = slopes[h] / SCALE
    ktp1 = np.tile(np.arange(1, 65, dtype=np.float32)[None, :], (128, 1))
    return dict(bias=bias.reshape(128, -1), rbc=rbc, negb=negb.reshape(128, -1), sl=sl.reshape(1, -1), ktp1=ktp1)


def build_pa(GR=4, HR=HA):
    nc = bass.Bass("TRN2", target_bir_lowering=False)
    qT_d = nc.dram_tensor("qT", [768, NT], BF16, kind="ExternalInput").ap()
    kT_d = nc.dram_tensor("kT", [768, SEQ], BF16, kind="ExternalInput").ap()
    v_d = nc.dram_tensor("v", [SEQ, 768], BF16, kind="ExternalInput").ap()
    qiT_d = nc.dram_tensor("qiT", [512, NT], BF16, kind="ExternalInput").ap()
    kiT_d = nc.dram_tensor("kiT", [64, SEQ], BF16, kind="ExternalInput").ap()
    wi_d = nc.dram_tensor("wi", [NT, 8], F32, kind="ExternalInput").ap()
    bias_d = nc.dram_tensor("bias", [128, HA * 4 * 64], F32, kind="ExternalInput").ap()
    rbc_d = nc.dram_tensor("rbc", [128, 4], F32, kind="ExternalInput").ap()
    negb_d = nc.dram_tensor("negb", [128, 16 * 2 * 128], BF16, kind="ExternalInput").ap()
    sl_d = nc.dram_tensor("sl", [1, HA * 128], BF16, kind="ExternalInput").ap()
    ktp1_d = nc.dram_tensor("ktp1", [128, 64], F32, kind="ExternalInput").ap()
    ident_d = nc.dram_tensor("ident", [128, 128], BF16, kind="ExternalInput").ap()
    identf_d = nc.dram_tensor("identf", [128, 128], F32, kind="ExternalInput").ap()
    y_d = nc.dram_tensor("y", [NT, 768], BF16, kind="ExternalOutput").ap()

    S = Sched(nc)
    S.open()
    ctxs = []

    def sb(name, shape, dt):
        c = nc.sbuf_tensor(name, shape, dt)
        t = c.__enter__()
        ctxs.append(c)
        return t

    def ps(name, shape, dt):
        c = nc.psum_tensor(name, shape, dt)
        t = c.__enter__()
        ctxs.append(c)
        return t

    ident = sb("ident_s", [128, 128], BF16)
    identf = sb("identf_s", [128, 128], F32)
    qsg = [sb("qsg%d" % i, [128, HA, 256], BF16) for i in range(2)]
    ks = sb("ks", [128, SEQ], BF16)
    vs2 = [sb("vs%d" % i, [128, 64, 132], BF16) for i in range(2)]
    kis = sb("kis", [64, SEQ], BF16)
    qisg = [sb("qisg%d" % i, [64, HI, 256], BF16) for i in range(2)]
    wi = sb("wi_s", [128, 8, 8], F32)
    wa = sb("wa", [128, 8, 8], F32)
    wsg = sb("wsg", [128, 8, 8], F32)
    bias = sb("bias_s", [128, HA * 4 * 64], F32)
    rbc = sb("rbc_s", [128, 4], F32)
    negb = sb("negb_s", [128, 16, 2, 128], BF16)
    sl = sb("sl_s", [1, HA, 128], BF16)
    ktp1 = sb("ktp1_s", [128, 64], F32)
    I = sb("I", [128, SEQ], F32)
    nsq2 = [sb("nsq%d" % i, [128, SEQ], BF16) for i in range(2)]
    nsT = sb("nsT", [128, 64, 256], BF16)
    rl = [sb("rl%d" % i, [128, 512], F32) for i in range(2)]
    bs = sb("bs", [128, 8 + NIT + 4], F32)
    an = sb("an", [128, 64], F32)
    dcol = sb("dcol", [128, 2, 2], F32)
    drow = sb("drow", [1, 256], BF16)
    pT = [sb("pT%d" % i, [128, 256], BF16) for i in range(3)]
    ysg = [sb("ysg%d" % i, [128, 2, 768], BF16) for i in range(2)]
    fs = [sb("fs%d" % i, [128, 2], F32) for i in range(2)]
    pI = [ps("pI%d" % i, [128, 512], F32) for i in range(2)]
    pTr = ps("pTr", [128, 512], BF16)
    pS = [ps("pS%d" % i, [128, 256], F32) for i in range(2)]
    pO = [ps("pO%d" % i, [128, 2, 132], F32) for i in range(2)]
    pD = ps("pD", [128, 128], F32)

    S.dma("sp", out=ident[:], in_=ident_d[:, :], writes=["ident"])
    S.dma("sp", out=identf[:], in_=identf_d[:, :], writes=["identf"])
    S.dma("sp", out=kis[:], in_=kiT_d[:, :], writes=["kis"])
    S.dma("sp", out=wi[:], in_=wi_d.rearrange("(j p) h -> p j h", p=128), writes=["wi"])
    S.dma("sp", out=bias[:], in_=bias_d[:, :], writes=["bias"])
    S.dma("sp", out=rbc[:], in_=rbc_d[:, :], writes=["rbc"])
    S.dma("sp", out=negb[:].rearrange("p a b c -> p (a b c)"), in_=negb_d[:, :], writes=["negb"])
    S.dma("sp", out=sl[:].rearrange("p a b -> p (a b)"), in_=sl_d[:, :], writes=["sl"])
    S.dma("sp", out=ktp1[:], in_=ktp1_d[:, :], writes=["ktp1"])
    for i in range(2):
        S.op("pool", "memset", vs2[i][:, :, 128:129], 1.0, writes=["vs1_%d" % i])
        S.op("pool", "memset", ysg[i][:], 0.0, writes=["ysg%d" % i])
    S.op("dve", "tensor_scalar", out=wa[:], in0=wi[:], scalar1=-1.0, scalar2=None, op0=ALU.mult, reads=["wi"], writes=["wa"])
    S.op("dve", "tensor_tensor", out=wa[:], in0=wa[:], in1=wi[:], op=ALU.max, reads=["wa", "wi"], writes=["wa"])
    S.op("act", "activation", out=wsg[:], in_=wi[:], func=AF.Sign, reads=["wi"], writes=["wsg"])

    ucnt = 0
    icnt = 0
    ocnt = 0
    def stage_a(g):
        nkt = 16 * g + 16
        NK = 128 * nkt
        nonlocal icnt
        S.dma("sp", out=qisg[g % 2][:], in_=qiT_d.rearrange("(h p) t -> p h t", p=64)[:, :, 256 * g:256 * g + 256],
              writes=["qis%d" % (g % 2)])
        S.dma("sp", out=qsg[g % 2][:], in_=qT_d.rearrange("(h p) t -> p h t", p=128)[:, :, 256 * g:256 * g + 256],
              writes=["qs%d" % (g % 2)])
        for tile in range(2):
            j = 2 * g + tile
            for kc in range(NK // 512):
                for h in range(HI):
                    pi = icnt % 2
                    icnt += 1
                    S.op("pe", "matmul", pI[pi][:], lhsT=qisg[g % 2][:, h, 128 * tile:128 * tile + 128], rhs=kis[:, 512 * kc:512 * kc + 512],
                         start=True, stop=True, reads=["qis%d" % (g % 2), "kis"], writes=["pI%d" % pi])
                    S.op("act", "activation", out=rl[pi][:], in_=pI[pi][:], func=AF.Relu, scale=wa[:, j, h:h + 1],
                         reads=["pI%d" % pi, "wa"], writes=["rl%d" % pi])
                    dst = I[:, 512 * kc:512 * kc + 512]
                    if h == 0:
                        S.op("dve", "tensor_scalar", out=dst, in0=rl[pi][:], scalar1=wsg[:, j, 0:1], scalar2=None,
                             op0=ALU.mult, reads=["rl%d" % pi, "wsg"], writes=["I%d" % kc])
                    else:
                        S.op("dve", "scalar_tensor_tensor", out=dst, in0=rl[pi][:], scalar=wsg[:, j, h:h + 1], in1=dst,
                             op0=ALU.mult, op1=ALU.add, reads=["rl%d" % pi, "wsg", "I%d" % kc], writes=["I%d" % kc])
            Iall = ["I%d" % kc for kc in range(NK // 512)]
            S.op("dve", "tensor_reduce", out=bs[:, 0:1], in_=I[:, 0:NK], axis=AX.X, op=ALU.min, reads=Iall, writes=["bs"])
            S.op("dve", "tensor_tensor", out=I[:, 2048 * g:NK].rearrange("p (a b) -> p a b", a=16),
                 in0=I[:, 2048 * g:NK].rearrange("p (a b) -> p a b", a=16), in1=negb[:, :, tile, :], op=ALU.add,
                 reads=Iall + ["negb"], writes=Iall)
            S.op("dve", "tensor_reduce", out=bs[:, 1:2], in_=I[:, 0:NK], axis=AX.X, op=ALU.max, reads=Iall, writes=["bs"])
            S.op("dve", "tensor_scalar", out=bs[:, 0:1], in0=bs[:, 0:1], scalar1=-1.0, scalar2=None, op0=ALU.add,
                 reads=["bs"], writes=["bs"])
            S.op("dve", "scalar_tensor_tensor", out=bs[:, 8:9], in0=bs[:, 1:2], scalar=1.0, in1=bs[:, 0:1],
                 op0=ALU.add, op1=ALU.subtract, reads=["bs"], writes=["bs"])
            for t in range(1, NIT + 1):
                S.op("dve", "tensor_scalar", out=bs[:, 8 + t:9 + t], in0=bs[:, 7 + t:8 + t], scalar1=0.5, scalar2=None,
                     op0=ALU.mult, reads=["bs"], writes=["bs"])
            for t in range(1, NIT + 1):
                S.op("dve", "tensor_tensor", out=bs[:, 2:3], in0=bs[:, 0:1], in1=bs[:, 8 + t:9 + t], op=ALU.add,
                     reads=["bs"], writes=["bs"])
                S.op("dve", "tensor_scalar", out=nsq2[tile][:, 0:NK], in0=I[:, 0:NK], scalar1=bs[:, 2:3], scalar2=0.0,
                     op0=ALU.is_ge, op1=ALU.add, accum_out=bs[:, 3:4], reads=Iall + ["bs"], writes=["nsq%d" % tile, "bs"])
                S.op("dve", "tensor_scalar", out=bs[:, 4:5], in0=bs[:, 3:4], scalar1=KSEL - 0.5, scalar2=bs[:, 8 + t:9 + t],
                     op0=ALU.is_ge, op1=ALU.mult, reads=["bs"], writes=["bs"])
                S.op("dve", "tensor_tensor", out=bs[:, 0:1], in0=bs[:, 0:1], in1=bs[:, 4:5], op=ALU.add,
                     reads=["bs"], writes=["bs"])
            S.op("dve", "tensor_scalar", out=nsq2[tile][:, 0:NK], in0=I[:, 0:NK], scalar1=bs[:, 0:1], scalar2=NEG,
                 op0=ALU.is_lt, op1=ALU.mult, reads=Iall + ["bs"], writes=["nsq%d" % tile])
            S.op("dve", "tensor_reduce", out=an[:, 0:nkt], in_=nsq2[tile][:, 0:NK].rearrange("p (a b) -> p a b", a=nkt),
                 axis=AX.X, op=ALU.max, reads=["nsq%d" % tile], writes=["an"])
            S.op("dve", "scalar_tensor_tensor", out=an[:, 0:nkt], in0=an[:, 0:nkt], scalar=-1.0, in1=ktp1[:, 0:nkt],
                 op0=ALU.is_ge, op1=ALU.mult, reads=["an", "ktp1"], writes=["an"])
            S.op("dve", "tensor_reduce", out=dcol[:, tile, 0:1], in_=an[:, 0:nkt], axis=AX.X, op=ALU.max, reads=["an"], writes=["dcol%d" % tile])
            S.op("dve", "tensor_scalar", out=dcol[:, tile, 1:2], in0=dcol[:, tile, 0:1], scalar1=-128.0, scalar2=rbc[:, g:g + 1],
                 op0=ALU.mult, op1=ALU.add, reads=["dcol%d" % tile, "rbc"], writes=["dcol%d" % tile])

    def stage_b(g):
        nkt = 16 * g + 16
        NK = 128 * nkt
        for tile in range(2):
            S.op("pe", "transpose", out=pD[0:1, 0:128], in_=dcol[:, tile, 1:2], identity=identf[:],
                 reads=["dcol%d" % tile, "identf"], writes=["pD"])
            S.op("act", "copy", out=drow[0:1, 128 * tile:128 * tile + 128], in_=pD[0:1, 0:128], reads=["pD"], writes=["drow"])
            for k4 in range(nkt // 4):
                for k in range(4):
                    kt = 4 * k4 + k
                    S.op("pe", "transpose", out=pTr[:, 128 * k:128 * k + 128], in_=nsq2[tile][:, 128 * kt:128 * kt + 128],
                         identity=ident[:], reads=["nsq%d" % tile, "ident"], writes=["pTr"])
                S.op("act", "copy", out=nsT[:, 4 * k4:4 * k4 + 4, 128 * tile:128 * tile + 128],
                     in_=pTr[:, 0:512].rearrange("p (k t) -> p k t", k=4), reads=["pTr"], writes=["nsT"])

    def attention(g):
        nkt = 16 * g + 16
        NK = 128 * nkt
        nonlocal ucnt, ocnt
        for h in range(HR):
            vi = (g * HR + h) % 2
            S.dma("sp", out=ks[:, 0:NK], in_=kT_d[128 * h:128 * h + 128, 0:NK], writes=["ks"])
            for q4 in range(g + 1):
                S.dma("sp", out=vs2[vi][:, 16 * q4:16 * q4 + 16, 0:128],
                      in_=v_d[2048 * q4:2048 * q4 + 2048, 128 * h:128 * h + 128].rearrange("(kt p) e -> p kt e", p=128),
                      writes=["vs%d_%d" % (vi, q4)])
            ob = ocnt % 2
            ocnt += 1
            def emit_qk(kt, si):
                S.op("pe", "matmul", pS[si][:], lhsT=ks[:, 128 * kt:128 * kt + 128], rhs=qsg[g % 2][:, h, :],
                     start=True, stop=False, reads=["ks", "qs%d" % (g % 2)], writes=["pS%d" % si])
                S.op("pe", "matmul", pS[si][:], lhsT=sl[0:1, h, :], rhs=drow[0:1, :], start=False, stop=False,
                     reads=["sl", "drow"], writes=["pS%d" % si])
                S.op("pe", "matmul", pS[si][:], lhsT=ident[:], rhs=nsT[:, kt, :], start=False, stop=True,
                     reads=["ident", "nsT"], writes=["pS%d" % si])

            def emit_rest(kt, si, pti):
                bcol = (h * 4 + g) * 64 + kt
                S.op("act", "activation", out=pT[pti][:], in_=pS[si][:], func=AF.Exp, bias=bias[:, bcol:bcol + 1],
                     scale=SCALE, reads=["pS%d" % si, "bias"], writes=["pT%d" % pti])
                for tile in range(2):
                    S.op("pe", "matmul", pO[ob][:, tile, 0:129], lhsT=pT[pti][:, 128 * tile:128 * tile + 128],
                         rhs=vs2[vi][:, kt, 0:129], start=(kt == 0 and tile == 0), stop=(kt == nkt - 1 and tile == 1),
                         reads=["pT%d" % pti, "vs%d_%d" % (vi, kt // 16), "vs1_%d" % vi], writes=["pO%d" % ob])

            for idx in range(nkt + 1):
                if idx < nkt:
                    emit_qk(idx, (ucnt + idx) % 2)
                if idx >= 1:
                    emit_rest(idx - 1, (ucnt + idx - 1) % 2, (ucnt + idx - 1) % 3)
            ucnt += nkt
            for tile in range(2):
                f = fs[tile]
                S.op("dve", "reciprocal", out=f[:, 0:1], in_=pO[ob][:, tile, 128:129], reads=["pO%d" % ob], writes=["fs%d" % tile])
                S.op("dve", "tensor_scalar", out=ysg[g % 2][:, tile, 128 * h:128 * h + 128], in0=pO[ob][:, tile, 0:128],
                     scalar1=f[:, 0:1], scalar2=None, op0=ALU.mult, reads=["pO%d" % ob, "fs%d" % tile], writes=["ysg%d" % (g % 2)])
        S.dma("sp", out=y_d[256 * g:256 * g + 256, :].rearrange("(j p) e -> p j e", p=128), in_=ysg[g % 2][:],
              reads=["ysg%d" % (g % 2)], writes=["yout"])

    stage_a(0)
    stage_b(0)
    for g in range(GR):
        if g + 1 < GR:
            stage_a(g + 1)
        attention(g)
        if g + 1 < GR:
            stage_b(g + 1)
    alltoks = [(k, v_[1]) for k, v_ in S.dma_sems.items() if v_[1] > 0]
    S.wait_all("sp", alltoks)
    S.emit()
    S.close()
    for c in reversed(ctxs):
        c.__exit__(None, None, None)
    return nc


F32 = mybir.dt.float32
BF16 = mybir.dt.bfloat16
AF = mybir.ActivationFunctionType
ALU = mybir.AluOpType
ALPHA = 8.0 ** 0.25
EPS = 1e-5

NT = 1024
D = 2048
FF = 8192
G = 512


def build_p3():
    nc = bass.Bass("TRN2", target_bir_lowering=False)
    xres = nc.dram_tensor("xres", [NT, D], F32, kind="ExternalInput").ap()
    mixT = nc.dram_tensor("mixT", [D, NT], BF16, kind="ExternalInput").ap()
    w_out = nc.dram_tensor("w_out", [D, D], F32, kind="ExternalInput").ap()
    w_up = nc.dram_tensor("w_up", [D, FF], F32, kind="ExternalInput").ap()
    w_down = nc.dram_tensor("w_down", [FF, D], F32, kind="ExternalInput").ap()
    lnp = nc.dram_tensor("lnp", [4, D], F32, kind="ExternalInput").ap()
    ident_d = nc.dram_tensor("identf", [128, 128], F32, kind="ExternalInput").ap()
    x2 = nc.dram_tensor("x2", [NT, D], F32, kind="ExternalOutput").ap()

    S = Sched(nc)
    S.open()
    ctxs = []

    def sb(name, shape, dt):
        c = nc.sbuf_tensor(name, shape, dt)
        t = c.__enter__()
        ctxs.append(c)
        return t

    def ps(name, shape, dt):
        c = nc.psum_tensor(name, shape, dt)
        t = c.__enter__()
        ctxs.append(c)
        return t

    ident = sb("ident_s", [128, 128], F32)
    lnb = sb("lnb", [128, 4, D], F32)
    big = sb("big", [128, 64 * G], BF16)
    wo = big[:].rearrange("p (c d) -> p c d", c=16)
    hT = big[:].rearrange("p (f t) -> p f t", f=64)
    mx = [sb("mx%d" % i, [128, 16, 128], BF16) for i in range(2)]
    xr = [sb("xr%d" % i, [128, D], F32) for i in range(1)]
    r = [sb("r%d" % i, [128, D], F32) for i in range(2)]
    x1 = sb("x1", [128, 4, D], F32)
    x1T = sb("x1T", [128, 16, G], BF16)
    wu = [sb("wu%d" % i, [128, 16, 256], BF16) for i in range(2)]
    wd = [sb("wd%d" % i, [128, 1024], BF16) for i in range(4)]
    sq = [sb("sq%d" % i, [128, G], F32) for i in range(2)]
    st = sb("st", [128, 4, 6], F32)
    mv = sb("mv", [128, 2], F32)
    rstd = sb("rstd", [128, 1], F32)
    nmr = sb("nmr", [128, 1], F32)
    epsb = sb("epsb", [128, 1], F32)
    pacc = [ps("pacc%d" % i, [128, 512], F32) for i in range(8)]

    E = S.eng
    S.dma("sp", out=ident[:], in_=ident_d[:, :], writes=["ident"])
    S.dma("sp", out=lnb[:].rearrange("p a d -> p (a d)"),
                                          in_=lnp.rearrange("a d -> (a d)").partition_broadcast(128),
          writes=["lnb"])
    S.op("pool", "memset", epsb[:], EPS, writes=["epsb"])

    pb = [0]

    def layer_norm(src, srcname, gi, outs):
        for c in range(4):
            S.op("dve", "bn_stats", out=st[:, c, :], in_=src[:, 512 * c:512 * c + 512],
                 reads=[srcname], writes=["st%d" % c])
        S.op("dve", "bn_aggr", out=mv[:], in_=st[:].rearrange("p a b -> p (a b)"),
             reads=["st%d" % c for c in range(4)], writes=["mv"])
        S.op("act", "activation", out=rstd[:], in_=mv[:, 1:2], func=AF.Sqrt, bias=epsb[:], scale=1.0,
             reads=["mv", "epsb"], writes=["rstd"])
        S.op("dve", "reciprocal", out=rstd[:], in_=rstd[:], reads=["rstd"], writes=["rstd"])
        S.op("dve", "tensor_scalar", out=src, in0=src, scalar1=mv[:, 0:1], scalar2=rstd[:],
                                                   op0=ALU.subtract, op1=ALU.mult,
             reads=[srcname, "mv", "rstd"], writes=[srcname])
        S.op("pool", "tensor_tensor", out=src, in0=src, in1=lnb[:, gi, :], op=ALU.mult,
             reads=[srcname, "lnb"], writes=[srcname])
        for (ap, name, eng) in outs:
            S.op(eng, "tensor_tensor", out=ap, in0=src, in1=lnb[:, gi + 1, :], op=ALU.add,
                 reads=[srcname, "lnb"], writes=[name])

    for g in range(NT // G):
        t0 = g * G
        for q in range(4):
            S.dma("pool",
                out=wo[:, 4 * q:4 * q + 4, :],
                in_=w_out[512 * q:512 * q + 512, :].rearrange("(c p) d -> p c d", p=128),
                writes=["wo%d" % q] + ["hT%d" % f for f in range(16 * q, 16 * q + 16)])
        for t in range(4):
            i = t % 2
            S.dma("sp",
                out=mx[i][:], in_=mixT[:, t0 + 128 * t:t0 + 128 * t + 128].rearrange("(c p) t -> p c t", p=128),
                writes=["mx%d" % i])
            S.dma("sp", out=xr[0][:], in_=xres[t0 + 128 * t:t0 + 128 * t + 128, :],
                  writes=["xr0"])
            for dg in range(4):
                for c in range(16):
                    S.op("pe", "matmul",
                        pacc[dg][:], lhsT=mx[i][:, c, :], rhs=wo[:, c, 512 * dg:512 * dg + 512],
                        start=(c == 0), stop=(c == 15),
                        reads=["mx%d" % i, "wo%d" % (c // 4)], writes=["pacc%d" % dg])
                S.op("dve", "scalar_tensor_tensor",
                    out=r[i][:, 512 * dg:512 * dg + 512], in0=xr[0][:, 512 * dg:512 * dg + 512], scalar=ALPHA,
                    in1=pacc[dg][:], op0=ALU.mult, op1=ALU.add,
                    reads=["xr0", "pacc%d" % dg], writes=["r%d" % i])
            layer_norm(r[i][:], "r%d" % i, 0, [(x1[:, t, :], "x1_%d" % t, "pool")])
            for c4 in range(4):
                pt = pacc[4 + (c4 % 2)]
                for k in range(4):
                    c = 4 * c4 + k
                    S.op("pe", "transpose", out=pt[:, 128 * k:128 * k + 128], in_=x1[:, t, 128 * c:128 * c + 128],
                         identity=ident[:], reads=["x1_%d" % t, "ident"], writes=["pacc%d" % (4 + c4 % 2)])
                S.op("act", "copy", out=x1T[:, 4 * c4:4 * c4 + 4, 128 * t:128 * t + 128],
                     in_=pt[:, 0:512].rearrange("p (k t) -> p k t", k=4),
                     reads=["pacc%d" % (4 + c4 % 2)], writes=["x1T_%d" % t])
        for f2 in range(32):
            wi = f2 % 2
            S.dma("pool", out=wu[wi][:], in_=w_up[:, 256 * f2:256 * f2 + 256].rearrange("(c p) f -> p c f", p=128),
                  writes=["wu%d" % wi])
            for fh in range(2):
                f = 2 * f2 + fh
                pa = pacc[f % 4]
                for c in range(16):
                    S.op("pe", "matmul", pa[:], lhsT=wu[wi][:, c, 128 * fh:128 * fh + 128], rhs=x1T[:, c, :],
                         start=(c == 0), stop=(c == 15),
                         reads=["wu%d" % wi] + ["x1T_%d" % t for t in range(4)], writes=["pacc%d" % (f % 4)])
                si = f % 2
                S.op("act", "activation", out=sq[si][:], in_=pa[:], func=AF.Square,
                     reads=["pacc%d" % (f % 4)], writes=["sq%d" % si])
                S.op("dve", "scalar_tensor_tensor", out=hT[:, f, :], in0=pa[:], scalar=0.0, in1=sq[si][:],
                     op0=ALU.is_gt, op1=ALU.mult,
                     reads=["pacc%d" % (f % 4), "sq%d" % si], writes=["hT%d" % f, "wo%d" % (f // 16)])
        for dh in range(2):
            for f in range(64):
                wi = f % 4
                S.dma("pool", out=wd[wi][:], in_=w_down[128 * f:128 * f + 128, 1024 * dh:1024 * dh + 1024],
                      writes=["wd%d" % wi])
                for t in range(4):
                    for d2 in range(2):
                        S.op("pe", "matmul", pacc[2 * t + d2][:], lhsT=hT[:, f, 128 * t:128 * t + 128],
                             rhs=wd[wi][:, 512 * d2:512 * d2 + 512], start=(f == 0), stop=(f == 63),
                             reads=["hT%d" % f, "wd%d" % wi], writes=["pacc%d" % (2 * t + d2)])
            for t in range(4):
                for d2 in range(2):
                    dg = 2 * dh + d2
                    S.op("dve", "scalar_tensor_tensor", out=x1[:, t, 512 * dg:512 * dg + 512],
                         in0=x1[:, t, 512 * dg:512 * dg + 512], scalar=ALPHA, in1=pacc[2 * t + d2][:],
                         op0=ALU.mult, op1=ALU.add, reads=["x1_%d" % t, "pacc%d" % (2 * t + d2)], writes=["x1_%d" % t])
        for t in range(4):
            layer_norm(x1[:, t, :], "x1_%d" % t, 2, [(x1[:, t, :], "x1_%d" % t, "pool")])
            S.dma("sp", out=x2[t0 + 128 * t:t0 + 128 * t + 128, :], in_=x1[:, t, :],
                  reads=["x1_%d" % t], writes=["x2out"])
    alltoks = [(k, v[1]) for k, v in S.dma_sems.items() if v[1] > 0]
    S.wait_all("sp", alltoks)
    S.emit()
    S.close()
    for c in reversed(ctxs):
        c.__exit__(None, None, None)
    return nc


N_CORES = 8
DEPTH = 4
_BF = ml_dtypes.bfloat16
_PROGS = {}


def _prog(name, builder):
    if name not in _PROGS:
        _PROGS[name] = builder()
    return _PROGS[name]


def _run(nc, in_maps):
    res = run_bass_kernel_spmd(nc, in_maps, core_ids=list(range(N_CORES)))
    return res.results


def kernel(x, w_in, conv_m, b_i, b_f, m_norm_g, lam_q1, lam_k1, lam_q2, lam_k2,
           c_norm_g, w_out, ln1_g, ln1_b, w_up, w_down, ln2_g, ln2_b):
    f32 = np.float32
    toks = [np.concatenate([np.arange(128 * b, 128 * b + 128) for b in core_blocks(c)]) for c in range(N_CORES)]
    ident_b = np.eye(128).astype(_BF)
    ident_f = np.eye(128, dtype=f32)
    ar = np.arange(128)
    tri = (ar[:, None] <= ar[None, :]).astype(f32)
    slopes_c = 2.0 ** (-8.0 * np.arange(1, 5) / 4)
    ctab = [c_tables(c, slopes_c, 4) for c in range(N_CORES)]
    atab = [a_tables(c) for c in range(N_CORES)]
    p1 = _prog("p1", build_p1)
    pa = _prog("pa", build_pa)
    pb = _prog("pb", build_pb)
    pc = _prog("pc", build_pc)
    p3 = _prog("p3", build_p3)
    xs = [np.ascontiguousarray(x[0][toks[c]]) for c in range(N_CORES)]
    for l in range(DEPTH):
        w_in_l = np.ascontiguousarray(w_in[l])
        r1 = _run(p1, [dict(x=xs[c], w_in=w_in_l, ident=ident_b) for c in range(N_CORES)])

        def gather_T(name, rows, dt):
            out = np.empty((rows, SEQ), dt)
            for c in range(N_CORES):
                out[:, toks[c]] = r1[c][name]
            return out

        def gather_tok(name, cols, dt):
            out = np.empty((SEQ, cols), dt)
            for c in range(N_CORES):
                out[toks[c]] = r1[c][name]
            return out

        kaT = gather_T("kaT", 768, _BF)
        va = gather_tok("va", 768, _BF)
        kiT = gather_T("kiT", 64, _BF)
        kcT = gather_T("kcT", 512, _BF)
        vc = gather_tok("vc", 512, _BF)
        qmT = gather_T("qmT", 768, f32)
        kmT = gather_T("kmT", 768, f32)
        vm = gather_tok("vm", 768, _BF)
        om = gather_tok("om", 768, f32)
        ifm = gather_tok("ifm", 12, f32)
        ra = _run(pa, [dict(qT=r1[c]["qaT"], kT=kaT, v=va, qiT=r1[c]["qiT"], kiT=kiT, wi=r1[c]["wi"],
                            bias=atab[c]["bias"], rbc=atab[c]["rbc"], negb=atab[c]["negb"].astype(_BF),
                            sl=atab[c]["sl"].astype(_BF), ktp1=atab[c]["ktp1"], ident=ident_b, identf=ident_f)
                       for c in range(N_CORES)])
        lam_init = 0.8 - 0.6 * math.exp(-0.3 * l)
        lamp = np.stack([lam_q1[l], lam_k1[l], lam_q2[l], lam_k2[l]]).astype(f32)
        cst = np.array([lam_init, 1.0 - lam_init], f32)
        rc = _run(pc, [dict(qT=r1[c]["qcT"], kT=kcT, v=vc, lam=lamp, gc=np.ascontiguousarray(c_norm_g[l]), cst=cst,
                            bias=ctab[c][0], mask=ctab[c][1].astype(_BF)) for c in range(N_CORES)])
        conv = conv_m[l][:, 0, :]
        inb = []
        for c in range(N_CORES):
            h = c % 6
            cw = np.concatenate([conv[:, 128 * h:128 * h + 128].T, conv[:, 768 + 128 * h:768 + 128 * h + 128].T], axis=1)
            inb.append(dict(qT=np.ascontiguousarray(qmT[128 * h:128 * h + 128]), kT=np.ascontiguousarray(kmT[128 * h:128 * h + 128]),
                            v=np.ascontiguousarray(vm[:, 128 * h:128 * h + 128]), o=np.ascontiguousarray(om[:, 128 * h:128 * h + 128]),
                            ifg=np.ascontiguousarray(np.stack([ifm[:, h], ifm[:, 6 + h]], 1)), cw=np.ascontiguousarray(cw.astype(f32)),
                            bif=np.array([b_i[l][h], b_f[l][h]], f32), gn=np.ascontiguousarray(m_norm_g[l][128 * h:128 * h + 128]),
                            tri=tri, ident=ident_b))
        rb = _run(pb, inb)
        yb = np.concatenate([rb[h]["y"] for h in range(6)], axis=1)
        lnp = np.stack([ln1_g[l], ln1_b[l], ln2_g[l], ln2_b[l]]).astype(f32)
        in3 = []
        for c in range(N_CORES):
            mixed = np.concatenate([ra[c]["y"], yb[toks[c]], rc[c]["y"]], axis=1)
            in3.append(dict(xres=xs[c], mixT=np.ascontiguousarray(mixed.T), w_out=np.ascontiguousarray(w_out[l]),
                            w_up=np.ascontiguousarray(w_up[l]), w_down=np.ascontiguousarray(w_down[l]), lnp=lnp, identf=ident_f))
        r3 = _run(p3, in3)
        xs = [r3[c]["x2"] for c in range(N_CORES)]
    out = np.empty((1, SEQ, D), f32)
    for c in range(N_CORES):
        out[0, toks[c]] = xs[c]
    return out
```

```python
import math
import numpy as np
import ml_dtypes
import concourse.bass as bass
import concourse.mybir as mybir
from concourse.bass_utils import run_bass_kernel_spmd


ENGS = ("pe", "act", "dve", "pool", "sp")
NO_SELF_WAIT = ()


class Sched:
    def __init__(self, nc, n_dma_sems=12):
        self.nc = nc
        self.eng = {"pe": nc.tensor, "act": nc.scalar, "dve": nc.vector,
                    "pool": nc.gpsimd, "sp": nc.sync}
        self.sem = {}
        self.cnt = {e: 0 for e in ENGS}
        self.stream = {e: [] for e in ENGS}
        self.waited = {e: {} for e in ENGS}
        self.last_w = {}
        self.readers = {}
        self.n_dma_sems = n_dma_sems
        self.dma_sems = {}
        self.dma_rr = {e: 0 for e in ENGS}
        self._ctx = []

    def open(self):
        nc = self.nc
        for e in ENGS:
            c = nc.semaphore("s_" + e)
            self.sem[e] = c.__enter__()
            self._ctx.append(c)
        for q in ("sp", "pool", "act"):
            for i in range(self.n_dma_sems):
                c = nc.semaphore("d_%s%d" % (q, i))
                self.dma_sems[(q, i)] = [c.__enter__(), 0]
                self._ctx.append(c)

    def _sem_of(self, key):
        return self.sem[key] if isinstance(key, str) else self.dma_sems[key][0]

    def _deps(self, eng, reads, writes):
        toks = []
        for b in reads:
            t = self.last_w.get(b)
            if t is not None:
                toks.append(t)
        for b in writes:
            t = self.last_w.get(b)
            if t is not None:
                toks.append(t)
            toks.extend(self.readers.get(b, ()))
        need = {}
        for key, val in toks:
            if key == eng and (eng == "pe" or eng in NO_SELF_WAIT):
                continue
            if val > need.get(key, 0):
                need[key] = val
        out = []
        w = self.waited[eng]
        for key, val in need.items():
            if w.get(key, 0) >= val:
                continue
            w[key] = val
            out.append((key, val))
        return out

    def _commit(self, tok, reads, writes):
        for b in reads:
            self.readers.setdefault(b, []).append(tok)
        for b in writes:
            self.last_w[b] = tok
            self.readers[b] = []

    def op(self, eng, fname, *args, reads=(), writes=(), **kw):
        fn = (fname, args, kw)
        waits = self._deps(eng, reads, writes)
        self.cnt[eng] += 1
        tok = (eng, self.cnt[eng])
        self.stream[eng].append((waits, fn, (eng, 1)))
        self._commit(tok, reads, writes)
        return tok

    def dma(self, queue, *args, reads=(), writes=(), fname="dma_start", **kw):
        if fname != "dma_start":
            args, kw = kw["args"], kw["kw"]
        fn = (fname, args, kw)
        idx = self.dma_rr[queue]
        self.dma_rr[queue] = (idx + 1) % self.n_dma_sems
        key = (queue, idx)
        ent = self.dma_sems[key]
        waits = self._deps(queue, reads, writes)
        w = self.waited[queue]
        if ent[1] > 0 and w.get(key, 0) < ent[1]:
            waits.append((key, ent[1]))
            w[key] = ent[1]
        ent[1] += 16
        tok = (key, ent[1])
        self.stream[queue].append((waits, fn, (key, 16)))
        self._commit(tok, reads, writes)
        return tok

    def wait_all(self, eng, toks):
        waits = []
        w = self.waited[eng]
        for key, val in toks:
            if w.get(key, 0) < val:
                w[key] = val
                waits.append((key, val))
        self.stream[eng].append((waits, None, None))

    def emit(self):
        nc = self.nc
        with nc.Block() as block:
            def mk(e):
                def body(engine):
                    for waits, fn, inc in self.stream[e]:
                        for key, val in waits:
                            engine.wait_ge(self._sem_of(key), val)
                        if fn is not None:
                            ins = getattr(engine, fn[0])(*fn[1], **fn[2])
                            ins.then_inc(self._sem_of(inc[0]), inc[1])
                return body
            block.tensor(mk("pe"))
            block.scalar(mk("act"))
            block.vector(mk("dve"))
            block.gpsimd(mk("pool"))
            block.sync(mk("sp"))

    def close(self):
        for c in reversed(self._ctx):
            c.__exit__(None, None, None)
        self._ctx = []


F32 = mybir.dt.float32
BF16 = mybir.dt.bfloat16
PW_SHAPES = {"w_in": (1024, 7508), "w_out": (1024, 2048), "w_up": (1024, 8192), "w_down": (4096, 2048)}


def build_pw():
    nc = bass.Bass("TRN2", target_bir_lowering=False)
    ins, outs = {}, {}
    for name, (r, c) in PW_SHAPES.items():
        ins[name] = nc.dram_tensor(name, [r, c], F32, kind="ExternalInput").ap()
        outs[name] = nc.dram_tensor(name + "_b", [r, c], BF16, kind="ExternalOutput").ap()
    S = Sched(nc)
    S.open()
    ctxs = []
    bufs = []
    for i in range(4):
        cm = nc.sbuf_tensor("cb%d" % i, [128, 4096], BF16)
        bufs.append(cm.__enter__())
        ctxs.append(cm)
    k = 0
    for name, (r, c) in PW_SHAPES.items():
        for r0 in range(0, r, 128):
            for c0 in range(0, c, 4096):
                n = min(4096, c - c0)
                b = bufs[k % 4]
                bn = "cb%d" % (k % 4)
                k += 1
                S.dma("pool", out=b[:, 0:n], in_=ins[name][r0:r0 + 128, c0:c0 + n], writes=[bn])
                S.dma("sp", out=outs[name][r0:r0 + 128, c0:c0 + n], in_=b[:, 0:n], reads=[bn], writes=["o_" + name])
    alltoks = [(kk, v[1]) for kk, v in S.dma_sems.items() if v[1] > 0]
    S.wait_all("sp", alltoks)
    S.emit()
    S.close()
    for cm in reversed(ctxs):
        cm.__exit__(None, None, None)
    return nc


F32 = mybir.dt.float32
BF16 = mybir.dt.bfloat16
AF = mybir.ActivationFunctionType
ALU = mybir.AluOpType
NT = 1024
D = 2048
DIN = 7508

GROUPS = [
    ("qaT", 0, 768, "F", BF16), ("kaT", 768, 768, "F", BF16), ("va", 1536, 768, "T", BF16),
    ("qiT", 2304, 512, "F", BF16), ("kiT", 2816, 64, "F", BF16), ("wi", 2880, 8, "T", F32),
    ("qmT", 2888, 768, "F", F32), ("kmT", 3656, 768, "F", F32), ("vm", 4424, 768, "T", BF16),
    ("om", 5192, 768, "T", F32), ("ifm", 5960, 12, "T", F32),
    ("qcT", 5972, 512, "F", BF16), ("kcT", 6484, 512, "F", BF16), ("vc", 6996, 512, "T", BF16),
]


def build_p1():
    nc = bass.Bass("TRN2", target_bir_lowering=False)
    x = nc.dram_tensor("x", [NT, D], F32, kind="ExternalInput").ap()
    w_in = nc.dram_tensor("w_in", [D, DIN], BF16, kind="ExternalInput").ap()
    ident_d = nc.dram_tensor("ident", [128, 128], BF16, kind="ExternalInput").ap()
    outs = {}
    for (name, off, n, lay, dt) in GROUPS:
        shape = [n, NT] if lay == "F" else [NT, n]
        outs[name] = nc.dram_tensor(name, shape, dt, kind="ExternalOutput").ap()

    S = Sched(nc)
    S.open()
    ctxs = []

    def sb(name, shape, dt):
        c = nc.sbuf_tensor(name, shape, dt)
        t = c.__enter__()
        ctxs.append(c)
        return t

    def ps(name, shape, dt):
        c = nc.psum_tensor(name, shape, dt)
        t = c.__enter__()
        ctxs.append(c)
        return t

    ident = sb("ident_s", [128, 128], BF16)
    xT = sb("xT", [128, 16, NT], BF16)
    xf = [sb("xf%d" % i, [128, D], F32) for i in range(2)]
    xb = [sb("xb%d" % i, [128, D], BF16) for i in range(2)]
    wb = [sb("wb%d" % i, [128, 16, 512], BF16) for i in range(3)]
    stg = [sb("stg%d" % i, [128, 512], F32) for i in range(4)]
    pacc = [ps("pacc%d" % i, [128, 512], F32) for i in range(4)]
    ptr = [ps("ptr%d" % i, [128, 512], BF16) for i in range(2)]

    S.dma("sp", out=ident[:], in_=ident_d[:, :], writes=["ident"])
    for t in range(8):
        i = t % 2
        S.dma("sp", out=xf[i][:], in_=x[128 * t:128 * t + 128, :], writes=["xf%d" % i])
        S.op("dve" if t % 2 == 0 else "pool", "tensor_copy", out=xb[i][:], in_=xf[i][:],
             reads=["xf%d" % i], writes=["xb%d" % i])
        for c4 in range(4):
            pt = ptr[c4 % 2]
            for k in range(4):
                c = 4 * c4 + k
                S.op("pe", "transpose", out=pt[:, 128 * k:128 * k + 128], in_=xb[i][:, 128 * c:128 * c + 128],
                     identity=ident[:], reads=["xb%d" % i, "ident"], writes=["ptr%d" % (c4 % 2)])
            S.op("act", "copy", out=xT[:, 4 * c4:4 * c4 + 4, 128 * t:128 * t + 128],
                 in_=pt[:, 0:512].rearrange("p (k t) -> p k t", k=4),
                 reads=["ptr%d" % (c4 % 2)], writes=["xT"])

    wcnt = [0]
    ecnt = [0]

    def evac(pa, pname, rows, cols, dt):
        k = ecnt[0] % 4
        ecnt[0] += 1
        sv = stg[k][:] if dt == F32 else stg[k][:].bitcast(BF16)
        dst = sv[0:rows, 0:cols]
        if k % 2 == 0:
            S.op("act", "copy", out=dst, in_=pa[0:rows, 0:cols], reads=[pname], writes=["stg%d" % k])
        else:
            S.op("dve", "tensor_copy", out=dst, in_=pa[0:rows, 0:cols], reads=[pname], writes=["stg%d" % k])
        return dst, "stg%d" % k

    for (name, off, n, lay, dt) in GROUPS:
        for c0 in range(0, n, 512):
            ncol = min(512, n - c0)
            wi = wcnt[0] % 3
            wcnt[0] += 1
            S.dma("pool", out=wb[wi][:, :, 0:ncol],
                  in_=w_in[:, off + c0:off + c0 + ncol].rearrange("(c p) e -> p c e", p=128),
                  writes=["wb%d" % wi])
            if lay == "F":
                for e0 in range(0, ncol, 128):
                    ne = min(128, ncol - e0)
                    for tg in range(2):
                        pi = ecnt[0] % 4
                        pa = pacc[pi]
                        for c in range(16):
                            S.op("pe", "matmul", pa[0:ne, :], lhsT=wb[wi][:, c, e0:e0 + ne],
                                 rhs=xT[:, c, 512 * tg:512 * tg + 512], start=(c == 0), stop=(c == 15),
                                 reads=["wb%d" % wi, "xT"], writes=["pacc%d" % pi])
                        dst, sname = evac(pa, "pacc%d" % pi, ne, 512, dt)
                        S.dma("sp", out=outs[name][c0 + e0:c0 + e0 + ne, 512 * tg:512 * tg + 512], in_=dst,
                              reads=[sname], writes=["out_" + name])
            else:
                for t in range(8):
                    pi = ecnt[0] % 4
                    pa = pacc[pi]
                    for c in range(16):
                        S.op("pe", "matmul", pa[:, 0:ncol], lhsT=xT[:, c, 128 * t:128 * t + 128],
                             rhs=wb[wi][:, c, 0:ncol], start=(c == 0), stop=(c == 15),
                             reads=["wb%d" % wi, "xT"], writes=["pacc%d" % pi])
                    dst, sname = evac(pa, "pacc%d" % pi, 128, ncol, dt)
                    S.dma("sp", out=outs[name][128 * t:128 * t + 128, c0:c0 + ncol], in_=dst,
                          reads=[sname], writes=["out_" + name])

    alltoks = [(k, v[1]) for k, v in S.dma_sems.items() if v[1] > 0]
    S.wait_all("sp", alltoks)
    S.emit()
    S.close()
    for c in reversed(ctxs):
        c.__exit__(None, None, None)
    return nc


FLAGS = ''

F32 = mybir.dt.float32
BF16 = mybir.dt.bfloat16
AF = mybir.ActivationFunctionType
ALU = mybir.AluOpType
NT = 1024
SEQ = 8192
H = 4
EPS = 1e-5


def core_blocks(c):
    out = []
    for g in range(4):
        out += [16 * g + c, 16 * g + 15 - c]
    return out


def c_tables(c, slopes, nheads):
    blocks = core_blocks(c)
    bias = np.zeros((128, nheads, 4, 64), np.float32)
    ar = np.arange(128, dtype=np.float32)
    drow = np.zeros((1, 4, 256), np.float32)
    for g in range(4):
        rb = blocks[2 * g + 1]
        R = 128 * rb + 127
        drow[0, g, 0:128] = 128.0 * (rb - blocks[2 * g])
        for kt in range(64):
            for h in range(nheads):
                bias[:, h, g, kt] = slopes[h] * (128 * kt + ar - R)
    mask = np.full((128, 16, 2, 128), -1.0e30, np.float32)
    tri = np.where(ar[:, None] <= ar[None, :], 0.0, -1.0e30)
    for tile in range(2):
        qslot = c if tile == 0 else 15 - c
        for i in range(16):
            if i < qslot:
                mask[:, i, tile, :] = 0.0
            elif i == qslot:
                mask[:, i, tile, :] = tri
    sl = np.zeros((1, nheads, 128), np.float32)
    for h in range(nheads):
        sl[0, h, :] = slopes[h] / 0.125
    return bias.reshape(128, -1), mask.reshape(128, -1), drow.reshape(1, -1), sl.reshape(1, -1)


def build_pc(HR=4, GR=4):
    nc = bass.Bass("TRN2", target_bir_lowering=False)
    qT = nc.dram_tensor("qT", [512, NT], BF16, kind="ExternalInput").ap()
    kT = nc.dram_tensor("kT", [512, SEQ], BF16, kind="ExternalInput").ap()
    v = nc.dram_tensor("v", [SEQ, 512], BF16, kind="ExternalInput").ap()
    lam_d = nc.dram_tensor("lam", [4, 64], F32, kind="ExternalInput").ap()
    gc_d = nc.dram_tensor("gc", [512], F32, kind="ExternalInput").ap()
    cst_d = nc.dram_tensor("cst", [2], F32, kind="ExternalInput").ap()
    bias_d = nc.dram_tensor("bias", [128, H * 4 * 64], F32, kind="ExternalInput").ap()
    drow_d = nc.dram_tensor("drow", [1, 4 * 256], BF16, kind="ExternalInput").ap()
    sl_d = nc.dram_tensor("sl", [1, H * 128], BF16, kind="ExternalInput").ap()
    ident_d = nc.dram_tensor("ident", [128, 128], BF16, kind="ExternalInput").ap()
    mask_d = nc.dram_tensor("mask", [128, 16 * 2 * 128], BF16, kind="ExternalInput").ap()
    y = nc.dram_tensor("y", [NT, 512], BF16, kind="ExternalOutput").ap()

    S = Sched(nc)
    S.open()
    ctxs = []

    def sb(name, shape, dt):
        c = nc.sbuf_tensor(name, shape, dt)
        t = c.__enter__()
        ctxs.append(c)
        return t

    def ps(name, shape, dt):
        c = nc.psum_tensor(name, shape, dt)
        t = c.__enter__()
        ctxs.append(c)
        return t

    qs = sb("qs", [128, H, 2, NT], BF16)
    ks = [sb("ks%d" % i, [128, SEQ], BF16) for i in range(2)]
    vs = [sb("vs%d" % i, [128, 64, 132], BF16) for i in range(2)]
    bias = sb("bias_s", [128, H * 4 * 64], F32)
    drow = sb("drow_s", [1, 4, 256], BF16)
    sl = sb("sl_s", [1, H, 128], BF16)
    ident = sb("ident_s", [128, 128], BF16)
    mask = sb("mask_s", [128, 16, 256], BF16)
    lamb = sb("lamb", [128, 4, 64], F32)
    gcb = sb("gcb", [128, 512], F32)
    cst = sb("cst_s", [128, 2], F32)
    sm = sb("sm", [128, 16], F32)
    epsb = sb("epsb", [128, 1], F32)
    pT = [sb("pT%d" % i, [128, 256], BF16) for i in range(4)]
    ys = sb("ys", [128, 8, 512], BF16)
    t1 = [sb("t1_%d" % i, [128, 128], F32) for i in range(2)]
    t2 = [sb("t2_%d" % i, [128, 128], F32) for i in range(2)]
    junk = sb("junk", [128, 128], F32)
    fs = [sb("fs%d" % i, [128, 8], F32) for i in range(2)]
    pS = [ps("pS%d" % i, [128, 512], F32) for i in range(2)]
    pO = [ps("pO%d" % i, [128, 2, 132], F32) for i in range(4)]

    S.op("pool", "memset", qs[:], 0.0, writes=["qs"])
    for m in range(2):
        S.dma("sp", out=qs[64 * m:64 * m + 64, :, m, :], in_=qT.rearrange("(h p) t -> p h t", p=128)[64 * m:64 * m + 64],
              writes=["qs"])
    S.dma("sp", out=bias[:], in_=bias_d[:, :], writes=["bias"])
    S.dma("sp", out=drow[:].rearrange("p a b -> p (a b)"), in_=drow_d[:, :], writes=["drow"])
    S.dma("sp", out=sl[:].rearrange("p a b -> p (a b)"), in_=sl_d[:, :], writes=["sl"])
    S.dma("sp", out=ident[:], in_=ident_d[:, :], writes=["ident"])
    S.dma("sp", out=mask[:].rearrange("p a b -> p (a b)"), in_=mask_d[:, :], writes=["mask"])
    S.dma("sp", out=lamb[:].rearrange("p a b -> p (a b)"), in_=lam_d.rearrange("a b -> (a b)").partition_broadcast(128),
          writes=["lamb"])
    S.dma("sp", out=gcb[:], in_=gc_d.partition_broadcast(128), writes=["gcb"])
    S.dma("sp", out=cst[:], in_=cst_d.partition_broadcast(128), writes=["cst"])
    S.op("pool", "memset", epsb[:], EPS, writes=["epsb"])
    S.op("pool", "memset", ys[:], 0.0, writes=["ys"])
    for i in range(2):
        S.op("pool", "memset", vs[i][:, :, 128:129], 1.0, writes=["vone%d" % i])
    S.op("dve", "tensor_tensor", out=lamb[:, 0, :], in0=lamb[:, 0, :], in1=lamb[:, 1, :], op=ALU.mult,
         reads=["lamb"], writes=["lamb"])
    S.op("dve", "tensor_tensor", out=lamb[:, 2, :], in0=lamb[:, 2, :], in1=lamb[:, 3, :], op=ALU.mult,
         reads=["lamb"], writes=["lamb"])
    S.op("dve", "reduce_sum", out=sm[:, 1:2], in_=lamb[:, 0, :], axis=mybir.AxisListType.X, reads=["lamb"], writes=["sm1"])
    S.op("dve", "reduce_sum", out=sm[:, 2:3], in_=lamb[:, 2, :], axis=mybir.AxisListType.X, reads=["lamb"], writes=["sm2"])
    S.op("act", "activation", out=sm[:, 1:3], in_=sm[:, 1:3], func=AF.Exp, reads=["sm1", "sm2"], writes=["sm12"])
    S.op("dve", "tensor_tensor", out=sm[:, 0:1], in0=sm[:, 2:3], in1=sm[:, 1:2], op=ALU.subtract,
         reads=["sm12"], writes=["sm0"])
    S.op("dve", "tensor_tensor", out=sm[:, 0:1], in0=sm[:, 0:1], in1=cst[:, 0:1], op=ALU.subtract,
         reads=["sm0", "cst"], writes=["sm0"])
    S.op("dve", "tensor_scalar", out=gcb[:], in0=gcb[:], scalar1=cst[:, 1:2], scalar2=None, op0=ALU.mult,
         reads=["gcb", "cst"], writes=["gcb"])

    ucnt = 0
    fcnt = 0
    for h in range(HR):
        hi = h % 2
        S.dma("sp", out=ks[hi][:], in_=kT[128 * h:128 * h + 128, :], writes=["ks%d" % hi])
        for q4 in range(4):
            S.dma("pool", out=vs[hi][:, 16 * q4:16 * q4 + 16, 0:128],
                  in_=v[2048 * q4:2048 * q4 + 2048, 128 * h:128 * h + 128].rearrange("(kt p) e -> p kt e", p=128),
                  writes=["vs%d_%d" % (hi, q4)])
        units = [(g, kt) for g in range(GR) for kt in range(16 * g + 16)]

        def emit_qk(u, sbuf_i):
            g, kt = u
            diag = kt >= 16 * g
            for m in range(2):
                dst = pS[sbuf_i][:, 256 * m:256 * m + 256]
                S.op("pe", "matmul", dst, lhsT=ks[hi][:, 128 * kt:128 * kt + 128],
                     rhs=qs[:, h, m, 256 * g:256 * g + 256], start=True, stop=False,
                     reads=["ks%d" % hi, "qs"], writes=["pS%d" % sbuf_i])
                S.op("pe", "matmul", dst, lhsT=sl[0:1, h, :], rhs=drow[0:1, g, :], start=False, stop=not diag,
                     reads=["sl", "drow"], writes=["pS%d" % sbuf_i])
                if diag:
                    S.op("pe", "matmul", dst, lhsT=ident[:], rhs=mask[:, kt - 16 * g, :], start=False, stop=True,
                         reads=["ident", "mask"], writes=["pS%d" % sbuf_i])

        def emit_rest(u, sbuf_i):
            nonlocal fcnt
            g, kt = u
            nkt = 16 * g + 16
            ob = (h * 4 + g) % 2
            pSb = pS[sbuf_i]
            for m in range(2):
                pt = pT[2 * sbuf_i + m]
                ptn = "pT%d" % (2 * sbuf_i + m)
                bcol = (h * 4 + g) * 64 + kt
                S.op("act", "activation", out=pt[:], in_=pSb[:, 256 * m:256 * m + 256], func=AF.Exp,
                     bias=bias[:, bcol:bcol + 1], scale=0.125,
                     reads=["pS%d" % sbuf_i, "bias"], writes=[ptn])
                for tile in range(2):
                    S.op("pe", "matmul", pO[2 * ob + m][:, tile, 0:129], lhsT=pt[:, 128 * tile:128 * tile + 128],
                         rhs=vs[hi][:, kt, 0:129], start=(kt == 0 and tile == 0), stop=(kt == nkt - 1 and tile == 1),
                         reads=[ptn, "vs%d_%d" % (hi, kt // 16), "vone%d" % hi], writes=["pO%d" % (2 * ob + m)])
            if kt != nkt - 1:
                return
            for tile in range(2):
                fi = fcnt % 2
                fcnt += 1
                f = fs[fi]
                fn = "fs%d" % fi
                a1 = pO[2 * ob + 0]
                a2 = pO[2 * ob + 1]
                S.op("dve", "reciprocal", out=f[:, 0:1], in_=a1[:, tile, 128:129], reads=["pO%d" % (2 * ob)], writes=[fn])
                S.op("dve", "reciprocal", out=f[:, 1:2], in_=a2[:, tile, 128:129], reads=["pO%d" % (2 * ob + 1)], writes=[fn])
                S.op("dve", "tensor_tensor", out=f[:, 1:2], in0=f[:, 1:2], in1=sm[:, 0:1], op=ALU.mult,
                     reads=[fn, "sm0"], writes=[fn])
                S.op("dve", "tensor_scalar", out=t1[fi][:], in0=a1[:, tile, 0:128], scalar1=f[:, 0:1], scalar2=None,
                     op0=ALU.mult, reads=["pO%d" % (2 * ob), fn], writes=["t1_%d" % fi])
                S.op("dve", "scalar_tensor_tensor", out=t2[fi][:], in0=a2[:, tile, 0:128], scalar=f[:, 1:2],
                     in1=t1[fi][:], op0=ALU.mult, op1=ALU.add,
                     reads=["pO%d" % (2 * ob + 1), fn, "t1_%d" % fi], writes=["t2_%d" % fi])
                S.op("act", "activation", out=junk[:], in_=t2[fi][:], func=AF.Square, accum_out=f[:, 2:3],
                     reads=["t2_%d" % fi], writes=["junk", fn])
                S.op("act", "activation", out=f[:, 3:4], in_=f[:, 2:3], func=AF.Sqrt, bias=epsb[:], scale=1.0 / 128,
                     reads=[fn, "epsb"], writes=[fn])
                S.op("dve", "reciprocal", out=f[:, 3:4], in_=f[:, 3:4], reads=[fn], writes=[fn])
                S.op("dve", "scalar_tensor_tensor", out=ys[:, 2 * g + tile, 128 * h:128 * h + 128], in0=t2[fi][:],
                     scalar=f[:, 3:4], in1=gcb[:, 128 * h:128 * h + 128], op0=ALU.mult, op1=ALU.mult,
                     reads=["t2_%d" % fi, fn, "gcb"], writes=["ys"])

        for idx in range(len(units) + 1):
            if idx < len(units):
                emit_qk(units[idx], (ucnt + idx) % 2)
            if idx >= 1:
                emit_rest(units[idx - 1], (ucnt + idx - 1) % 2)
        ucnt += len(units)
    S.dma("sp", out=y.rearrange("(j p) e -> p j e", p=128), in_=ys[:], reads=["ys"], writes=["yout"])
    alltoks = [(k, v_[1]) for k, v_ in S.dma_sems.items() if v_[1] > 0]
    S.wait_all("sp", alltoks)
    S.emit()
    S.close()
    for c in reversed(ctxs):
        c.__exit__(None, None, None)
    return nc


F32 = mybir.dt.float32
BF16 = mybir.dt.bfloat16
AF = mybir.ActivationFunctionType
ALU = mybir.AluOpType
AX = mybir.AxisListType
SEQ = 8192
NCH = 64
EPS = 1e-5
LNS = math.log(128 ** -0.5)


def build_pb(NJ=NCH):
    nc = bass.Bass("TRN2", target_bir_lowering=False)
    qT_d = nc.dram_tensor("qT", [128, SEQ], F32, kind="ExternalInput").ap()
    kT_d = nc.dram_tensor("kT", [128, SEQ], F32, kind="ExternalInput").ap()
    v_d = nc.dram_tensor("v", [SEQ, 128], BF16, kind="ExternalInput").ap()
    o_d = nc.dram_tensor("o", [SEQ, 128], F32, kind="ExternalInput").ap()
    if_d = nc.dram_tensor("ifg", [SEQ, 2], F32, kind="ExternalInput").ap()
    cw_d = nc.dram_tensor("cw", [128, 8], F32, kind="ExternalInput").ap()
    bif_d = nc.dram_tensor("bif", [2], F32, kind="ExternalInput").ap()
    gn_d = nc.dram_tensor("gn", [128], F32, kind="ExternalInput").ap()
    tri_d = nc.dram_tensor("tri", [128, 128], F32, kind="ExternalInput").ap()
    ident_d = nc.dram_tensor("ident", [128, 128], BF16, kind="ExternalInput").ap()
    y_d = nc.dram_tensor("y", [SEQ, 128], BF16, kind="ExternalOutput").ap()

    S = Sched(nc)
    S.open()
    ctxs = []

    def sb(name, shape, dt):
        c = nc.sbuf_tensor(name, shape, dt)
        t = c.__enter__()
        ctxs.append(c)
        return t

    def ps(name, shape, dt):
        c = nc.psum_tensor(name, shape, dt)
        t = c.__enter__()
        ctxs.append(c)
        return t

    ident = sb("ident_s", [128, 128], BF16)
    tri = sb("tri_s", [128, 128], F32)
    trib = sb("trib", [128, 128], BF16)
    ones = sb("ones", [128, 128], F32)
    cw = sb("cw_s", [128, 8], F32)
    bif = sb("bif_s", [128, 2], F32)
    gnb = sb("gnb", [128, 128], F32)
    epsb = sb("epsb", [128, 1], F32)
    gi = sb("gi", [128, NCH, 2], F32)
    lf = sb("lf", [128, NCH], F32)
    li = sb("li", [128, NCH], F32)
    a_s = sb("a_s", [128, NCH], F32)
    g_s = sb("g_s", [128, NCH], F32)
    u_s = sb("u_s", [128, NCH], F32)
    wi_s = sb("wi_s", [128, NCH], F32)
    ws_s = sb("ws_s", [128, NCH], F32)
    eg = sb("eg", [128, NCH], F32)
    ea = sb("ea", [128, NCH], F32)
    xr = [sb("xr%d" % i, [128, 2048 + 4], F32) for i in range(2)]
    ac = [sb("ac%d" % i, [128, 2048], F32) for i in range(2)]
    QT = sb("QT", [128, SEQ], BF16)
    KT = sb("KT", [128, SEQ], BF16)
    Kt = sb("Kt", [128, NCH, 128], BF16)
    vs = sb("vs", [128, NCH, 132], BF16)
    vi = sb("vi", [128, NCH, 132], BF16)
    vst = sb("vst", [128, NCH, 132], BF16)
    og = sb("og", [128, NCH, 128], BF16)
    of = [sb("of%d" % i, [128, 128], F32) for i in range(2)]
    ys = sb("ys", [128, NCH, 128], BF16)
    C = sb("C", [128, 132], F32)
    Cb = [sb("Cb%d" % i, [128, 132], BF16) for i in range(2)]
    qk = [sb("qk%d" % i, [128, 128], BF16) for i in range(2)]
    hh = [sb("hh%d" % i, [128, 128], F32) for i in range(2)]
    junk = sb("junk", [128, 128], F32)
    fs = [sb("fs%d" % i, [128, 4], F32) for i in range(2)]
    pS = [ps("pS%d" % i, [128, 128], F32) for i in range(2)]
    pX = [ps("pX%d" % i, [128, 132], F32) for i in range(2)]
    pC = [ps("pC%d" % i, [128, 132], F32) for i in range(2)]
    pT = ps("pT", [128, 512], BF16)
    pG = ps("pG", [128, 2, NCH], F32)

    S.dma("sp", out=ident[:], in_=ident_d[:, :], writes=["ident"])
    S.dma("sp", out=tri[:], in_=tri_d[:, :], writes=["tri"])
    S.dma("sp", out=cw[:], in_=cw_d[:, :], writes=["cw"])
    S.dma("sp", out=bif[:], in_=bif_d.partition_broadcast(128), writes=["bif"])
    S.dma("sp", out=gnb[:], in_=gn_d.partition_broadcast(128), writes=["gnb"])
    S.dma("sp", out=gi[:], in_=if_d.rearrange("(j p) c -> p j c", p=128), writes=["gi"])
    S.dma("sp", out=vs[:, :, 0:128], in_=v_d.rearrange("(j p) e -> p j e", p=128), writes=["vs"])
    S.op("pool", "memset", epsb[:], EPS, writes=["epsb"])
    S.op("pool", "memset", ones[:], 1.0, writes=["ones"])
    S.op("pool", "memset", vs[:, :, 128:129], 1.0, reads=[], writes=["vs1"])
    S.op("pool", "memset", C[:], 0.0, writes=["C"])
    S.op("pool", "tensor_copy", out=trib[:], in_=tri[:], reads=["tri"], writes=["trib"])
    S.op("dve", "tensor_scalar", out=li[:], in0=gi[:, :, 0], scalar1=bif[:, 0:1], scalar2=None, op0=ALU.add,
         reads=["gi", "bif"], writes=["li"])
    S.op("dve", "tensor_scalar", out=lf[:], in0=gi[:, :, 1], scalar1=bif[:, 1:2], scalar2=None, op0=ALU.add,
         reads=["gi", "bif"], writes=["lf"])
    S.op("act", "activation", out=lf[:], in_=lf[:], func=AF.Exp, scale=-1.0, reads=["lf"], writes=["lf"])
    S.op("act", "activation", out=lf[:], in_=lf[:], func=AF.Ln, bias=1.0, scale=1.0, reads=["lf"], writes=["lf"])
    S.op("dve", "tensor_scalar", out=lf[:], in0=lf[:], scalar1=-1.0, scalar2=None, op0=ALU.mult,
         reads=["lf"], writes=["lf"])
    S.op("pe", "matmul", pG[:, 0, :], lhsT=tri[:], rhs=lf[:], start=True, stop=False, reads=["tri", "lf"], writes=["pG"])
    S.op("pe", "matmul", pG[:, 1, :], lhsT=ones[:], rhs=lf[:], start=False, stop=True, reads=["ones", "lf"], writes=["pG"])
    S.op("dve", "tensor_copy", out=a_s[:], in_=pG[:, 0, :], reads=["pG"], writes=["a_s"])
    S.op("dve", "tensor_copy", out=g_s[:], in_=pG[:, 1, :], reads=["pG"], writes=["g_s"])
    S.op("dve", "tensor_tensor", out=u_s[:], in0=li[:], in1=a_s[:], op=ALU.subtract, reads=["li", "a_s"], writes=["u_s"])
    S.op("act", "activation", out=wi_s[:], in_=u_s[:], func=AF.Exp, bias=LNS, scale=1.0, reads=["u_s"], writes=["wi_s"])
    S.op("dve", "tensor_tensor", out=u_s[:], in0=u_s[:], in1=g_s[:], op=ALU.add, reads=["u_s", "g_s", "wi_s"], writes=["u_s"])
    S.op("act", "activation", out=ws_s[:], in_=u_s[:], func=AF.Exp, bias=LNS, scale=1.0, reads=["u_s"], writes=["ws_s"])
    S.op("act", "activation", out=eg[:], in_=g_s[:], func=AF.Exp, reads=["g_s"], writes=["eg"])
    S.op("act", "activation", out=ea[:], in_=a_s[:], func=AF.Exp, scale=-1.0, reads=["a_s"], writes=["ea"])
    S.op("dve", "tensor_tensor", out=vi[:, :, 0:129], in0=vs[:, :, 0:129],
         in1=wi_s[:].unsqueeze(2).to_broadcast([128, NCH, 129]), op=ALU.mult,
         reads=["vs", "vs1", "wi_s"], writes=["vi"])
    S.op("pool", "tensor_tensor", out=vst[:, :, 0:129], in0=vs[:, :, 0:129],
         in1=ws_s[:].unsqueeze(2).to_broadcast([128, NCH, 129]), op=ALU.mult,
         reads=["vs", "vs1", "ws_s"], writes=["vst"])
    for which, (src, dst, dname) in enumerate(((qT_d, QT, "QT"), (kT_d, KT, "KT"))):
        for p in range(4):
            i = (which * 4 + p) % 2
            if p == 0:
                S.op("pool", "memset", xr[i][:, 0:4], 0.0, writes=["xr%d" % i])
                S.dma("sp", out=xr[i][:, 4:2052], in_=src[:, 0:2048], writes=["xr%d" % i])
            else:
                S.dma("sp", out=xr[i][:, 1:2052], in_=src[:, 2048 * p - 3:2048 * p + 2048], writes=["xr%d" % i])
            S.op("dve", "tensor_scalar", out=ac[i][:], in0=xr[i][:, 1:2049], scalar1=cw[:, 4 * which:4 * which + 1],
                 scalar2=None, op0=ALU.mult, reads=["xr%d" % i, "cw"], writes=["ac%d" % i])
            for w in range(1, 4):
                S.op("dve", "scalar_tensor_tensor", out=ac[i][:], in0=xr[i][:, 1 + w:2049 + w],
                     scalar=cw[:, 4 * which + w:4 * which + w + 1], in1=ac[i][:], op0=ALU.mult, op1=ALU.add,
                     reads=["xr%d" % i, "cw", "ac%d" % i], writes=["ac%d" % i])
            S.op("act", "activation", out=dst[:, 2048 * p:2048 * p + 2048], in_=ac[i][:], func=AF.Silu,
                 reads=["ac%d" % i], writes=[dname + "%d" % p])
    for j in range(NJ):
        i = j % 2
        S.dma("sp", out=of[i][:], in_=o_d[128 * j:128 * j + 128, :], writes=["of%d" % i])
        S.op("act", "activation", out=of[i][:], in_=of[i][:], func=AF.Sigmoid, reads=["of%d" % i], writes=["of%d" % i])
        S.op("pool", "tensor_tensor", out=og[:, j, :], in0=of[i][:], in1=gnb[:], op=ALU.mult,
             reads=["of%d" % i, "gnb"], writes=["og%d" % j])
    for j4 in range(NJ // 4):
        for k in range(4):
            j = 4 * j4 + k
            S.op("pe", "transpose", out=pT[:, 128 * k:128 * k + 128], in_=KT[:, 128 * j:128 * j + 128], identity=ident[:],
                 reads=["KT%d" % (j // 16), "ident"], writes=["pT"])
        S.op("dve", "tensor_copy", out=Kt[:, 4 * j4:4 * j4 + 4, :], in_=pT[:, 0:512].rearrange("p (k t) -> p k t", k=4),
             reads=["pT"], writes=["Kt%d" % j4])

    for j in range(NJ):
        i = j % 2
        qn = "QT%d" % (j // 16)
        kn = "KT%d" % (j // 16)
        S.op("pe", "matmul", pS[i][:], lhsT=KT[:, 128 * j:128 * j + 128], rhs=QT[:, 128 * j:128 * j + 128],
             start=True, stop=True, reads=[qn, kn], writes=["pS%d" % i])
        S.op("dve", "tensor_tensor", out=qk[i][:], in0=pS[i][:], in1=trib[:], op=ALU.mult,
             reads=["pS%d" % i, "trib"], writes=["qk%d" % i])
        S.op("pe", "matmul", pX[i][:, 0:129], lhsT=qk[i][:], rhs=vi[:, j, 0:129], start=True, stop=(j == 0),
             reads=["qk%d" % i, "vi"], writes=["pX%d" % i])
        if j > 0:
            S.op("pe", "matmul", pX[i][:, 0:129], lhsT=QT[:, 128 * j:128 * j + 128], rhs=Cb[i][:, 0:129],
                 start=False, stop=True, reads=[qn, "Cb%d" % i], writes=["pX%d" % i])
        if j < NJ - 1:
            S.op("pe", "matmul", pC[i][:, 0:129], lhsT=Kt[:, j, :], rhs=vst[:, j, 0:129], start=True, stop=True,
                 reads=["Kt%d" % (j // 4), "vst"], writes=["pC%d" % i])
            S.op("dve", "scalar_tensor_tensor", out=C[:, 0:129], in0=C[:, 0:129], scalar=eg[:, j:j + 1],
                 in1=pC[i][:, 0:129], op0=ALU.mult, op1=ALU.add, reads=["C", "eg", "pC%d" % i], writes=["C"])
            S.op("pool", "tensor_copy", out=Cb[1 - i][:, 0:129], in_=C[:, 0:129], reads=["C"], writes=["Cb%d" % (1 - i)])
        f = fs[i]
        fn = "fs%d" % i
        S.op("dve", "tensor_scalar", out=f[:, 3:4], in0=pX[i][:, 128:129], scalar1=-1.0, scalar2=ea[:, j:j + 1],
             op0=ALU.mult, op1=ALU.max, reads=["pX%d" % i, "ea"], writes=[fn])
        S.op("dve", "tensor_tensor", out=f[:, 0:1], in0=pX[i][:, 128:129], in1=f[:, 3:4], op=ALU.max,
             reads=["pX%d" % i, fn], writes=[fn])
        S.op("dve", "reciprocal", out=f[:, 0:1], in_=f[:, 0:1], reads=[fn], writes=[fn])
        S.op("dve", "tensor_scalar", out=hh[i][:], in0=pX[i][:, 0:128], scalar1=f[:, 0:1], scalar2=None, op0=ALU.mult,
             reads=["pX%d" % i, fn], writes=["hh%d" % i])
        S.op("dve", "scalar_tensor_tensor", out=junk[:], in0=hh[i][:], scalar=1.0, in1=hh[i][:], op0=ALU.mult,
             op1=ALU.mult, accum_out=f[:, 1:2], reads=["hh%d" % i], writes=["junk", fn])
        S.op("act", "activation", out=f[:, 2:3], in_=f[:, 1:2], func=AF.Sqrt, bias=epsb[:], scale=1.0 / 128,
             reads=[fn, "epsb"], writes=[fn])
        S.op("dve", "reciprocal", out=f[:, 2:3], in_=f[:, 2:3], reads=[fn], writes=[fn])
        S.op("dve", "scalar_tensor_tensor", out=ys[:, j, :], in0=hh[i][:], scalar=f[:, 2:3], in1=og[:, j, :],
             op0=ALU.mult, op1=ALU.mult, reads=["hh%d" % i, fn, "og%d" % j], writes=["ys"])
    if NJ < NCH:
        S.op("pool", "memset", ys[:, NJ:NCH, :], 0.0, reads=[], writes=["ys"])
    S.dma("sp", out=y_d.rearrange("(j p) e -> p j e", p=128), in_=ys[:], reads=["ys"], writes=["yout"])
    alltoks = [(k, v_[1]) for k, v_ in S.dma_sems.items() if v_[1] > 0]
    S.wait_all("sp", alltoks)
    S.emit()
    S.close()
    for c in reversed(ctxs):
        c.__exit__(None, None, None)
    return nc


F32 = mybir.dt.float32
BF16 = mybir.dt.bfloat16
AF = mybir.ActivationFunctionType
ALU = mybir.AluOpType
AX = mybir.AxisListType
NT = 1024
SEQ = 8192
HA = 6
HI = 8
KSEL = 256
NIT = 18
SCALE = 128 ** -0.5
NEG = -1.0e30


def core_blocks(c):
    out = []
    for g in range(4):
        out += [16 * g + c, 16 * g + 15 - c]
    return out


def a_tables(c):
    slopes = 2.0 ** (-8.0 * np.arange(1, HA + 1) / HA)
    ar = np.arange(128, dtype=np.float32)
    bias = np.zeros((128, HA, 4, 64), np.float32)
    rbc = np.zeros((128, 4), np.float32)
    for g in range(4):
        rb = 16 * g + 15 - c
        R = 128 * rb + 127
        rbc[:, g] = 128.0 * (rb + 1)
        for h in range(HA):
            for kt in range(64):
                bias[:, h, g, kt] = slopes[h] * (128 * kt + ar - R)
    negb = np.full((128, 16, 2, 128), NEG, np.float32)
    tri = np.where(ar[None, :] <= ar[:, None], 0.0, NEG)
    for tile in range(2):
        qslot = c if tile == 0 else 15 - c
        for i in range(16):
            if i < qslot:
                negb[:, i, tile, :] = 0.0
            elif i == qslot:
                negb[:, i, tile, :] = tri
    sl = np.zeros((1, HA, 128), np.float32)
    for h in range(HA):
        sl[0, h, :] = slopes[h] / SCALE
    ktp1 = np.tile(np.arange(1, 65, dtype=np.float32)[None, :], (128, 1))
    return dict(bias=bias.reshape(128, -1), rbc=rbc, negb=negb.reshape(128, -1), sl=sl.reshape(1, -1), ktp1=ktp1)


def build_pa(GR=4, HR=HA):
    nc = bass.Bass("TRN2", target_bir_lowering=False)
    qT_d = nc.dram_tensor("qT", [768, NT], BF16, kind="ExternalInput").ap()
    kT_d = nc.dram_tensor("kT", [768, SEQ], BF16, kind="ExternalInput").ap()
    v_d = nc.dram_tensor("v", [SEQ, 768], BF16, kind="ExternalInput").ap()
    qiT_d = nc.dram_tensor("qiT", [512, NT], BF16, kind="ExternalInput").ap()
    kiT_d = nc.dram_tensor("kiT", [64, SEQ], BF16, kind="ExternalInput").ap()
    wi_d = nc.dram_tensor("wi", [NT, 8], F32, kind="ExternalInput").ap()
    bias_d = nc.dram_tensor("bias", [128, HA * 4 * 64], F32, kind="ExternalInput").ap()
    rbc_d = nc.dram_tensor("rbc", [128, 4], F32, kind="ExternalInput").ap()
    negb_d = nc.dram_tensor("negb", [128, 16 * 2 * 128], BF16, kind="ExternalInput").ap()
    sl_d = nc.dram_tensor("sl", [1, HA * 128], BF16, kind="ExternalInput").ap()
    ktp1_d = nc.dram_tensor("ktp1", [128, 64], F32, kind="ExternalInput").ap()
    ident_d = nc.dram_tensor("ident", [128, 128], BF16, kind="ExternalInput").ap()
    identf_d = nc.dram_tensor("identf", [128, 128], F32, kind="ExternalInput").ap()
    y_d = nc.dram_tensor("y", [NT, 768], BF16, kind="ExternalOutput").ap()

    S = Sched(nc)
    S.open()
    ctxs = []

    def sb(name, shape, dt):
        c = nc.sbuf_tensor(name, shape, dt)
        t = c.__enter__()
        ctxs.append(c)
        return t

    def ps(name, shape, dt):
        c = nc.psum_tensor(name, shape, dt)
        t = c.__enter__()
        ctxs.append(c)
        return t

    ident = sb("ident_s", [128, 128], BF16)
    identf = sb("identf_s", [128, 128], F32)
    qsg = [sb("qsg%d" % i, [128, HA, 256], BF16) for i in range(2)]
    ks = sb("ks", [128, SEQ], BF16)
    vs2 = [sb("vs%d" % i, [128, 64, 132], BF16) for i in range(2)]
    kis = sb("kis", [64, SEQ], BF16)
    qisg = [sb("qisg%d" % i, [64, HI, 256], BF16) for i in range(2)]
    wi = sb("wi_s", [128, 8, 8], F32)
    wa = sb("wa", [128, 8, 8], F32)
    wsg = sb("wsg", [128, 8, 8], F32)
    bias = sb("bias_s", [128, HA * 4 * 64], F32)
    rbc = sb("rbc_s", [128, 4], F32)
    negb = sb("negb_s", [128, 16, 2, 128], BF16)
    sl = sb("sl_s", [1, HA, 128], BF16)
    ktp1 = sb("ktp1_s", [128, 64], F32)
    I = sb("I", [128, SEQ], F32)
    nsq2 = [sb("nsq%d" % i, [128, SEQ], BF16) for i in range(2)]
    nsT = sb("nsT", [128, 64, 256], BF16)
    rl = [sb("rl%d" % i, [128, 512], F32) for i in range(2)]
    bs = sb("bs", [128, 8 + NIT + 4], F32)
    an = sb("an", [128, 64], F32)
    dcol = sb("dcol", [128, 2, 2], F32)
    drow = sb("drow", [1, 256], BF16)
    pT = [sb("pT%d" % i, [128, 256], BF16) for i in range(3)]
    ysg = [sb("ysg%d" % i, [128, 2, 768], BF16) for i in range(2)]
    fs = [sb("fs%d" % i, [128, 2], F32) for i in range(2)]
    pI = [ps("pI%d" % i, [128, 512], F32) for i in range(2)]
    pTr = ps("pTr", [128, 512], BF16)
    pS = [ps("pS%d" % i, [128, 256], F32) for i in range(2)]
    pO = [ps("pO%d" % i, [128, 2, 132], F32) for i in range(2)]
    pD = ps("pD", [128, 128], F32)

    S.dma("sp", out=ident[:], in_=ident_d[:, :], writes=["ident"])
    S.dma("sp", out=identf[:], in_=identf_d[:, :], writes=["identf"])
    S.dma("sp", out=kis[:], in_=kiT_d[:, :], writes=["kis"])
    S.dma("sp", out=wi[:], in_=wi_d.rearrange("(j p) h -> p j h", p=128), writes=["wi"])
    S.dma("sp", out=bias[:], in_=bias_d[:, :], writes=["bias"])
    S.dma("sp", out=rbc[:], in_=rbc_d[:, :], writes=["rbc"])
    S.dma("sp", out=negb[:].rearrange("p a b c -> p (a b c)"), in_=negb_d[:, :], writes=["negb"])
    S.dma("sp", out=sl[:].rearrange("p a b -> p (a b)"), in_=sl_d[:, :], writes=["sl"])
    S.dma("sp", out=ktp1[:], in_=ktp1_d[:, :], writes=["ktp1"])
    for i in range(2):
        S.op("pool", "memset", vs2[i][:, :, 128:129], 1.0, writes=["vs1_%d" % i])
        S.op("pool", "memset", ysg[i][:], 0.0, writes=["ysg%d" % i])
    S.op("dve", "tensor_scalar", out=wa[:], in0=wi[:], scalar1=-1.0, scalar2=None, op0=ALU.mult, reads=["wi"], writes=["wa"])
    S.op("dve", "tensor_tensor", out=wa[:], in0=wa[:], in1=wi[:], op=ALU.max, reads=["wa", "wi"], writes=["wa"])
    S.op("act", "activation", out=wsg[:], in_=wi[:], func=AF.Sign, reads=["wi"], writes=["wsg"])

    ucnt = 0
    icnt = 0
    ocnt = 0
    def stage_a(g):
        nkt = 16 * g + 16
        NK = 128 * nkt
        nonlocal icnt
        S.dma("sp", out=qisg[g % 2][:], in_=qiT_d.rearrange("(h p) t -> p h t", p=64)[:, :, 256 * g:256 * g + 256],
              writes=["qis%d" % (g % 2)])
        S.dma("sp", out=qsg[g % 2][:], in_=qT_d.rearrange("(h p) t -> p h t", p=128)[:, :, 256 * g:256 * g + 256],
              writes=["qs%d" % (g % 2)])
        for tile in range(2):
            j = 2 * g + tile
            for kc in range(NK // 512):
                for h in range(HI):
                    pi = icnt % 2
                    icnt += 1
                    S.op("pe", "matmul", pI[pi][:], lhsT=qisg[g % 2][:, h, 128 * tile:128 * tile + 128], rhs=kis[:, 512 * kc:512 * kc + 512],
                         start=True, stop=True, reads=["qis%d" % (g % 2), "kis"], writes=["pI%d" % pi])
                    S.op("act", "activation", out=rl[pi][:], in_=pI[pi][:], func=AF.Relu, scale=wa[:, j, h:h + 1],
                         reads=["pI%d" % pi, "wa"], writes=["rl%d" % pi])
                    dst = I[:, 512 * kc:512 * kc + 512]
                    if h == 0:
                        S.op("dve", "tensor_scalar", out=dst, in0=rl[pi][:], scalar1=wsg[:, j, 0:1], scalar2=None,
                             op0=ALU.mult, reads=["rl%d" % pi, "wsg"], writes=["I%d" % kc])
                    else:
                        S.op("dve", "scalar_tensor_tensor", out=dst, in0=rl[pi][:], scalar=wsg[:, j, h:h + 1], in1=dst,
                             op0=ALU.mult, op1=ALU.add, reads=["rl%d" % pi, "wsg", "I%d" % kc], writes=["I%d" % kc])
            Iall = ["I%d" % kc for kc in range(NK // 512)]
            S.op("dve", "tensor_reduce", out=bs[:, 0:1], in_=I[:, 0:NK], axis=AX.X, op=ALU.min, reads=Iall, writes=["bs"])
            S.op("dve", "tensor_tensor", out=I[:, 2048 * g:NK].rearrange("p (a b) -> p a b", a=16),
                 in0=I[:, 2048 * g:NK].rearrange("p (a b) -> p a b", a=16), in1=negb[:, :, tile, :], op=ALU.add,
                 reads=Iall + ["negb"], writes=Iall)
            S.op("dve", "tensor_reduce", out=bs[:, 1:2], in_=I[:, 0:NK], axis=AX.X, op=ALU.max, reads=Iall, writes=["bs"])
            S.op("dve", "tensor_scalar", out=bs[:, 0:1], in0=bs[:, 0:1], scalar1=-1.0, scalar2=None, op0=ALU.add,
                 reads=["bs"], writes=["bs"])
            S.op("dve", "scalar_tensor_tensor", out=bs[:, 8:9], in0=bs[:, 1:2], scalar=1.0, in1=bs[:, 0:1],
                 op0=ALU.add, op1=ALU.subtract, reads=["bs"], writes=["bs"])
            for t in range(1, NIT + 1):
                S.op("dve", "tensor_scalar", out=bs[:, 8 + t:9 + t], in0=bs[:, 7 + t:8 + t], scalar1=0.5, scalar2=None,
                     op0=ALU.mult, reads=["bs"], writes=["bs"])
            for t in range(1, NIT + 1):
                S.op("dve", "tensor_tensor", out=bs[:, 2:3], in0=bs[:, 0:1], in1=bs[:, 8 + t:9 + t], op=ALU.add,
                     reads=["bs"], writes=["bs"])
                S.op("dve", "tensor_scalar", out=nsq2[tile][:, 0:NK], in0=I[:, 0:NK], scalar1=bs[:, 2:3], scalar2=0.0,
                     op0=ALU.is_ge, op1=ALU.add, accum_out=bs[:, 3:4], reads=Iall + ["bs"], writes=["nsq%d" % tile, "bs"])
                S.op("dve", "tensor_scalar", out=bs[:, 4:5], in0=bs[:, 3:4], scalar1=KSEL - 0.5, scalar2=bs[:, 8 + t:9 + t],
                     op0=ALU.is_ge, op1=ALU.mult, reads=["bs"], writes=["bs"])
                S.op("dve", "tensor_tensor", out=bs[:, 0:1], in0=bs[:, 0:1], in1=bs[:, 4:5], op=ALU.add,
                     reads=["bs"], writes=["bs"])
            S.op("dve", "tensor_scalar", out=nsq2[tile][:, 0:NK], in0=I[:, 0:NK], scalar1=bs[:, 0:1], scalar2=NEG,
                 op0=ALU.is_lt, op1=ALU.mult, reads=Iall + ["bs"], writes=["nsq%d" % tile])
            S.op("dve", "tensor_reduce", out=an[:, 0:nkt], in_=nsq2[tile][:, 0:NK].rearrange("p (a b) -> p a b", a=nkt),
                 axis=AX.X, op=ALU.max, reads=["nsq%d" % tile], writes=["an"])
            S.op("dve", "scalar_tensor_tensor", out=an[:, 0:nkt], in0=an[:, 0:nkt], scalar=-1.0, in1=ktp1[:, 0:nkt],
                 op0=ALU.is_ge, op1=ALU.mult, reads=["an", "ktp1"], writes=["an"])
            S.op("dve", "tensor_reduce", out=dcol[:, tile, 0:1], in_=an[:, 0:nkt], axis=AX.X, op=ALU.max, reads=["an"], writes=["dcol%d" % tile])
            S.op("dve", "tensor_scalar", out=dcol[:, tile, 1:2], in0=dcol[:, tile, 0:1], scalar1=-128.0, scalar2=rbc[:, g:g + 1],
                 op0=ALU.mult, op1=ALU.add, reads=["dcol%d" % tile, "rbc"], writes=["dcol%d" % tile])

    def stage_b(g):
        nkt = 16 * g + 16
        NK = 128 * nkt
        for tile in range(2):
            S.op("pe", "transpose", out=pD[0:1, 0:128], in_=dcol[:, tile, 1:2], identity=identf[:],
                 reads=["dcol%d" % tile, "identf"], writes=["pD"])
            S.op("act", "copy", out=drow[0:1, 128 * tile:128 * tile + 128], in_=pD[0:1, 0:128], reads=["pD"], writes=["drow"])
            for k4 in range(nkt // 4):
                for k in range(4):
                    kt = 4 * k4 + k
                    S.op("pe", "transpose", out=pTr[:, 128 * k:128 * k + 128], in_=nsq2[tile][:, 128 * kt:128 * kt + 128],
                         identity=ident[:], reads=["nsq%d" % tile, "ident"], writes=["pTr"])
                S.op("act", "copy", out=nsT[:, 4 * k4:4 * k4 + 4, 128 * tile:128 * tile + 128],
                     in_=pTr[:, 0:512].rearrange("p (k t) -> p k t", k=4), reads=["pTr"], writes=["nsT"])

    def attention(g):
        nkt = 16 * g + 16
        NK = 128 * nkt
        nonlocal ucnt, ocnt
        for h in range(HR):
            vi = (g * HR + h) % 2
            S.dma("sp", out=ks[:, 0:NK], in_=kT_d[128 * h:128 * h + 128, 0:NK], writes=["ks"])
            for q4 in range(g + 1):
                S.dma("sp", out=vs2[vi][:, 16 * q4:16 * q4 + 16, 0:128],
                      in_=v_d[2048 * q4:2048 * q4 + 2048, 128 * h:128 * h + 128].rearrange("(kt p) e -> p kt e", p=128),
                      writes=["vs%d_%d" % (vi, q4)])
            ob = ocnt % 2
            ocnt += 1
            def emit_qk(kt, si):
                S.op("pe", "matmul", pS[si][:], lhsT=ks[:, 128 * kt:128 * kt + 128], rhs=qsg[g % 2][:, h, :],
                     start=True, stop=False, reads=["ks", "qs%d" % (g % 2)], writes=["pS%d" % si])
                S.op("pe", "matmul", pS[si][:], lhsT=sl[0:1, h, :], rhs=drow[0:1, :], start=False, stop=False,
                     reads=["sl", "drow"], writes=["pS%d" % si])
                S.op("pe", "matmul", pS[si][:], lhsT=ident[:], rhs=nsT[:, kt, :], start=False, stop=True,
                     reads=["ident", "nsT"], writes=["pS%d" % si])

            def emit_rest(kt, si, pti):
                bcol = (h * 4 + g) * 64 + kt
                S.op("act", "activation", out=pT[pti][:], in_=pS[si][:], func=AF.Exp, bias=bias[:, bcol:bcol + 1],
                     scale=SCALE, reads=["pS%d" % si, "bias"], writes=["pT%d" % pti])
                for tile in range(2):
                    S.op("pe", "matmul", pO[ob][:, tile, 0:129], lhsT=pT[pti][:, 128 * tile:128 * tile + 128],
                         rhs=vs2[vi][:, kt, 0:129], start=(kt == 0 and tile == 0), stop=(kt == nkt - 1 and tile == 1),
                         reads=["pT%d" % pti, "vs%d_%d" % (vi, kt // 16), "vs1_%d" % vi], writes=["pO%d" % ob])

            for idx in range(nkt + 1):
                if idx < nkt:
                    emit_qk(idx, (ucnt + idx) % 2)
                if idx >= 1:
                    emit_rest(idx - 1, (ucnt + idx - 1) % 2, (ucnt + idx - 1) % 3)
            ucnt += nkt
            for tile in range(2):
                f = fs[tile]
                S.op("dve", "reciprocal", out=f[:, 0:1], in_=pO[ob][:, tile, 128:129], reads=["pO%d" % ob], writes=["fs%d" % tile])
                S.op("dve", "tensor_scalar", out=ysg[g % 2][:, tile, 128 * h:128 * h + 128], in0=pO[ob][:, tile, 0:128],
                     scalar1=f[:, 0:1], scalar2=None, op0=ALU.mult, reads=["pO%d" % ob, "fs%d" % tile], writes=["ysg%d" % (g % 2)])
        S.dma("sp", out=y_d[256 * g:256 * g + 256, :].rearrange("(j p) e -> p j e", p=128), in_=ysg[g % 2][:],
              reads=["ysg%d" % (g % 2)], writes=["yout"])

    stage_a(0)
    stage_b(0)
    for g in range(GR):
        if g + 1 < GR:
            stage_a(g + 1)
        attention(g)
        if g + 1 < GR:
            stage_b(g + 1)
    alltoks = [(k, v_[1]) for k, v_ in S.dma_sems.items() if v_[1] > 0]
    S.wait_all("sp", alltoks)
    S.emit()
    S.close()
    for c in reversed(ctxs):
        c.__exit__(None, None, None)
    return nc


F32 = mybir.dt.float32
BF16 = mybir.dt.bfloat16
AF = mybir.ActivationFunctionType
ALU = mybir.AluOpType
ALPHA = 8.0 ** 0.25
EPS = 1e-5

NT = 1024
D = 2048
FF = 8192
G = 512


def build_p3():
    nc = bass.Bass("TRN2", target_bir_lowering=False)
    xres = nc.dram_tensor("xres", [NT, D], F32, kind="ExternalInput").ap()
    mixT = nc.dram_tensor("mixT", [D, NT], BF16, kind="ExternalInput").ap()
    w_out = nc.dram_tensor("w_out", [D, D], BF16, kind="ExternalInput").ap()
    w_up = nc.dram_tensor("w_up", [D, FF], BF16, kind="ExternalInput").ap()
    w_down = nc.dram_tensor("w_down", [FF, D], BF16, kind="ExternalInput").ap()
    lnp = nc.dram_tensor("lnp", [4, D], F32, kind="ExternalInput").ap()
    ident_d = nc.dram_tensor("identf", [128, 128], F32, kind="ExternalInput").ap()
    x2 = nc.dram_tensor("x2", [NT, D], F32, kind="ExternalOutput").ap()

    S = Sched(nc)
    S.open()
    ctxs = []

    def sb(name, shape, dt):
        c = nc.sbuf_tensor(name, shape, dt)
        t = c.__enter__()
        ctxs.append(c)
        return t

    def ps(name, shape, dt):
        c = nc.psum_tensor(name, shape, dt)
        t = c.__enter__()
        ctxs.append(c)
        return t

    ident = sb("ident_s", [128, 128], F32)
    lnb = sb("lnb", [128, 4, D], F32)
    big = sb("big", [128, 64 * G], BF16)
    wo = big[:].rearrange("p (c d) -> p c d", c=16)
    hT = big[:].rearrange("p (f t) -> p f t", f=64)
    mx = [sb("mx%d" % i, [128, 16, 128], BF16) for i in range(2)]
    xr = [sb("xr%d" % i, [128, D], F32) for i in range(1)]
    r = [sb("r%d" % i, [128, D], F32) for i in range(2)]
    x1 = sb("x1", [128, 4, D], F32)
    x1T = sb("x1T", [128, 16, G], BF16)
    wu = [sb("wu%d" % i, [128, 16, 256], BF16) for i in range(2)]
    wd = [sb("wd%d" % i, [128, 1024], BF16) for i in range(4)]
    sq = [sb("sq%d" % i, [128, G], F32) for i in range(2)]
    st = sb("st", [128, 4, 6], F32)
    mv = sb("mv", [128, 2], F32)
    rstd = sb("rstd", [128, 1], F32)
    nmr = sb("nmr", [128, 1], F32)
    epsb = sb("epsb", [128, 1], F32)
    pacc = [ps("pacc%d" % i, [128, 512], F32) for i in range(8)]

    E = S.eng
    S.dma("sp", out=ident[:], in_=ident_d[:, :], writes=["ident"])
    S.dma("sp", out=lnb[:].rearrange("p a d -> p (a d)"),
                                          in_=lnp.rearrange("a d -> (a d)").partition_broadcast(128),
          writes=["lnb"])
    S.op("pool", "memset", epsb[:], EPS, writes=["epsb"])

    pb = [0]

    def layer_norm(src, srcname, gi, outs):
        for c in range(4):
            S.op("dve", "bn_stats", out=st[:, c, :], in_=src[:, 512 * c:512 * c + 512],
                 reads=[srcname], writes=["st%d" % c])
        S.op("dve", "bn_aggr", out=mv[:], in_=st[:].rearrange("p a b -> p (a b)"),
             reads=["st%d" % c for c in range(4)], writes=["mv"])
        S.op("act", "activation", out=rstd[:], in_=mv[:, 1:2], func=AF.Sqrt, bias=epsb[:], scale=1.0,
             reads=["mv", "epsb"], writes=["rstd"])
        S.op("dve", "reciprocal", out=rstd[:], in_=rstd[:], reads=["rstd"], writes=["rstd"])
        S.op("dve", "tensor_scalar", out=src, in0=src, scalar1=mv[:, 0:1], scalar2=rstd[:],
                                                   op0=ALU.subtract, op1=ALU.mult,
             reads=[srcname, "mv", "rstd"], writes=[srcname])
        S.op("pool", "tensor_tensor", out=src, in0=src, in1=lnb[:, gi, :], op=ALU.mult,
             reads=[srcname, "lnb"], writes=[srcname])
        for (ap, name, eng) in outs:
            S.op(eng, "tensor_tensor", out=ap, in0=src, in1=lnb[:, gi + 1, :], op=ALU.add,
                 reads=[srcname, "lnb"], writes=[name])

    for g in range(NT // G):
        t0 = g * G
        for q in range(4):
            S.dma("pool",
                out=wo[:, 4 * q:4 * q + 4, :],
                in_=w_out[512 * q:512 * q + 512, :].rearrange("(c p) d -> p c d", p=128),
                writes=["wo%d" % q] + ["hT%d" % f for f in range(16 * q, 16 * q + 16)])
        for t in range(4):
            i = t % 2
            S.dma("sp",
                out=mx[i][:], in_=mixT[:, t0 + 128 * t:t0 + 128 * t + 128].rearrange("(c p) t -> p c t", p=128),
                writes=["mx%d" % i])
            S.dma("sp", out=xr[0][:], in_=xres[t0 + 128 * t:t0 + 128 * t + 128, :],
                  writes=["xr0"])
            for dg in range(4):
                for c in range(16):
                    S.op("pe", "matmul",
                        pacc[dg][:], lhsT=mx[i][:, c, :], rhs=wo[:, c, 512 * dg:512 * dg + 512],
                        start=(c == 0), stop=(c == 15),
                        reads=["mx%d" % i, "wo%d" % (c // 4)], writes=["pacc%d" % dg])
                S.op("dve", "scalar_tensor_tensor",
                    out=r[i][:, 512 * dg:512 * dg + 512], in0=xr[0][:, 512 * dg:512 * dg + 512], scalar=ALPHA,
                    in1=pacc[dg][:], op0=ALU.mult, op1=ALU.add,
                    reads=["xr0", "pacc%d" % dg], writes=["r%d" % i])
            layer_norm(r[i][:], "r%d" % i, 0, [(x1[:, t, :], "x1_%d" % t, "pool")])
            for c4 in range(4):
                pt = pacc[4 + (c4 % 2)]
                for k in range(4):
                    c = 4 * c4 + k
                    S.op("pe", "transpose", out=pt[:, 128 * k:128 * k + 128], in_=x1[:, t, 128 * c:128 * c + 128],
                         identity=ident[:], reads=["x1_%d" % t, "ident"], writes=["pacc%d" % (4 + c4 % 2)])
                S.op("act", "copy", out=x1T[:, 4 * c4:4 * c4 + 4, 128 * t:128 * t + 128],
                     in_=pt[:, 0:512].rearrange("p (k t) -> p k t", k=4),
                     reads=["pacc%d" % (4 + c4 % 2)], writes=["x1T_%d" % t])
        for f2 in range(32):
            wi = f2 % 2
            S.dma("pool", out=wu[wi][:], in_=w_up[:, 256 * f2:256 * f2 + 256].rearrange("(c p) f -> p c f", p=128),
                  writes=["wu%d" % wi])
            for fh in range(2):
                f = 2 * f2 + fh
                pa = pacc[f % 4]
                for c in range(16):
                    S.op("pe", "matmul", pa[:], lhsT=wu[wi][:, c, 128 * fh:128 * fh + 128], rhs=x1T[:, c, :],
                         start=(c == 0), stop=(c == 15),
                         reads=["wu%d" % wi] + ["x1T_%d" % t for t in range(4)], writes=["pacc%d" % (f % 4)])
                si = f % 2
                S.op("act", "activation", out=sq[si][:], in_=pa[:], func=AF.Square,
                     reads=["pacc%d" % (f % 4)], writes=["sq%d" % si])
                S.op("dve", "scalar_tensor_tensor", out=hT[:, f, :], in0=pa[:], scalar=0.0, in1=sq[si][:],
                     op0=ALU.is_gt, op1=ALU.mult,
                     reads=["pacc%d" % (f % 4), "sq%d" % si], writes=["hT%d" % f, "wo%d" % (f // 16)])
        for dh in range(2):
            for f in range(64):
                wi = f % 4
                S.dma("pool", out=wd[wi][:], in_=w_down[128 * f:128 * f + 128, 1024 * dh:1024 * dh + 1024],
                      writes=["wd%d" % wi])
                for t in range(4):
                    for d2 in range(2):
                        S.op("pe", "matmul", pacc[2 * t + d2][:], lhsT=hT[:, f, 128 * t:128 * t + 128],
                             rhs=wd[wi][:, 512 * d2:512 * d2 + 512], start=(f == 0), stop=(f == 63),
                             reads=["hT%d" % f, "wd%d" % wi], writes=["pacc%d" % (2 * t + d2)])
            for t in range(4):
                for d2 in range(2):
                    dg = 2 * dh + d2
                    S.op("dve", "scalar_tensor_tensor", out=x1[:, t, 512 * dg:512 * dg + 512],
                         in0=x1[:, t, 512 * dg:512 * dg + 512], scalar=ALPHA, in1=pacc[2 * t + d2][:],
                         op0=ALU.mult, op1=ALU.add, reads=["x1_%d" % t, "pacc%d" % (2 * t + d2)], writes=["x1_%d" % t])
        for t in range(4):
            layer_norm(x1[:, t, :], "x1_%d" % t, 2, [(x1[:, t, :], "x1_%d" % t, "pool")])
            S.dma("sp", out=x2[t0 + 128 * t:t0 + 128 * t + 128, :], in_=x1[:, t, :],
                  reads=["x1_%d" % t], writes=["x2out"])
    alltoks = [(k, v[1]) for k, v in S.dma_sems.items() if v[1] > 0]
    S.wait_all("sp", alltoks)
    S.emit()
    S.close()
    for c in reversed(ctxs):
        c.__exit__(None, None, None)
    return nc


N_CORES = 8
DEPTH = 4
_BF = ml_dtypes.bfloat16
_PROGS = {}


def _prog(name, builder):
    if name not in _PROGS:
        _PROGS[name] = builder()
    return _PROGS[name]


def _run(nc, in_maps):
    res = run_bass_kernel_spmd(nc, in_maps, core_ids=list(range(N_CORES)))
    return res.results


def kernel(x, w_in, conv_m, b_i, b_f, m_norm_g, lam_q1, lam_k1, lam_q2, lam_k2,
           c_norm_g, w_out, ln1_g, ln1_b, w_up, w_down, ln2_g, ln2_b):
    f32 = np.float32
    toks = [np.concatenate([np.arange(128 * b, 128 * b + 128) for b in core_blocks(c)]) for c in range(N_CORES)]
    ident_b = np.eye(128).astype(_BF)
    ident_f = np.eye(128, dtype=f32)
    ar = np.arange(128)
    tri = (ar[:, None] <= ar[None, :]).astype(f32)
    slopes_c = 2.0 ** (-8.0 * np.arange(1, 5) / 4)
    ctab = [c_tables(c, slopes_c, 4) for c in range(N_CORES)]
    atab = [a_tables(c) for c in range(N_CORES)]
    pw = _prog("pw", build_pw)
    rw = _run(pw, [dict(w_in=np.ascontiguousarray(w_in[:, 256 * c:256 * c + 256, :]).reshape(1024, DIN),
                        w_out=np.ascontiguousarray(w_out[:, 256 * c:256 * c + 256, :]).reshape(1024, D),
                        w_up=np.ascontiguousarray(w_up[:, 256 * c:256 * c + 256, :]).reshape(1024, FF),
                        w_down=np.ascontiguousarray(w_down[:, 1024 * c:1024 * c + 1024, :]).reshape(4096, D))
                   for c in range(N_CORES)])

    def wcat(name, l, rows):
        return np.concatenate([rw[c][name][rows * l:rows * l + rows] for c in range(N_CORES)], axis=0)

    p1 = _prog("p1", build_p1)
    pa = _prog("pa", build_pa)
    pb = _prog("pb", build_pb)
    pc = _prog("pc", build_pc)
    p3 = _prog("p3", build_p3)
    xs = [np.ascontiguousarray(x[0][toks[c]]) for c in range(N_CORES)]
    for l in range(DEPTH):
        w_in_l = wcat("w_in_b", l, 256)
        w_out_l = wcat("w_out_b", l, 256)
        w_up_l = wcat("w_up_b", l, 256)
        w_down_l = wcat("w_down_b", l, 1024)
        r1 = _run(p1, [dict(x=xs[c], w_in=w_in_l, ident=ident_b) for c in range(N_CORES)])

        def gather_T(name, rows, dt):
            out = np.empty((rows, SEQ), dt)
            for c in range(N_CORES):
                out[:, toks[c]] = r1[c][name]
            return out

        def gather_tok(name, cols, dt):
            out = np.empty((SEQ, cols), dt)
            for c in range(N_CORES):
                out[toks[c]] = r1[c][name]
            return out

        kaT = gather_T("kaT", 768, _BF)
        va = gather_tok("va", 768, _BF)
        kiT = gather_T("kiT", 64, _BF)
        kcT = gather_T("kcT", 512, _BF)
        vc = gather_tok("vc", 512, _BF)
        qmT = gather_T("qmT", 768, f32)
        kmT = gather_T("kmT", 768, f32)
        vm = gather_tok("vm", 768, _BF)
        om = gather_tok("om", 768, f32)
        ifm = gather_tok("ifm", 12, f32)
        ra = _run(pa, [dict(qT=r1[c]["qaT"], kT=kaT, v=va, qiT=r1[c]["qiT"], kiT=kiT, wi=r1[c]["wi"],
                            bias=atab[c]["bias"], rbc=atab[c]["rbc"], negb=atab[c]["negb"].astype(_BF),
                            sl=atab[c]["sl"].astype(_BF), ktp1=atab[c]["ktp1"], ident=ident_b, identf=ident_f)
                       for c in range(N_CORES)])
        lam_init = 0.8 - 0.6 * math.exp(-0.3 * l)
        lamp = np.stack([lam_q1[l], lam_k1[l], lam_q2[l], lam_k2[l]]).astype(f32)
        cst = np.array([lam_init, 1.0 - lam_init], f32)
        rc = _run(pc, [dict(qT=r1[c]["qcT"], kT=kcT, v=vc, lam=lamp, gc=np.ascontiguousarray(c_norm_g[l]), cst=cst,
                            bias=ctab[c][0], mask=ctab[c][1].astype(_BF), drow=ctab[c][2].astype(_BF), sl=ctab[c][3].astype(_BF),
                            ident=ident_b) for c in range(N_CORES)])
        conv = conv_m[l][:, 0, :]
        inb = []
        for c in range(N_CORES):
            h = c % 6
            cw = np.concatenate([conv[:, 128 * h:128 * h + 128].T, conv[:, 768 + 128 * h:768 + 128 * h + 128].T], axis=1)
            inb.append(dict(qT=np.ascontiguousarray(qmT[128 * h:128 * h + 128]), kT=np.ascontiguousarray(kmT[128 * h:128 * h + 128]),
                            v=np.ascontiguousarray(vm[:, 128 * h:128 * h + 128]), o=np.ascontiguousarray(om[:, 128 * h:128 * h + 128]),
                            ifg=np.ascontiguousarray(np.stack([ifm[:, h], ifm[:, 6 + h]], 1)), cw=np.ascontiguousarray(cw.astype(f32)),
                            bif=np.array([b_i[l][h], b_f[l][h]], f32), gn=np.ascontiguousarray(m_norm_g[l][128 * h:128 * h + 128]),
                            tri=tri, ident=ident_b))
        rb = _run(pb, inb)
        yb = np.concatenate([rb[h]["y"] for h in range(6)], axis=1)
        lnp = np.stack([ln1_g[l], ln1_b[l], ln2_g[l], ln2_b[l]]).astype(f32)
        in3 = []
        for c in range(N_CORES):
            mixed = np.concatenate([ra[c]["y"], yb[toks[c]], rc[c]["y"]], axis=1)
            in3.append(dict(xres=xs[c], mixT=np.ascontiguousarray(mixed.T), w_out=w_out_l,
                            w_up=w_up_l, w_down=w_down_l, lnp=lnp, identf=ident_f))
        r3 = _run(p3, in3)
        xs = [r3[c]["x2"] for c in range(N_CORES)]
    out = np.empty((1, SEQ, D), f32)
    for c in range(N_CORES):
        out[0, toks[c]] = xs[c]
    return out
```

```python
import math
import numpy as np
import ml_dtypes
import concourse.bass as bass
import concourse.mybir as mybir
from concourse.bass_utils import run_bass_kernel_spmd


ENGS = ("pe", "act", "dve", "pool", "sp")
NO_SELF_WAIT = ()


class Sched:
    def __init__(self, nc, n_dma_sems=12):
        self.nc = nc
        self.eng = {"pe": nc.tensor, "act": nc.scalar, "dve": nc.vector,
                    "pool": nc.gpsimd, "sp": nc.sync}
        self.sem = {}
        self.cnt = {e: 0 for e in ENGS}
        self.stream = {e: [] for e in ENGS}
        self.waited = {e: {} for e in ENGS}
        self.last_w = {}
        self.readers = {}
        self.n_dma_sems = n_dma_sems
        self.dma_sems = {}
        self.dma_rr = {e: 0 for e in ENGS}
        self._ctx = []

    def open(self):
        nc = self.nc
        for e in ENGS:
            c = nc.semaphore("s_" + e)
            self.sem[e] = c.__enter__()
            self._ctx.append(c)
        for q in ("sp", "pool", "act"):
            for i in range(self.n_dma_sems):
                c = nc.semaphore("d_%s%d" % (q, i))
                self.dma_sems[(q, i)] = [c.__enter__(), 0]
                self._ctx.append(c)

    def _sem_of(self, key):
        return self.sem[key] if isinstance(key, str) else self.dma_sems[key][0]

    def _deps(self, eng, reads, writes):
        toks = []
        for b in reads:
            t = self.last_w.get(b)
            if t is not None:
                toks.append(t)
        for b in writes:
            t = self.last_w.get(b)
            if t is not None:
                toks.append(t)
            toks.extend(self.readers.get(b, ()))
        need = {}
        for key, val in toks:
            if key == eng and (eng == "pe" or eng in NO_SELF_WAIT):
                continue
            if val > need.get(key, 0):
                need[key] = val
        out = []
        w = self.waited[eng]
        for key, val in need.items():
            if w.get(key, 0) >= val:
                continue
            w[key] = val
            out.append((key, val))
        return out

    def _commit(self, tok, reads, writes):
        for b in reads:
            self.readers.setdefault(b, []).append(tok)
        for b in writes:
            self.last_w[b] = tok
            self.readers[b] = []

    def op(self, eng, fname, *args, reads=(), writes=(), **kw):
        fn = (fname, args, kw)
        waits = self._deps(eng, reads, writes)
        self.cnt[eng] += 1
        tok = (eng, self.cnt[eng])
        self.stream[eng].append((waits, fn, (eng, 1)))
        self._commit(tok, reads, writes)
        return tok

    def dma(self, queue, *args, reads=(), writes=(), fname="dma_start", **kw):
        if fname != "dma_start":
            args, kw = kw["args"], kw["kw"]
        fn = (fname, args, kw)
        idx = self.dma_rr[queue]
        self.dma_rr[queue] = (idx + 1) % self.n_dma_sems
        key = (queue, idx)
        ent = self.dma_sems[key]
        waits = self._deps(queue, reads, writes)
        w = self.waited[queue]
        if ent[1] > 0 and w.get(key, 0) < ent[1]:
            waits.append((key, ent[1]))
            w[key] = ent[1]
        ent[1] += 16
        tok = (key, ent[1])
        self.stream[queue].append((waits, fn, (key, 16)))
        self._commit(tok, reads, writes)
        return tok

    def wait_all(self, eng, toks):
        waits = []
        w = self.waited[eng]
        for key, val in toks:
            if w.get(key, 0) < val:
                w[key] = val
                waits.append((key, val))
        self.stream[eng].append((waits, None, None))

    def emit(self):
        nc = self.nc
        with nc.Block() as block:
            def mk(e):
                def body(engine):
                    for waits, fn, inc in self.stream[e]:
                        for key, val in waits:
                            engine.wait_ge(self._sem_of(key), val)
                        if fn is not None:
                            ins = getattr(engine, fn[0])(*fn[1], **fn[2])
                            ins.then_inc(self._sem_of(inc[0]), inc[1])
                return body
            block.tensor(mk("pe"))
            block.scalar(mk("act"))
            block.vector(mk("dve"))
            block.gpsimd(mk("pool"))
            block.sync(mk("sp"))

    def close(self):
        for c in reversed(self._ctx):
            c.__exit__(None, None, None)
        self._ctx = []


F32 = mybir.dt.float32
BF16 = mybir.dt.bfloat16
PW_SHAPES = {"w_in": (1024, 7508), "w_out": (1024, 2048), "w_up": (1024, 8192), "w_down": (4096, 2048)}


def build_pw():
    nc = bass.Bass("TRN2", target_bir_lowering=False)
    ins, outs = {}, {}
    for name, (r, c) in PW_SHAPES.items():
        ins[name] = nc.dram_tensor(name, [r, c], F32, kind="ExternalInput").ap()
        outs[name] = nc.dram_tensor(name + "_b", [r, c], BF16, kind="ExternalOutput").ap()
    S = Sched(nc)
    S.open()
    ctxs = []
    bufs = []
    for i in range(4):
        cm = nc.sbuf_tensor("cb%d" % i, [128, 4096], BF16)
        bufs.append(cm.__enter__())
        ctxs.append(cm)
    k = 0
    for name, (r, c) in PW_SHAPES.items():
        for r0 in range(0, r, 128):
            for c0 in range(0, c, 4096):
                n = min(4096, c - c0)
                b = bufs[k % 4]
                bn = "cb%d" % (k % 4)
                k += 1
                S.dma("pool", out=b[:, 0:n], in_=ins[name][r0:r0 + 128, c0:c0 + n], writes=[bn])
                S.dma("sp", out=outs[name][r0:r0 + 128, c0:c0 + n], in_=b[:, 0:n], reads=[bn], writes=["o_" + name])
    alltoks = [(kk, v[1]) for kk, v in S.dma_sems.items() if v[1] > 0]
    S.wait_all("sp", alltoks)
    S.emit()
    S.close()
    for cm in reversed(ctxs):
        cm.__exit__(None, None, None)
    return nc


F32 = mybir.dt.float32
BF16 = mybir.dt.bfloat16
AF = mybir.ActivationFunctionType
ALU = mybir.AluOpType
NT = 1024
D = 2048
DIN = 7508

GROUPS = [
    ("qaT", 0, 768, "F", BF16), ("kaT", 768, 768, "F", BF16), ("va", 1536, 768, "T", BF16),
    ("qiT", 2304, 512, "F", BF16), ("kiT", 2816, 64, "F", BF16), ("wi", 2880, 8, "T", F32),
    ("qmT", 2888, 768, "F", F32), ("kmT", 3656, 768, "F", F32), ("vm", 4424, 768, "T", BF16),
    ("om", 5192, 768, "T", F32), ("ifm", 5960, 12, "T", F32),
    ("qcT", 5972, 512, "F", BF16), ("kcT", 6484, 512, "F", BF16), ("vc", 6996, 512, "T", BF16),
]


def build_p1():
    nc = bass.Bass("TRN2", target_bir_lowering=False)
    x = nc.dram_tensor("x", [NT, D], F32, kind="ExternalInput").ap()
    w_in = nc.dram_tensor("w_in", [D, DIN], BF16, kind="ExternalInput").ap()
    ident_d = nc.dram_tensor("ident", [128, 128], BF16, kind="ExternalInput").ap()
    outs = {}
    for (name, off, n, lay, dt) in GROUPS:
        shape = [n, NT] if lay == "F" else [NT, n]
        outs[name] = nc.dram_tensor(name, shape, dt, kind="ExternalOutput").ap()

    S = Sched(nc)
    S.open()
    ctxs = []

    def sb(name, shape, dt):
        c = nc.sbuf_tensor(name, shape, dt)
        t = c.__enter__()
        ctxs.append(c)
        return t

    def ps(name, shape, dt):
        c = nc.psum_tensor(name, shape, dt)
        t = c.__enter__()
        ctxs.append(c)
        return t

    ident = sb("ident_s", [128, 128], BF16)
    xT = sb("xT", [128, 16, NT], BF16)
    xf = [sb("xf%d" % i, [128, D], F32) for i in range(2)]
    xb = [sb("xb%d" % i, [128, D], BF16) for i in range(2)]
    wb = [sb("wb%d" % i, [128, 16, 512], BF16) for i in range(3)]
    stg = [sb("stg%d" % i, [128, 512], F32) for i in range(4)]
    pacc = [ps("pacc%d" % i, [128, 512], F32) for i in range(4)]
    ptr = [ps("ptr%d" % i, [128, 512], BF16) for i in range(2)]

    S.dma("sp", out=ident[:], in_=ident_d[:, :], writes=["ident"])
    for t in range(8):
        i = t % 2
        S.dma("sp", out=xf[i][:], in_=x[128 * t:128 * t + 128, :], writes=["xf%d" % i])
        S.op("dve" if t % 2 == 0 else "pool", "tensor_copy", out=xb[i][:], in_=xf[i][:],
             reads=["xf%d" % i], writes=["xb%d" % i])
        for c4 in range(4):
            pt = ptr[c4 % 2]
            for k in range(4):
                c = 4 * c4 + k
                S.op("pe", "transpose", out=pt[:, 128 * k:128 * k + 128], in_=xb[i][:, 128 * c:128 * c + 128],
                     identity=ident[:], reads=["xb%d" % i, "ident"], writes=["ptr%d" % (c4 % 2)])
            S.op("act", "copy", out=xT[:, 4 * c4:4 * c4 + 4, 128 * t:128 * t + 128],
                 in_=pt[:, 0:512].rearrange("p (k t) -> p k t", k=4),
                 reads=["ptr%d" % (c4 % 2)], writes=["xT"])

    wcnt = [0]
    ecnt = [0]

    def evac(pa, pname, rows, cols, dt):
        k = ecnt[0] % 4
        ecnt[0] += 1
        sv = stg[k][:] if dt == F32 else stg[k][:].bitcast(BF16)
        dst = sv[0:rows, 0:cols]
        if k % 2 == 0:
            S.op("act", "copy", out=dst, in_=pa[0:rows, 0:cols], reads=[pname], writes=["stg%d" % k])
        else:
            S.op("dve", "tensor_copy", out=dst, in_=pa[0:rows, 0:cols], reads=[pname], writes=["stg%d" % k])
        return dst, "stg%d" % k

    for (name, off, n, lay, dt) in GROUPS:
        for c0 in range(0, n, 512):
            ncol = min(512, n - c0)
            wi = wcnt[0] % 3
            wcnt[0] += 1
            S.dma("pool", out=wb[wi][:, :, 0:ncol],
                  in_=w_in[:, off + c0:off + c0 + ncol].rearrange("(c p) e -> p c e", p=128),
                  writes=["wb%d" % wi])
            if lay == "F":
                for e0 in range(0, ncol, 128):
                    ne = min(128, ncol - e0)
                    for tg in range(2):
                        pi = ecnt[0] % 4
                        pa = pacc[pi]
                        for c in range(16):
                            S.op("pe", "matmul", pa[0:ne, :], lhsT=wb[wi][:, c, e0:e0 + ne],
                                 rhs=xT[:, c, 512 * tg:512 * tg + 512], start=(c == 0), stop=(c == 15),
                                 reads=["wb%d" % wi, "xT"], writes=["pacc%d" % pi])
                        dst, sname = evac(pa, "pacc%d" % pi, ne, 512, dt)
                        S.dma("sp", out=outs[name][c0 + e0:c0 + e0 + ne, 512 * tg:512 * tg + 512], in_=dst,
                              reads=[sname], writes=["out_" + name])
            else:
                for t in range(8):
                    pi = ecnt[0] % 4
                    pa = pacc[pi]
                    for c in range(16):
                        S.op("pe", "matmul", pa[:, 0:ncol], lhsT=xT[:, c, 128 * t:128 * t + 128],
                             rhs=wb[wi][:, c, 0:ncol], start=(c == 0), stop=(c == 15),
                             reads=["wb%d" % wi, "xT"], writes=["pacc%d" % pi])
                    dst, sname = evac(pa, "pacc%d" % pi, 128, ncol, dt)
                    S.dma("sp", out=outs[name][128 * t:128 * t + 128, c0:c0 + ncol], in_=dst,
                          reads=[sname], writes=["out_" + name])

    alltoks = [(k, v[1]) for k, v in S.dma_sems.items() if v[1] > 0]
    S.wait_all("sp", alltoks)
    S.emit()
    S.close()
    for c in reversed(ctxs):
        c.__exit__(None, None, None)
    return nc


FLAGS = ''

F32 = mybir.dt.float32
BF16 = mybir.dt.bfloat16
AF = mybir.ActivationFunctionType
ALU = mybir.AluOpType
NT = 1024
SEQ = 8192
H = 4
EPS = 1e-5


def core_blocks(c):
    out = []
    for g in range(4):
        out += [16 * g + c, 16 * g + 15 - c]
    return out


def c_tables(c, slopes, nheads):
    blocks = core_blocks(c)
    bias = np.zeros((128, nheads, 4, 64), np.float32)
    ar = np.arange(128, dtype=np.float32)
    drow = np.zeros((1, 4, 256), np.float32)
    for g in range(4):
        rb = blocks[2 * g + 1]
        R = 128 * rb + 127
        drow[0, g, 0:128] = 128.0 * (rb - blocks[2 * g])
        for kt in range(64):
            for h in range(nheads):
                bias[:, h, g, kt] = slopes[h] * (128 * kt + ar - R)
    mask = np.full((128, 16, 2, 128), -1.0e30, np.float32)
    tri = np.where(ar[:, None] <= ar[None, :], 0.0, -1.0e30)
    for tile in range(2):
        qslot = c if tile == 0 else 15 - c
        for i in range(16):
            if i < qslot:
                mask[:, i, tile, :] = 0.0
            elif i == qslot:
                mask[:, i, tile, :] = tri
    sl = np.zeros((1, nheads, 128), np.float32)
    for h in range(nheads):
        sl[0, h, :] = slopes[h] / 0.125
    return bias.reshape(128, -1), mask.reshape(128, -1), drow.reshape(1, -1), sl.reshape(1, -1)


def build_pc(HR=4, GR=4):
    nc = bass.Bass("TRN2", target_bir_lowering=False)
    qT = nc.dram_tensor("qT", [512, NT], BF16, kind="ExternalInput").ap()
    kT = nc.dram_tensor("kT", [512, SEQ], BF16, kind="ExternalInput").ap()
    v = nc.dram_tensor("v", [SEQ, 512], BF16, kind="ExternalInput").ap()
    lam_d = nc.dram_tensor("lam", [4, 64], F32, kind="ExternalInput").ap()
    gc_d = nc.dram_tensor("gc", [512], F32, kind="ExternalInput").ap()
    cst_d = nc.dram_tensor("cst", [2], F32, kind="ExternalInput").ap()
    bias_d = nc.dram_tensor("bias", [128, H * 4 * 64], F32, kind="ExternalInput").ap()
    drow_d = nc.dram_tensor("drow", [1, 4 * 256], BF16, kind="ExternalInput").ap()
    sl_d = nc.dram_tensor("sl", [1, H * 128], BF16, kind="ExternalInput").ap()
    ident_d = nc.dram_tensor("ident", [128, 128], BF16, kind="ExternalInput").ap()
    mask_d = nc.dram_tensor("mask", [128, 16 * 2 * 128], BF16, kind="ExternalInput").ap()
    y = nc.dram_tensor("y", [NT, 512], BF16, kind="ExternalOutput").ap()

    S = Sched(nc)
    S.open()
    ctxs = []

    def sb(name, shape, dt):
        c = nc.sbuf_tensor(name, shape, dt)
        t = c.__enter__()
        ctxs.append(c)
        return t

    def ps(name, shape, dt):
        c = nc.psum_tensor(name, shape, dt)
        t = c.__enter__()
        ctxs.append(c)
        return t

    qs = sb("qs", [128, H, 2, NT], BF16)
    ks = [sb("ks%d" % i, [128, SEQ], BF16) for i in range(2)]
    vs = [sb("vs%d" % i, [128, 64, 132], BF16) for i in range(2)]
    bias = sb("bias_s", [128, H * 4 * 64], F32)
    drow = sb("drow_s", [1, 4, 256], BF16)
    drow2 = sb("drow2_s", [1, 4, 2, 256], BF16)
    sl = sb("sl_s", [1, H, 128], BF16)
    ident = sb("ident_s", [128, 128], BF16)
    mask = sb("mask_s", [128, 16, 256], BF16)
    lamb = sb("lamb", [128, 4, 64], F32)
    gcb = sb("gcb", [128, 512], F32)
    cst = sb("cst_s", [128, 2], F32)
    sm = sb("sm", [128, 16], F32)
    epsb = sb("epsb", [128, 1], F32)
    pT = [sb("pT%d" % i, [128, 256], BF16) for i in range(4)]
    ys = sb("ys", [128, 8, 512], BF16)
    t1 = [sb("t1_%d" % i, [128, 128], F32) for i in range(2)]
    t2 = [sb("t2_%d" % i, [128, 128], F32) for i in range(2)]
    junk = sb("junk", [128, 128], F32)
    fs = [sb("fs%d" % i, [128, 8], F32) for i in range(2)]
    pS = [ps("pS%d" % i, [128, 512], F32) for i in range(2)]
    pO = [ps("pO%d" % i, [128, 2, 132], F32) for i in range(4)]

    S.op("pool", "memset", qs[:], 0.0, writes=["qs"])
    for m in range(2):
        S.dma("sp", out=qs[64 * m:64 * m + 64, :, m, :], in_=qT.rearrange("(h p) t -> p h t", p=128)[64 * m:64 * m + 64],
              writes=["qs"])
    S.dma("sp", out=bias[:], in_=bias_d[:, :], writes=["bias"])
    S.dma("sp", out=drow[:].rearrange("p a b -> p (a b)"), in_=drow_d[:, :], writes=["drow"])
    S.dma("sp", out=sl[:].rearrange("p a b -> p (a b)"), in_=sl_d[:, :], writes=["sl"])
    for m in range(2):
        S.dma("sp", out=drow2[0:1, :, m, :], in_=drow_d.rearrange("p (a b) -> p a b", a=4), writes=["drow"])
    S.dma("sp", out=ident[:], in_=ident_d[:, :], writes=["ident"])
    S.dma("sp", out=mask[:].rearrange("p a b -> p (a b)"), in_=mask_d[:, :], writes=["mask"])
    S.dma("sp", out=lamb[:].rearrange("p a b -> p (a b)"), in_=lam_d.rearrange("a b -> (a b)").partition_broadcast(128),
          writes=["lamb"])
    S.dma("sp", out=gcb[:], in_=gc_d.partition_broadcast(128), writes=["gcb"])
    S.dma("sp", out=cst[:], in_=cst_d.partition_broadcast(128), writes=["cst"])
    S.op("pool", "memset", epsb[:], EPS, writes=["epsb"])
    S.op("pool", "memset", ys[:], 0.0, writes=["ys"])
    for i in range(2):
        S.op("pool", "memset", vs[i][:, :, 128:129], 1.0, writes=["vone%d" % i])
    S.op("dve", "tensor_tensor", out=lamb[:, 0, :], in0=lamb[:, 0, :], in1=lamb[:, 1, :], op=ALU.mult,
         reads=["lamb"], writes=["lamb"])
    S.op("dve", "tensor_tensor", out=lamb[:, 2, :], in0=lamb[:, 2, :], in1=lamb[:, 3, :], op=ALU.mult,
         reads=["lamb"], writes=["lamb"])
    S.op("dve", "reduce_sum", out=sm[:, 1:2], in_=lamb[:, 0, :], axis=mybir.AxisListType.X, reads=["lamb"], writes=["sm1"])
    S.op("dve", "reduce_sum", out=sm[:, 2:3], in_=lamb[:, 2, :], axis=mybir.AxisListType.X, reads=["lamb"], writes=["sm2"])
    S.op("act", "activation", out=sm[:, 1:3], in_=sm[:, 1:3], func=AF.Exp, reads=["sm1", "sm2"], writes=["sm12"])
    S.op("dve", "tensor_tensor", out=sm[:, 0:1], in0=sm[:, 2:3], in1=sm[:, 1:2], op=ALU.subtract,
         reads=["sm12"], writes=["sm0"])
    S.op("dve", "tensor_tensor", out=sm[:, 0:1], in0=sm[:, 0:1], in1=cst[:, 0:1], op=ALU.subtract,
         reads=["sm0", "cst"], writes=["sm0"])
    S.op("dve", "tensor_scalar", out=gcb[:], in0=gcb[:], scalar1=cst[:, 1:2], scalar2=None, op0=ALU.mult,
         reads=["gcb", "cst"], writes=["gcb"])

    ucnt = 0
    fcnt = 0
    for h in range(HR):
        hi = h % 2
        S.dma("sp", out=ks[hi][:], in_=kT[128 * h:128 * h + 128, :], writes=["ks%d" % hi])
        for q4 in range(4):
            S.dma("pool", out=vs[hi][:, 16 * q4:16 * q4 + 16, 0:128],
                  in_=v[2048 * q4:2048 * q4 + 2048, 128 * h:128 * h + 128].rearrange("(kt p) e -> p kt e", p=128),
                  writes=["vs%d_%d" % (hi, q4)])
        units = [(g, kt) for g in range(GR) for kt in range(16 * g + 16)]

        def emit_qk(u, sbuf_i):
            g, kt = u
            diag = kt >= 16 * g
            dst = pS[sbuf_i][:, 0:512].rearrange("p (m q) -> p m q", m=2)
            S.op("pe", "matmul", dst, lhsT=ks[hi][:, 128 * kt:128 * kt + 128],
                 rhs=qs[:, h, :, 256 * g:256 * g + 256], start=True, stop=False,
                 reads=["ks%d" % hi, "qs"], writes=["pS%d" % sbuf_i])
            S.op("pe", "matmul", dst, lhsT=sl[0:1, h, :], rhs=drow2[0:1, g, :, :], start=False, stop=not diag,
                 reads=["sl", "drow"], writes=["pS%d" % sbuf_i])
            if diag:
                for m in range(2):
                    S.op("pe", "matmul", pS[sbuf_i][:, 256 * m:256 * m + 256], lhsT=ident[:], rhs=mask[:, kt - 16 * g, :],
                         start=False, stop=(m == 1), reads=["ident", "mask"], writes=["pS%d" % sbuf_i])

        def emit_rest(u, sbuf_i):
            nonlocal fcnt
            g, kt = u
            nkt = 16 * g + 16
            ob = (h * 4 + g) % 2
            pSb = pS[sbuf_i]
            for m in range(2):
                pt = pT[2 * sbuf_i + m]
                ptn = "pT%d" % (2 * sbuf_i + m)
                bcol = (h * 4 + g) * 64 + kt
                S.op("act", "activation", out=pt[:], in_=pSb[:, 256 * m:256 * m + 256], func=AF.Exp,
                     bias=bias[:, bcol:bcol + 1], scale=0.125,
                     reads=["pS%d" % sbuf_i, "bias"], writes=[ptn])
                for tile in range(2):
                    S.op("pe", "matmul", pO[2 * ob + m][:, tile, 0:129], lhsT=pt[:, 128 * tile:128 * tile + 128],
                         rhs=vs[hi][:, kt, 0:129], start=(kt == 0 and tile == 0), stop=(kt == nkt - 1 and tile == 1),
                         reads=[ptn, "vs%d_%d" % (hi, kt // 16), "vone%d" % hi], writes=["pO%d" % (2 * ob + m)])
            if kt != nkt - 1:
                return
            for tile in range(2):
                fi = fcnt % 2
                fcnt += 1
                f = fs[fi]
                fn = "fs%d" % fi
                a1 = pO[2 * ob + 0]
                a2 = pO[2 * ob + 1]
                S.op("dve", "reciprocal", out=f[:, 0:1], in_=a1[:, tile, 128:129], reads=["pO%d" % (2 * ob)], writes=[fn])
                S.op("dve", "reciprocal", out=f[:, 1:2], in_=a2[:, tile, 128:129], reads=["pO%d" % (2 * ob + 1)], writes=[fn])
                S.op("dve", "tensor_tensor", out=f[:, 1:2], in0=f[:, 1:2], in1=sm[:, 0:1], op=ALU.mult,
                     reads=[fn, "sm0"], writes=[fn])
                S.op("dve", "tensor_scalar", out=t1[fi][:], in0=a1[:, tile, 0:128], scalar1=f[:, 0:1], scalar2=None,
                     op0=ALU.mult, reads=["pO%d" % (2 * ob), fn], writes=["t1_%d" % fi])
                S.op("dve", "scalar_tensor_tensor", out=t2[fi][:], in0=a2[:, tile, 0:128], scalar=f[:, 1:2],
                     in1=t1[fi][:], op0=ALU.mult, op1=ALU.add,
                     reads=["pO%d" % (2 * ob + 1), fn, "t1_%d" % fi], writes=["t2_%d" % fi])
                S.op("act", "activation", out=junk[:], in_=t2[fi][:], func=AF.Square, accum_out=f[:, 2:3],
                     reads=["t2_%d" % fi], writes=["junk", fn])
                S.op("act", "activation", out=f[:, 3:4], in_=f[:, 2:3], func=AF.Sqrt, bias=epsb[:], scale=1.0 / 128,
                     reads=[fn, "epsb"], writes=[fn])
                S.op("dve", "reciprocal", out=f[:, 3:4], in_=f[:, 3:4], reads=[fn], writes=[fn])
                S.op("dve", "scalar_tensor_tensor", out=ys[:, 2 * g + tile, 128 * h:128 * h + 128], in0=t2[fi][:],
                     scalar=f[:, 3:4], in1=gcb[:, 128 * h:128 * h + 128], op0=ALU.mult, op1=ALU.mult,
                     reads=["t2_%d" % fi, fn, "gcb"], writes=["ys"])

        for idx in range(len(units) + 1):
            if idx < len(units):
                emit_qk(units[idx], (ucnt + idx) % 2)
            if idx >= 1:
                emit_rest(units[idx - 1], (ucnt + idx - 1) % 2)
        ucnt += len(units)
    S.dma("sp", out=y.rearrange("(j p) e -> p j e", p=128), in_=ys[:], reads=["ys"], writes=["yout"])
    alltoks = [(k, v_[1]) for k, v_ in S.dma_sems.items() if v_[1] > 0]
    S.wait_all("sp", alltoks)
    S.emit()
    S.close()
    for c in reversed(ctxs):
        c.__exit__(None, None, None)
    return nc


F32 = mybir.dt.float32
BF16 = mybir.dt.bfloat16
AF = mybir.ActivationFunctionType
ALU = mybir.AluOpType
AX = mybir.AxisListType
SEQ = 8192
NCH = 64
EPS = 1e-5
LNS = math.log(128 ** -0.5)


def build_pb(NJ=NCH):
    nc = bass.Bass("TRN2", target_bir_lowering=False)
    qT_d = nc.dram_tensor("qT", [128, SEQ], F32, kind="ExternalInput").ap()
    kT_d = nc.dram_tensor("kT", [128, SEQ], F32, kind="ExternalInput").ap()
    v_d = nc.dram_tensor("v", [SEQ, 128], BF16, kind="ExternalInput").ap()
    o_d = nc.dram_tensor("o", [SEQ, 128], F32, kind="ExternalInput").ap()
    if_d = nc.dram_tensor("ifg", [SEQ, 2], F32, kind="ExternalInput").ap()
    cw_d = nc.dram_tensor("cw", [128, 8], F32, kind="ExternalInput").ap()
    bif_d = nc.dram_tensor("bif", [2], F32, kind="ExternalInput").ap()
    gn_d = nc.dram_tensor("gn", [128], F32, kind="ExternalInput").ap()
    tri_d = nc.dram_tensor("tri", [128, 128], F32, kind="ExternalInput").ap()
    ident_d = nc.dram_tensor("ident", [128, 128], BF16, kind="ExternalInput").ap()
    y_d = nc.dram_tensor("y", [SEQ, 128], BF16, kind="ExternalOutput").ap()

    S = Sched(nc)
    S.open()
    ctxs = []

    def sb(name, shape, dt):
        c = nc.sbuf_tensor(name, shape, dt)
        t = c.__enter__()
        ctxs.append(c)
        return t

    def ps(name, shape, dt):
        c = nc.psum_tensor(name, shape, dt)
        t = c.__enter__()
        ctxs.append(c)
        return t

    ident = sb("ident_s", [128, 128], BF16)
    tri = sb("tri_s", [128, 128], F32)
    trib = sb("trib", [128, 128], BF16)
    ones = sb("ones", [128, 128], F32)
    cw = sb("cw_s", [128, 8], F32)
    bif = sb("bif_s", [128, 2], F32)
    gnb = sb("gnb", [128, 128], F32)
    epsb = sb("epsb", [128, 1], F32)
    gi = sb("gi", [128, NCH, 2], F32)
    lf = sb("lf", [128, NCH], F32)
    li = sb("li", [128, NCH], F32)
    a_s = sb("a_s", [128, NCH], F32)
    g_s = sb("g_s", [128, NCH], F32)
    u_s = sb("u_s", [128, NCH], F32)
    wi_s = sb("wi_s", [128, NCH], F32)
    ws_s = sb("ws_s", [128, NCH], F32)
    eg = sb("eg", [128, NCH], F32)
    ea = sb("ea", [128, NCH], F32)
    xr = [sb("xr%d" % i, [128, 2048 + 4], F32) for i in range(2)]
    ac = [sb("ac%d" % i, [128, 2048], F32) for i in range(2)]
    QT = sb("QT", [128, SEQ], BF16)
    KT = sb("KT", [128, SEQ], BF16)
    Kt = sb("Kt", [128, NCH, 128], BF16)
    vs = sb("vs", [128, NCH, 132], BF16)
    vi = sb("vi", [128, NCH, 132], BF16)
    vst = sb("vst", [128, NCH, 132], BF16)
    og = sb("og", [128, NCH, 128], BF16)
    of = [sb("of%d" % i, [128, 128], F32) for i in range(2)]
    ys = sb("ys", [128, NCH, 128], BF16)
    C = sb("C", [128, 132], F32)
    Cb = [sb("Cb%d" % i, [128, 132], BF16) for i in range(2)]
    qk = [sb("qk%d" % i, [128, 128], BF16) for i in range(2)]
    hh = [sb("hh%d" % i, [128, 128], F32) for i in range(2)]
    junk = sb("junk", [128, 128], F32)
    fs = [sb("fs%d" % i, [128, 4], F32) for i in range(2)]
    pS = [ps("pS%d" % i, [128, 128], F32) for i in range(2)]
    pX = [ps("pX%d" % i, [128, 132], F32) for i in range(2)]
    pC = [ps("pC%d" % i, [128, 132], F32) for i in range(2)]
    pT = ps("pT", [128, 512], BF16)
    pG = ps("pG", [128, 2, NCH], F32)

    S.dma("sp", out=ident[:], in_=ident_d[:, :], writes=["ident"])
    S.dma("sp", out=tri[:], in_=tri_d[:, :], writes=["tri"])
    S.dma("sp", out=cw[:], in_=cw_d[:, :], writes=["cw"])
    S.dma("sp", out=bif[:], in_=bif_d.partition_broadcast(128), writes=["bif"])
    S.dma("sp", out=gnb[:], in_=gn_d.partition_broadcast(128), writes=["gnb"])
    S.dma("sp", out=gi[:], in_=if_d.rearrange("(j p) c -> p j c", p=128), writes=["gi"])
    S.dma("sp", out=vs[:, :, 0:128], in_=v_d.rearrange("(j p) e -> p j e", p=128), writes=["vs"])
    S.op("pool", "memset", epsb[:], EPS, writes=["epsb"])
    S.op("pool", "memset", ones[:], 1.0, writes=["ones"])
    S.op("pool", "memset", vs[:, :, 128:129], 1.0, reads=[], writes=["vs1"])
    S.op("pool", "memset", C[:], 0.0, writes=["C"])
    S.op("pool", "tensor_copy", out=trib[:], in_=tri[:], reads=["tri"], writes=["trib"])
    S.op("dve", "tensor_scalar", out=li[:], in0=gi[:, :, 0], scalar1=bif[:, 0:1], scalar2=None, op0=ALU.add,
         reads=["gi", "bif"], writes=["li"])
    S.op("dve", "tensor_scalar", out=lf[:], in0=gi[:, :, 1], scalar1=bif[:, 1:2], scalar2=None, op0=ALU.add,
         reads=["gi", "bif"], writes=["lf"])
    S.op("act", "activation", out=lf[:], in_=lf[:], func=AF.Exp, scale=-1.0, reads=["lf"], writes=["lf"])
    S.op("act", "activation", out=lf[:], in_=lf[:], func=AF.Ln, bias=1.0, scale=1.0, reads=["lf"], writes=["lf"])
    S.op("dve", "tensor_scalar", out=lf[:], in0=lf[:], scalar1=-1.0, scalar2=None, op0=ALU.mult,
         reads=["lf"], writes=["lf"])
    S.op("pe", "matmul", pG[:, 0, :], lhsT=tri[:], rhs=lf[:], start=True, stop=False, reads=["tri", "lf"], writes=["pG"])
    S.op("pe", "matmul", pG[:, 1, :], lhsT=ones[:], rhs=lf[:], start=False, stop=True, reads=["ones", "lf"], writes=["pG"])
    S.op("dve", "tensor_copy", out=a_s[:], in_=pG[:, 0, :], reads=["pG"], writes=["a_s"])
    S.op("dve", "tensor_copy", out=g_s[:], in_=pG[:, 1, :], reads=["pG"], writes=["g_s"])
    S.op("dve", "tensor_tensor", out=u_s[:], in0=li[:], in1=a_s[:], op=ALU.subtract, reads=["li", "a_s"], writes=["u_s"])
    S.op("act", "activation", out=wi_s[:], in_=u_s[:], func=AF.Exp, bias=LNS, scale=1.0, reads=["u_s"], writes=["wi_s"])
    S.op("dve", "tensor_tensor", out=u_s[:], in0=u_s[:], in1=g_s[:], op=ALU.add, reads=["u_s", "g_s", "wi_s"], writes=["u_s"])
    S.op("act", "activation", out=ws_s[:], in_=u_s[:], func=AF.Exp, bias=LNS, scale=1.0, reads=["u_s"], writes=["ws_s"])
    S.op("act", "activation", out=eg[:], in_=g_s[:], func=AF.Exp, reads=["g_s"], writes=["eg"])
    S.op("act", "activation", out=ea[:], in_=a_s[:], func=AF.Exp, scale=-1.0, reads=["a_s"], writes=["ea"])
    S.op("dve", "tensor_tensor", out=vi[:, :, 0:129], in0=vs[:, :, 0:129],
         in1=wi_s[:].unsqueeze(2).to_broadcast([128, NCH, 129]), op=ALU.mult,
         reads=["vs", "vs1", "wi_s"], writes=["vi"])
    S.op("pool", "tensor_tensor", out=vst[:, :, 0:129], in0=vs[:, :, 0:129],
         in1=ws_s[:].unsqueeze(2).to_broadcast([128, NCH, 129]), op=ALU.mult,
         reads=["vs", "vs1", "ws_s"], writes=["vst"])
    for which, (src, dst, dname) in enumerate(((qT_d, QT, "QT"), (kT_d, KT, "KT"))):
        for p in range(4):
            i = (which * 4 + p) % 2
            if p == 0:
                S.op("pool", "memset", xr[i][:, 0:4], 0.0, writes=["xr%d" % i])
                S.dma("sp", out=xr[i][:, 4:2052], in_=src[:, 0:2048], writes=["xr%d" % i])
            else:
                S.dma("sp", out=xr[i][:, 1:2052], in_=src[:, 2048 * p - 3:2048 * p + 2048], writes=["xr%d" % i])
            S.op("dve", "tensor_scalar", out=ac[i][:], in0=xr[i][:, 1:2049], scalar1=cw[:, 4 * which:4 * which + 1],
                 scalar2=None, op0=ALU.mult, reads=["xr%d" % i, "cw"], writes=["ac%d" % i])
            for w in range(1, 4):
                S.op("dve", "scalar_tensor_tensor", out=ac[i][:], in0=xr[i][:, 1 + w:2049 + w],
                     scalar=cw[:, 4 * which + w:4 * which + w + 1], in1=ac[i][:], op0=ALU.mult, op1=ALU.add,
                     reads=["xr%d" % i, "cw", "ac%d" % i], writes=["ac%d" % i])
            S.op("act", "activation", out=dst[:, 2048 * p:2048 * p + 2048], in_=ac[i][:], func=AF.Silu,
                 reads=["ac%d" % i], writes=[dname + "%d" % p])
    for j in range(NJ):
        i = j % 2
        S.dma("sp", out=of[i][:], in_=o_d[128 * j:128 * j + 128, :], writes=["of%d" % i])
        S.op("act", "activation", out=of[i][:], in_=of[i][:], func=AF.Sigmoid, reads=["of%d" % i], writes=["of%d" % i])
        S.op("pool", "tensor_tensor", out=og[:, j, :], in0=of[i][:], in1=gnb[:], op=ALU.mult,
             reads=["of%d" % i, "gnb"], writes=["og%d" % j])
    for j4 in range(NJ // 4):
        for k in range(4):
            j = 4 * j4 + k
            S.op("pe", "transpose", out=pT[:, 128 * k:128 * k + 128], in_=KT[:, 128 * j:128 * j + 128], identity=ident[:],
                 reads=["KT%d" % (j // 16), "ident"], writes=["pT"])
        S.op("dve", "tensor_copy", out=Kt[:, 4 * j4:4 * j4 + 4, :], in_=pT[:, 0:512].rearrange("p (k t) -> p k t", k=4),
             reads=["pT"], writes=["Kt%d" % j4])

    for j in range(NJ):
        i = j % 2
        qn = "QT%d" % (j // 16)
        kn = "KT%d" % (j // 16)
        S.op("pe", "matmul", pS[i][:], lhsT=KT[:, 128 * j:128 * j + 128], rhs=QT[:, 128 * j:128 * j + 128],
             start=True, stop=True, reads=[qn, kn], writes=["pS%d" % i])
        S.op("dve", "tensor_tensor", out=qk[i][:], in0=pS[i][:], in1=trib[:], op=ALU.mult,
             reads=["pS%d" % i, "trib"], writes=["qk%d" % i])
        S.op("pe", "matmul", pX[i][:, 0:129], lhsT=qk[i][:], rhs=vi[:, j, 0:129], start=True, stop=(j == 0),
             reads=["qk%d" % i, "vi"], writes=["pX%d" % i])
        if j > 0:
            S.op("pe", "matmul", pX[i][:, 0:129], lhsT=QT[:, 128 * j:128 * j + 128], rhs=Cb[i][:, 0:129],
                 start=False, stop=True, reads=[qn, "Cb%d" % i], writes=["pX%d" % i])
        if j < NJ - 1:
            S.op("pe", "matmul", pC[i][:, 0:129], lhsT=Kt[:, j, :], rhs=vst[:, j, 0:129], start=True, stop=True,
                 reads=["Kt%d" % (j // 4), "vst"], writes=["pC%d" % i])
            S.op("dve", "scalar_tensor_tensor", out=C[:, 0:129], in0=C[:, 0:129], scalar=eg[:, j:j + 1],
                 in1=pC[i][:, 0:129], op0=ALU.mult, op1=ALU.add, reads=["C", "eg", "pC%d" % i], writes=["C"])
            S.op("pool", "tensor_copy", out=Cb[1 - i][:, 0:129], in_=C[:, 0:129], reads=["C"], writes=["Cb%d" % (1 - i)])
        f = fs[i]
        fn = "fs%d" % i
        S.op("dve", "tensor_scalar", out=f[:, 3:4], in0=pX[i][:, 128:129], scalar1=-1.0, scalar2=ea[:, j:j + 1],
             op0=ALU.mult, op1=ALU.max, reads=["pX%d" % i, "ea"], writes=[fn])
        S.op("dve", "tensor_tensor", out=f[:, 0:1], in0=pX[i][:, 128:129], in1=f[:, 3:4], op=ALU.max,
             reads=["pX%d" % i, fn], writes=[fn])
        S.op("dve", "reciprocal", out=f[:, 0:1], in_=f[:, 0:1], reads=[fn], writes=[fn])
        S.op("dve", "tensor_scalar", out=hh[i][:], in0=pX[i][:, 0:128], scalar1=f[:, 0:1], scalar2=None, op0=ALU.mult,
             reads=["pX%d" % i, fn], writes=["hh%d" % i])
        S.op("dve", "scalar_tensor_tensor", out=junk[:], in0=hh[i][:], scalar=1.0, in1=hh[i][:], op0=ALU.mult,
             op1=ALU.mult, accum_out=f[:, 1:2], reads=["hh%d" % i], writes=["junk", fn])
        S.op("act", "activation", out=f[:, 2:3], in_=f[:, 1:2], func=AF.Sqrt, bias=epsb[:], scale=1.0 / 128,
             reads=[fn, "epsb"], writes=[fn])
        S.op("dve", "reciprocal", out=f[:, 2:3], in_=f[:, 2:3], reads=[fn], writes=[fn])
        S.op("dve", "scalar_tensor_tensor", out=ys[:, j, :], in0=hh[i][:], scalar=f[:, 2:3], in1=og[:, j, :],
             op0=ALU.mult, op1=ALU.mult, reads=["hh%d" % i, fn, "og%d" % j], writes=["ys"])
    if NJ < NCH:
        S.op("pool", "memset", ys[:, NJ:NCH, :], 0.0, reads=[], writes=["ys"])
    S.dma("sp", out=y_d.rearrange("(j p) e -> p j e", p=128), in_=ys[:], reads=["ys"], writes=["yout"])
    alltoks = [(k, v_[1]) for k, v_ in S.dma_sems.items() if v_[1] > 0]
    S.wait_all("sp", alltoks)
    S.emit()
    S.close()
    for c in reversed(ctxs):
        c.__exit__(None, None, None)
    return nc


F32 = mybir.dt.float32
BF16 = mybir.dt.bfloat16
AF = mybir.ActivationFunctionType
ALU = mybir.AluOpType
AX = mybir.AxisListType
NT = 1024
SEQ = 8192
HA = 6
HI = 8
KSEL = 256
NIT = 18
SCALE = 128 ** -0.5
NEG = -1.0e30


def core_blocks(c):
    out = []
    for g in range(4):
        out += [16 * g + c, 16 * g + 15 - c]
    return out


def a_tables(c):
    slopes = 2.0 ** (-8.0 * np.arange(1, HA + 1) / HA)
    ar = np.arange(128, dtype=np.float32)
    bias = np.zeros((128, HA, 4, 64), np.float32)
    rbc = np.zeros((128, 4), np.float32)
    for g in range(4):
        rb = 16 * g + 15 - c
        R = 128 * rb + 127
        rbc[:, g] = 128.0 * (rb + 1)
        for h in range(HA):
            for kt in range(64):
                bias[:, h, g, kt] = slopes[h] * (128 * kt + ar - R)
    negb = np.full((128, 16, 2, 128), NEG, np.float32)
    tri = np.where(ar[None, :] <= ar[:, None], 0.0, NEG)
    for tile in range(2):
        qslot = c if tile == 0 else 15 - c
        for i in range(16):
            if i < qslot:
                negb[:, i, tile, :] = 0.0
            elif i == qslot:
                negb[:, i, tile, :] = tri
    sl = np.zeros((1, HA, 128), np.float32)
    for h in range(HA):
        sl[0, h, :] = slopes[h] / SCALE
    ktp1 = np.tile(np.arange(1, 65, dtype=np.float32)[None, :], (128, 1))
    return dict(bias=bias.reshape(128, -1), rbc=rbc, negb=negb.reshape(128, -1), sl=sl.reshape(1, -1), ktp1=ktp1)


def build_pa(GR=4, HR=HA):
    nc = bass.Bass("TRN2", target_bir_lowering=False)
    qT_d = nc.dram_tensor("qT", [768, NT], BF16, kind="ExternalInput").ap()
    kT_d = nc.dram_tensor("kT", [768, SEQ], BF16, kind="ExternalInput").ap()
    v_d = nc.dram_tensor("v", [SEQ, 768], BF16, kind="ExternalInput").ap()
    qiT_d = nc.dram_tensor("qiT", [512, NT], BF16, kind="ExternalInput").ap()
    kiT_d = nc.dram_tensor("kiT", [64, SEQ], BF16, kind="ExternalInput").ap()
    wi_d = nc.dram_tensor("wi", [NT, 8], F32, kind="ExternalInput").ap()
    bias_d = nc.dram_tensor("bias", [128, HA * 4 * 64], F32, kind="ExternalInput").ap()
    rbc_d = nc.dram_tensor("rbc", [128, 4], F32, kind="ExternalInput").ap()
    negb_d = nc.dram_tensor("negb", [128, 16 * 2 * 128], BF16, kind="ExternalInput").ap()
    sl_d = nc.dram_tensor("sl", [1, HA * 128], BF16, kind="ExternalInput").ap()
    ktp1_d = nc.dram_tensor("ktp1", [128, 64], F32, kind="ExternalInput").ap()
    ident_d = nc.dram_tensor("ident", [128, 128], BF16, kind="ExternalInput").ap()
    identf_d = nc.dram_tensor("identf", [128, 128], F32, kind="ExternalInput").ap()
    y_d = nc.dram_tensor("y", [NT, 768], BF16, kind="ExternalOutput").ap()

    S = Sched(nc)
    S.open()
    ctxs = []

    def sb(name, shape, dt):
        c = nc.sbuf_tensor(name, shape, dt)
        t = c.__enter__()
        ctxs.append(c)
        return t

    def ps(name, shape, dt):
        c = nc.psum_tensor(name, shape, dt)
        t = c.__enter__()
        ctxs.append(c)
        return t

    ident = sb("ident_s", [128, 128], BF16)
    identf = sb("identf_s", [128, 128], F32)
    qsg = [sb("qsg%d" % i, [128, HA, 256], BF16) for i in range(2)]
    ks = sb("ks", [128, SEQ], BF16)
    vs2 = [sb("vs%d" % i, [128, 64, 132], BF16) for i in range(2)]
    kis = sb("kis", [64, SEQ], BF16)
    qisg = [sb("qisg%d" % i, [64, HI, 256], BF16) for i in range(2)]
    wi = sb("wi_s", [128, 8, 8], F32)
    wa = sb("wa", [128, 8, 8], F32)
    wsg = sb("wsg", [128, 8, 8], F32)
    bias = sb("bias_s", [128, HA * 4 * 64], F32)
    rbc = sb("rbc_s", [128, 4], F32)
    negb = sb("negb_s", [128, 16, 2, 128], BF16)
    sl = sb("sl_s", [1, HA, 128], BF16)
    ktp1 = sb("ktp1_s", [128, 64], F32)
    I = sb("I", [128, SEQ], F32)
    nsq2 = [sb("nsq%d" % i, [128, SEQ], BF16) for i in range(2)]
    nsT = sb("nsT", [128, 64, 256], BF16)
    rl = [sb("rl%d" % i, [128, 512], F32) for i in range(2)]
    bs = sb("bs", [128, 8 + NIT + 4], F32)
    an = sb("an", [128, 64], F32)
    dcol = sb("dcol", [128, 2, 2], F32)
    drow = sb("drow", [1, 256], BF16)
    pT = [sb("pT%d" % i, [128, 256], BF16) for i in range(3)]
    ysg = [sb("ysg%d" % i, [128, 2, 768], BF16) for i in range(2)]
    fs = [sb("fs%d" % i, [128, 2], F32) for i in range(2)]
    pI = [ps("pI%d" % i, [128, 512], F32) for i in range(2)]
    pTr = ps("pTr", [128, 512], BF16)
    pS = [ps("pS%d" % i, [128, 256], F32) for i in range(2)]
    pO = [ps("pO%d" % i, [128, 2, 132], F32) for i in range(2)]
    pD = ps("pD", [128, 128], F32)

    S.dma("sp", out=ident[:], in_=ident_d[:, :], writes=["ident"])
    S.dma("sp", out=identf[:], in_=identf_d[:, :], writes=["identf"])
    S.dma("sp", out=kis[:], in_=kiT_d[:, :], writes=["kis"])
    S.dma("sp", out=wi[:], in_=wi_d.rearrange("(j p) h -> p j h", p=128), writes=["wi"])
    S.dma("sp", out=bias[:], in_=bias_d[:, :], writes=["bias"])
    S.dma("sp", out=rbc[:], in_=rbc_d[:, :], writes=["rbc"])
    S.dma("sp", out=negb[:].rearrange("p a b c -> p (a b c)"), in_=negb_d[:, :], writes=["negb"])
    S.dma("sp", out=sl[:].rearrange("p a b -> p (a b)"), in_=sl_d[:, :], writes=["sl"])
    S.dma("sp", out=ktp1[:], in_=ktp1_d[:, :], writes=["ktp1"])
    for i in range(2):
        S.op("pool", "memset", vs2[i][:, :, 128:129], 1.0, writes=["vs1_%d" % i])
        S.op("pool", "memset", ysg[i][:], 0.0, writes=["ysg%d" % i])
    S.op("dve", "tensor_scalar", out=wa[:], in0=wi[:], scalar1=-1.0, scalar2=None, op0=ALU.mult, reads=["wi"], writes=["wa"])
    S.op("dve", "tensor_tensor", out=wa[:], in0=wa[:], in1=wi[:], op=ALU.max, reads=["wa", "wi"], writes=["wa"])
    S.op("act", "activation", out=wsg[:], in_=wi[:], func=AF.Sign, reads=["wi"], writes=["wsg"])

    ucnt = 0
    icnt = 0
    ocnt = 0
    def stage_a(g):
        nkt = 16 * g + 16
        NK = 128 * nkt
        nonlocal icnt
        S.dma("sp", out=qisg[g % 2][:], in_=qiT_d.rearrange("(h p) t -> p h t", p=64)[:, :, 256 * g:256 * g + 256],
              writes=["qis%d" % (g % 2)])
        S.dma("sp", out=qsg[g % 2][:], in_=qT_d.rearrange("(h p) t -> p h t", p=128)[:, :, 256 * g:256 * g + 256],
              writes=["qs%d" % (g % 2)])
        for tile in range(2):
            j = 2 * g + tile
            for kc in range(NK // 512):
                for h in range(HI):
                    pi = icnt % 2
                    icnt += 1
                    S.op("pe", "matmul", pI[pi][:], lhsT=qisg[g % 2][:, h, 128 * tile:128 * tile + 128], rhs=kis[:, 512 * kc:512 * kc + 512],
                         start=True, stop=True, reads=["qis%d" % (g % 2), "kis"], writes=["pI%d" % pi])
                    S.op("act", "activation", out=rl[pi][:], in_=pI[pi][:], func=AF.Relu, scale=wa[:, j, h:h + 1],
                         reads=["pI%d" % pi, "wa"], writes=["rl%d" % pi])
                    dst = I[:, 512 * kc:512 * kc + 512]
                    if h == 0:
                        S.op("dve", "tensor_scalar", out=dst, in0=rl[pi][:], scalar1=wsg[:, j, 0:1], scalar2=None,
                             op0=ALU.mult, reads=["rl%d" % pi, "wsg"], writes=["I%d" % kc])
                    else:
                        S.op("dve", "scalar_tensor_tensor", out=dst, in0=rl[pi][:], scalar=wsg[:, j, h:h + 1], in1=dst,
                             op0=ALU.mult, op1=ALU.add, reads=["rl%d" % pi, "wsg", "I%d" % kc], writes=["I%d" % kc])
            Iall = ["I%d" % kc for kc in range(NK // 512)]
            S.op("dve", "tensor_reduce", out=bs[:, 0:1], in_=I[:, 0:NK], axis=AX.X, op=ALU.min, reads=Iall, writes=["bs"])
            S.op("dve", "tensor_tensor", out=I[:, 2048 * g:NK].rearrange("p (a b) -> p a b", a=16),
                 in0=I[:, 2048 * g:NK].rearrange("p (a b) -> p a b", a=16), in1=negb[:, :, tile, :], op=ALU.add,
                 reads=Iall + ["negb"], writes=Iall)
            S.op("dve", "tensor_reduce", out=bs[:, 1:2], in_=I[:, 0:NK], axis=AX.X, op=ALU.max, reads=Iall, writes=["bs"])
            S.op("dve", "tensor_scalar", out=bs[:, 0:1], in0=bs[:, 0:1], scalar1=-1.0, scalar2=None, op0=ALU.add,
                 reads=["bs"], writes=["bs"])
            S.op("dve", "scalar_tensor_tensor", out=bs[:, 8:9], in0=bs[:, 1:2], scalar=1.0, in1=bs[:, 0:1],
                 op0=ALU.add, op1=ALU.subtract, reads=["bs"], writes=["bs"])
            for t in range(1, NIT + 1):
                S.op("dve", "tensor_scalar", out=bs[:, 8 + t:9 + t], in0=bs[:, 7 + t:8 + t], scalar1=0.5, scalar2=None,
                     op0=ALU.mult, reads=["bs"], writes=["bs"])
            for t in range(1, NIT + 1):
                S.op("dve", "tensor_tensor", out=bs[:, 2:3], in0=bs[:, 0:1], in1=bs[:, 8 + t:9 + t], op=ALU.add,
                     reads=["bs"], writes=["bs"])
                S.op("dve", "tensor_scalar", out=nsq2[tile][:, 0:NK], in0=I[:, 0:NK], scalar1=bs[:, 2:3], scalar2=0.0,
                     op0=ALU.is_ge, op1=ALU.add, accum_out=bs[:, 3:4], reads=Iall + ["bs"], writes=["nsq%d" % tile, "bs"])
                S.op("dve", "tensor_scalar", out=bs[:, 4:5], in0=bs[:, 3:4], scalar1=KSEL - 0.5, scalar2=bs[:, 8 + t:9 + t],
                     op0=ALU.is_ge, op1=ALU.mult, reads=["bs"], writes=["bs"])
                S.op("dve", "tensor_tensor", out=bs[:, 0:1], in0=bs[:, 0:1], in1=bs[:, 4:5], op=ALU.add,
                     reads=["bs"], writes=["bs"])
            S.op("dve", "tensor_scalar", out=nsq2[tile][:, 0:NK], in0=I[:, 0:NK], scalar1=bs[:, 0:1], scalar2=NEG,
                 op0=ALU.is_lt, op1=ALU.mult, reads=Iall + ["bs"], writes=["nsq%d" % tile])
            S.op("dve", "tensor_reduce", out=an[:, 0:nkt], in_=nsq2[tile][:, 0:NK].rearrange("p (a b) -> p a b", a=nkt),
                 axis=AX.X, op=ALU.max, reads=["nsq%d" % tile], writes=["an"])
            S.op("dve", "scalar_tensor_tensor", out=an[:, 0:nkt], in0=an[:, 0:nkt], scalar=-1.0, in1=ktp1[:, 0:nkt],
                 op0=ALU.is_ge, op1=ALU.mult, reads=["an", "ktp1"], writes=["an"])
            S.op("dve", "tensor_reduce", out=dcol[:, tile, 0:1], in_=an[:, 0:nkt], axis=AX.X, op=ALU.max, reads=["an"], writes=["dcol%d" % tile])
            S.op("dve", "tensor_scalar", out=dcol[:, tile, 1:2], in0=dcol[:, tile, 0:1], scalar1=-128.0, scalar2=rbc[:, g:g + 1],
                 op0=ALU.mult, op1=ALU.add, reads=["dcol%d" % tile, "rbc"], writes=["dcol%d" % tile])

    def stage_b(g):
        nkt = 16 * g + 16
        NK = 128 * nkt
        for tile in range(2):
            S.op("pe", "transpose", out=pD[0:1, 0:128], in_=dcol[:, tile, 1:2], identity=identf[:],
                 reads=["dcol%d" % tile, "identf"], writes=["pD"])
            S.op("act", "copy", out=drow[0:1, 128 * tile:128 * tile + 128], in_=pD[0:1, 0:128], reads=["pD"], writes=["drow"])
            for k4 in range(nkt // 4):
                for k in range(4):
                    kt = 4 * k4 + k
                    S.op("pe", "transpose", out=pTr[:, 128 * k:128 * k + 128], in_=nsq2[tile][:, 128 * kt:128 * kt + 128],
                         identity=ident[:], reads=["nsq%d" % tile, "ident"], writes=["pTr"])
                S.op("act", "copy", out=nsT[:, 4 * k4:4 * k4 + 4, 128 * tile:128 * tile + 128],
                     in_=pTr[:, 0:512].rearrange("p (k t) -> p k t", k=4), reads=["pTr"], writes=["nsT"])

    def attention(g):
        nkt = 16 * g + 16
        NK = 128 * nkt
        nonlocal ucnt, ocnt
        for h in range(HR):
            vi = (g * HR + h) % 2
            S.dma("sp", out=ks[:, 0:NK], in_=kT_d[128 * h:128 * h + 128, 0:NK], writes=["ks"])
            for q4 in range(g + 1):
                S.dma("sp", out=vs2[vi][:, 16 * q4:16 * q4 + 16, 0:128],
                      in_=v_d[2048 * q4:2048 * q4 + 2048, 128 * h:128 * h + 128].rearrange("(kt p) e -> p kt e", p=128),
                      writes=["vs%d_%d" % (vi, q4)])
            ob = ocnt % 2
            ocnt += 1
            def emit_qk(kt, si):
                S.op("pe", "matmul", pS[si][:], lhsT=ks[:, 128 * kt:128 * kt + 128], rhs=qsg[g % 2][:, h, :],
                     start=True, stop=False, reads=["ks", "qs%d" % (g % 2)], writes=["pS%d" % si])
                S.op("pe", "matmul", pS[si][:], lhsT=sl[0:1, h, :], rhs=drow[0:1, :], start=False, stop=False,
                     reads=["sl", "drow"], writes=["pS%d" % si])
                S.op("pe", "matmul", pS[si][:], lhsT=ident[:], rhs=nsT[:, kt, :], start=False, stop=True,
                     reads=["ident", "nsT"], writes=["pS%d" % si])

            def emit_rest(kt, si, pti):
                bcol = (h * 4 + g) * 64 + kt
                S.op("act", "activation", out=pT[pti][:], in_=pS[si][:], func=AF.Exp, bias=bias[:, bcol:bcol + 1],
                     scale=SCALE, reads=["pS%d" % si, "bias"], writes=["pT%d" % pti])
                for tile in range(2):
                    S.op("pe", "matmul", pO[ob][:, tile, 0:129], lhsT=pT[pti][:, 128 * tile:128 * tile + 128],
                         rhs=vs2[vi][:, kt, 0:129], start=(kt == 0 and tile == 0), stop=(kt == nkt - 1 and tile == 1),
                         reads=["pT%d" % pti, "vs%d_%d" % (vi, kt // 16), "vs1_%d" % vi], writes=["pO%d" % ob])

            for idx in range(nkt + 1):
                if idx < nkt:
                    emit_qk(idx, (ucnt + idx) % 2)
                if idx >= 1:
                    emit_rest(idx - 1, (ucnt + idx - 1) % 2, (ucnt + idx - 1) % 3)
            ucnt += nkt
            for tile in range(2):
                f = fs[tile]
                S.op("dve", "reciprocal", out=f[:, 0:1], in_=pO[ob][:, tile, 128:129], reads=["pO%d" % ob], writes=["fs%d" % tile])
                S.op("dve", "tensor_scalar", out=ysg[g % 2][:, tile, 128 * h:128 * h + 128], in0=pO[ob][:, tile, 0:128],
                     scalar1=f[:, 0:1], scalar2=None, op0=ALU.mult, reads=["pO%d" % ob, "fs%d" % tile], writes=["ysg%d" % (g % 2)])
        S.dma("sp", out=y_d[256 * g:256 * g + 256, :].rearrange("(j p) e -> p j e", p=128), in_=ysg[g % 2][:],
              reads=["ysg%d" % (g % 2)], writes=["yout"])

    stage_a(0)
    stage_b(0)
    for g in range(GR):
        if g + 1 < GR:
            stage_a(g + 1)
        attention(g)
        if g + 1 < GR:
            stage_b(g + 1)
    alltoks = [(k, v_[1]) for k, v_ in S.dma_sems.items() if v_[1] > 0]
    S.wait_all("sp", alltoks)
    S.emit()
    S.close()
    for c in reversed(ctxs):
        c.__exit__(None, None, None)
    return nc


F32 = mybir.dt.float32
BF16 = mybir.dt.bfloat16
AF = mybir.ActivationFunctionType
ALU = mybir.AluOpType
ALPHA = 8.0 ** 0.25
EPS = 1e-5

NT = 1024
D = 2048
FF = 8192
G = 512


def build_p3():
    nc = bass.Bass("TRN2", target_bir_lowering=False)
    xres = nc.dram_tensor("xres", [NT, D], F32, kind="ExternalInput").ap()
    mixT = nc.dram_tensor("mixT", [D, NT], BF16, kind="ExternalInput").ap()
    w_out = nc.dram_tensor("w_out", [D, D], BF16, kind="ExternalInput").ap()
    w_up = nc.dram_tensor("w_up", [D, FF], BF16, kind="ExternalInput").ap()
    w_down = nc.dram_tensor("w_down", [FF, D], BF16, kind="ExternalInput").ap()
    lnp = nc.dram_tensor("lnp", [4, D], F32, kind="ExternalInput").ap()
    ident_d = nc.dram_tensor("identf", [128, 128], F32, kind="ExternalInput").ap()
    x2 = nc.dram_tensor("x2", [NT, D], F32, kind="ExternalOutput").ap()

    S = Sched(nc)
    S.open()
    ctxs = []

    def sb(name, shape, dt):
        c = nc.sbuf_tensor(name, shape, dt)
        t = c.__enter__()
        ctxs.append(c)
        return t

    def ps(name, shape, dt):
        c = nc.psum_tensor(name, shape, dt)
        t = c.__enter__()
        ctxs.append(c)
        return t

    ident = sb("ident_s", [128, 128], F32)
    lnb = sb("lnb", [128, 4, D], F32)
    big = sb("big", [128, 64 * G], BF16)
    wo = big[:].rearrange("p (c d) -> p c d", c=16)
    hT = big[:].rearrange("p (f t) -> p f t", f=64)
    mx = [sb("mx%d" % i, [128, 16, 128], BF16) for i in range(2)]
    xr = [sb("xr%d" % i, [128, D], F32) for i in range(1)]
    r = [sb("r%d" % i, [128, D], F32) for i in range(2)]
    x1 = sb("x1", [128, 4, D], F32)
    x1T = sb("x1T", [128, 16, G], BF16)
    wu = [sb("wu%d" % i, [128, 16, 256], BF16) for i in range(2)]
    wd = [sb("wd%d" % i, [128, 1024], BF16) for i in range(4)]
    sq = [sb("sq%d" % i, [128, G], F32) for i in range(2)]
    st = sb("st", [128, 4, 6], F32)
    mv = sb("mv", [128, 2], F32)
    rstd = sb("rstd", [128, 1], F32)
    nmr = sb("nmr", [128, 1], F32)
    epsb = sb("epsb", [128, 1], F32)
    pacc = [ps("pacc%d" % i, [128, 512], F32) for i in range(8)]

    E = S.eng
    S.dma("sp", out=ident[:], in_=ident_d[:, :], writes=["ident"])
    S.dma("sp", out=lnb[:].rearrange("p a d -> p (a d)"),
                                          in_=lnp.rearrange("a d -> (a d)").partition_broadcast(128),
          writes=["lnb"])
    S.op("pool", "memset", epsb[:], EPS, writes=["epsb"])

    pb = [0]

    def layer_norm(src, srcname, gi, outs):
        for c in range(4):
            S.op("dve", "bn_stats", out=st[:, c, :], in_=src[:, 512 * c:512 * c + 512],
                 reads=[srcname], writes=["st%d" % c])
        S.op("dve", "bn_aggr", out=mv[:], in_=st[:].rearrange("p a b -> p (a b)"),
             reads=["st%d" % c for c in range(4)], writes=["mv"])
        S.op("act", "activation", out=rstd[:], in_=mv[:, 1:2], func=AF.Sqrt, bias=epsb[:], scale=1.0,
             reads=["mv", "epsb"], writes=["rstd"])
        S.op("dve", "reciprocal", out=rstd[:], in_=rstd[:], reads=["rstd"], writes=["rstd"])
        S.op("dve", "tensor_scalar", out=src, in0=src, scalar1=mv[:, 0:1], scalar2=rstd[:],
                                                   op0=ALU.subtract, op1=ALU.mult,
             reads=[srcname, "mv", "rstd"], writes=[srcname])
        S.op("pool", "tensor_tensor", out=src, in0=src, in1=lnb[:, gi, :], op=ALU.mult,
             reads=[srcname, "lnb"], writes=[srcname])
        for (ap, name, eng) in outs:
            S.op(eng, "tensor_tensor", out=ap, in0=src, in1=lnb[:, gi + 1, :], op=ALU.add,
                 reads=[srcname, "lnb"], writes=[name])

    for g in range(NT // G):
        t0 = g * G
        for q in range(4):
            S.dma("pool",
                out=wo[:, 4 * q:4 * q + 4, :],
                in_=w_out[512 * q:512 * q + 512, :].rearrange("(c p) d -> p c d", p=128),
                writes=["wo%d" % q] + ["hT%d" % f for f in range(16 * q, 16 * q + 16)])
        for t in range(4):
            i = t % 2
            S.dma("sp",
                out=mx[i][:], in_=mixT[:, t0 + 128 * t:t0 + 128 * t + 128].rearrange("(c p) t -> p c t", p=128),
                writes=["mx%d" % i])
            S.dma("sp", out=xr[0][:], in_=xres[t0 + 128 * t:t0 + 128 * t + 128, :],
                  writes=["xr0"])
            for dg in range(4):
                for c in range(16):
                    S.op("pe", "matmul",
                        pacc[dg][:], lhsT=mx[i][:, c, :], rhs=wo[:, c, 512 * dg:512 * dg + 512],
                        start=(c == 0), stop=(c == 15),
                        reads=["mx%d" % i, "wo%d" % (c // 4)], writes=["pacc%d" % dg])
                S.op("dve", "scalar_tensor_tensor",
                    out=r[i][:, 512 * dg:512 * dg + 512], in0=xr[0][:, 512 * dg:512 * dg + 512], scalar=ALPHA,
                    in1=pacc[dg][:], op0=ALU.mult, op1=ALU.add,
                    reads=["xr0", "pacc%d" % dg], writes=["r%d" % i])
            layer_norm(r[i][:], "r%d" % i, 0, [(x1[:, t, :], "x1_%d" % t, "pool")])
            for c4 in range(4):
                pt = pacc[4 + (c4 % 2)]
                for k in range(4):
                    c = 4 * c4 + k
                    S.op("pe", "transpose", out=pt[:, 128 * k:128 * k + 128], in_=x1[:, t, 128 * c:128 * c + 128],
                         identity=ident[:], reads=["x1_%d" % t, "ident"], writes=["pacc%d" % (4 + c4 % 2)])
                S.op("act", "copy", out=x1T[:, 4 * c4:4 * c4 + 4, 128 * t:128 * t + 128],
                     in_=pt[:, 0:512].rearrange("p (k t) -> p k t", k=4),
                     reads=["pacc%d" % (4 + c4 % 2)], writes=["x1T_%d" % t])
        for f2 in range(32):
            wi = f2 % 2
            S.dma("pool", out=wu[wi][:], in_=w_up[:, 256 * f2:256 * f2 + 256].rearrange("(c p) f -> p c f", p=128),
                  writes=["wu%d" % wi])
            for fh in range(2):
                f = 2 * f2 + fh
                pa = pacc[f % 4]
                for c in range(16):
                    S.op("pe", "matmul", pa[:], lhsT=wu[wi][:, c, 128 * fh:128 * fh + 128], rhs=x1T[:, c, :],
                         start=(c == 0), stop=(c == 15),
                         reads=["wu%d" % wi] + ["x1T_%d" % t for t in range(4)], writes=["pacc%d" % (f % 4)])
                si = f % 2
                S.op("act", "activation", out=sq[si][:], in_=pa[:], func=AF.Square,
                     reads=["pacc%d" % (f % 4)], writes=["sq%d" % si])
                S.op("dve", "scalar_tensor_tensor", out=hT[:, f, :], in0=pa[:], scalar=0.0, in1=sq[si][:],
                     op0=ALU.is_gt, op1=ALU.mult,
                     reads=["pacc%d" % (f % 4), "sq%d" % si], writes=["hT%d" % f, "wo%d" % (f // 16)])
        for dh in range(2):
            for f in range(64):
                wi = f % 4
                S.dma("pool", out=wd[wi][:], in_=w_down[128 * f:128 * f + 128, 1024 * dh:1024 * dh + 1024],
                      writes=["wd%d" % wi])
                for t in range(4):
                    for d2 in range(2):
                        S.op("pe", "matmul", pacc[2 * t + d2][:], lhsT=hT[:, f, 128 * t:128 * t + 128],
                             rhs=wd[wi][:, 512 * d2:512 * d2 + 512], start=(f == 0), stop=(f == 63),
                             reads=["hT%d" % f, "wd%d" % wi], writes=["pacc%d" % (2 * t + d2)])
            for t in range(4):
                for d2 in range(2):
                    dg = 2 * dh + d2
                    S.op("dve", "scalar_tensor_tensor", out=x1[:, t, 512 * dg:512 * dg + 512],
                         in0=x1[:, t, 512 * dg:512 * dg + 512], scalar=ALPHA, in1=pacc[2 * t + d2][:],
                         op0=ALU.mult, op1=ALU.add, reads=["x1_%d" % t, "pacc%d" % (2 * t + d2)], writes=["x1_%d" % t])
        for t in range(4):
            layer_norm(x1[:, t, :], "x1_%d" % t, 2, [(x1[:, t, :], "x1_%d" % t, "pool")])
            S.dma("sp", out=x2[t0 + 128 * t:t0 + 128 * t + 128, :], in_=x1[:, t, :],
                  reads=["x1_%d" % t], writes=["x2out"])
    alltoks = [(k, v[1]) for k, v in S.dma_sems.items() if v[1] > 0]
    S.wait_all("sp", alltoks)
    S.emit()
    S.close()
    for c in reversed(ctxs):
        c.__exit__(None, None, None)
    return nc


N_CORES = 8
DEPTH = 4
_BF = ml_dtypes.bfloat16
_PROGS = {}


def _prog(name, builder):
    if name not in _PROGS:
        _PROGS[name] = builder()
    return _PROGS[name]


def _run(nc, in_maps):
    res = run_bass_kernel_spmd(nc, in_maps, core_ids=list(range(N_CORES)))
    return res.results


def kernel(x, w_in, conv_m, b_i, b_f, m_norm_g, lam_q1, lam_k1, lam_q2, lam_k2,
           c_norm_g, w_out, ln1_g, ln1_b, w_up, w_down, ln2_g, ln2_b):
    f32 = np.float32
    toks = [np.concatenate([np.arange(128 * b, 128 * b + 128) for b in core_blocks(c)]) for c in range(N_CORES)]
    ident_b = np.eye(128).astype(_BF)
    ident_f = np.eye(128, dtype=f32)
    ar = np.arange(128)
    tri = (ar[:, None] <= ar[None, :]).astype(f32)
    slopes_c = 2.0 ** (-8.0 * np.arange(1, 5) / 4)
    ctab = [c_tables(c, slopes_c, 4) for c in range(N_CORES)]
    atab = [a_tables(c) for c in range(N_CORES)]
    pw = _prog("pw", build_pw)
    rw = _run(pw, [dict(w_in=np.ascontiguousarray(w_in[:, 256 * c:256 * c + 256, :]).reshape(1024, DIN),
                        w_out=np.ascontiguousarray(w_out[:, 256 * c:256 * c + 256, :]).reshape(1024, D),
                        w_up=np.ascontiguousarray(w_up[:, 256 * c:256 * c + 256, :]).reshape(1024, FF),
                        w_down=np.ascontiguousarray(w_down[:, 1024 * c:1024 * c + 1024, :]).reshape(4096, D))
                   for c in range(N_CORES)])

    def wcat(name, l, rows):
        return np.concatenate([rw[c][name][rows * l:rows * l + rows] for c in range(N_CORES)], axis=0)

    p1 = _prog("p1", build_p1)
    pa = _prog("pa", build_pa)
    pb = _prog("pb", build_pb)
    pc = _prog("pc", build_pc)
    p3 = _prog("p3", build_p3)
    xs = [np.ascontiguousarray(x[0][toks[c]]) for c in range(N_CORES)]
    for l in range(DEPTH):
        w_in_l = wcat("w_in_b", l, 256)
        w_out_l = wcat("w_out_b", l, 256)
        w_up_l = wcat("w_up_b", l, 256)
        w_down_l = wcat("w_down_b", l, 1024)
        r1 = _run(p1, [dict(x=xs[c], w_in=w_in_l, ident=ident_b) for c in range(N_CORES)])

        def gather_T(name, rows, dt):
            out = np.empty((rows, SEQ), dt)
            for c in range(N_CORES):
                out[:, toks[c]] = r1[c][name]
            return out

        def gather_tok(name, cols, dt):
            out = np.empty((SEQ, cols), dt)
            for c in range(N_CORES):
                out[toks[c]] = r1[c][name]
            return out

        kaT = gather_T("kaT", 768, _BF)
        va = gather_tok("va", 768, _BF)
        kiT = gather_T("kiT", 64, _BF)
        kcT = gather_T("kcT", 512, _BF)
        vc = gather_tok("vc", 512, _BF)
        qmT = gather_T("qmT", 768, f32)
        kmT = gather_T("kmT", 768, f32)
        vm = gather_tok("vm", 768, _BF)
        om = gather_tok("om", 768, f32)
        ifm = gather_tok("ifm", 12, f32)
        ra = _run(pa, [dict(qT=r1[c]["qaT"], kT=kaT, v=va, qiT=r1[c]["qiT"], kiT=kiT, wi=r1[c]["wi"],
                            bias=atab[c]["bias"], rbc=atab[c]["rbc"], negb=atab[c]["negb"].astype(_BF),
                            sl=atab[c]["sl"].astype(_BF), ktp1=atab[c]["ktp1"], ident=ident_b, identf=ident_f)
                       for c in range(N_CORES)])
        lam_init = 0.8 - 0.6 * math.exp(-0.3 * l)
        lamp = np.stack([lam_q1[l], lam_k1[l], lam_q2[l], lam_k2[l]]).astype(f32)
        cst = np.array([lam_init, 1.0 - lam_init], f32)
        rc = _run(pc, [dict(qT=r1[c]["qcT"], kT=kcT, v=vc, lam=lamp, gc=np.ascontiguousarray(c_norm_g[l]), cst=cst,
                            bias=ctab[c][0], mask=ctab[c][1].astype(_BF), drow=ctab[c][2].astype(_BF), sl=ctab[c][3].astype(_BF),
                            ident=ident_b) for c in range(N_CORES)])
        conv = conv_m[l][:, 0, :]
        inb = []
        for c in range(N_CORES):
            h = c % 6
            cw = np.concatenate([conv[:, 128 * h:128 * h + 128].T, conv[:, 768 + 128 * h:768 + 128 * h + 128].T], axis=1)
            inb.append(dict(qT=np.ascontiguousarray(qmT[128 * h:128 * h + 128]), kT=np.ascontiguousarray(kmT[128 * h:128 * h + 128]),
                            v=np.ascontiguousarray(vm[:, 128 * h:128 * h + 128]), o=np.ascontiguousarray(om[:, 128 * h:128 * h + 128]),
                            ifg=np.ascontiguousarray(np.stack([ifm[:, h], ifm[:, 6 + h]], 1)), cw=np.ascontiguousarray(cw.astype(f32)),
                            bif=np.array([b_i[l][h], b_f[l][h]], f32), gn=np.ascontiguousarray(m_norm_g[l][128 * h:128 * h + 128]),
                            tri=tri, ident=ident_b))
        rb = _run(pb, inb)
        yb = np.concatenate([rb[h]["y"] for h in range(6)], axis=1)
        lnp = np.stack([ln1_g[l], ln1_b[l], ln2_g[l], ln2_b[l]]).astype(f32)
        in3 = []
        for c in range(N_CORES):
            mixed = np.concatenate([ra[c]["y"], yb[toks[c]], rc[c]["y"]], axis=1)
            in3.append(dict(xres=xs[c], mixT=np.ascontiguousarray(mixed.T), w_out=w_out_l,
                            w_up=w_up_l, w_down=w_down_l, lnp=lnp, identf=ident_f))
        r3 = _run(p3, in3)
        xs = [r3[c]["x2"] for c in range(N_CORES)]
    out = np.empty((1, SEQ, D), f32)
    for c in range(N_CORES):
        out[0, toks[c]] = xs[c]
    return out
```
